# Optimizing a Trainium2 kernel written in Bass

```python
import math
import jax, jax.numpy as jnp
from jax import lax
import numpy as np

D_MODEL = 1024
BATCH = 2
SEQ = 8192
DEPTH = 1

MIX_WIDTH = D_MODEL
CONV_WIDTH = MIX_WIDTH // 2
FFT_WIDTH = MIX_WIDTH - CONV_WIDTH
GROUP_DIM = 64
CONV_HEADS = CONV_WIDTH // GROUP_DIM
FFT_GROUPS = FFT_WIDTH // GROUP_DIM
IN_PROJ_WIDTH = 3 * CONV_WIDTH + FFT_WIDTH
CONV_K = 3
MEM_LEN = 256
XATTN_HEADS = 4
XATTN_HEAD_DIM = D_MODEL // XATTN_HEADS
N_EXPERTS = 32
TOP_K = 4
D_EXPERT = D_MODEL
SWIGLU_LIMIT = 7.0
SWIGLU_ALPHA = 1.702
ROW_BLOCK = 128
EPS = 1e-5

kernel_name = "hymba_conv_fnet_xmem_moe_encoder"


def rmsnorm(x, g):
    xf = x.astype(jnp.float32)
    y = xf * lax.rsqrt(jnp.mean(xf * xf, axis=-1, keepdims=True) + EPS)
    return (y * g.astype(jnp.float32)).astype(x.dtype)


def short_conv3(u, w):
    up = jnp.pad(u, ((0, 0), (1, 1), (0, 0)))
    return w[0] * up[:, :-2] + w[1] * up[:, 1:-1] + w[2] * up[:, 2:]


def hybrid_mixer(h, w_in, conv_w, g_conv_out, g_fft_out, w_out):
    bsz, s, _ = h.shape
    z = h @ w_in
    b_gate = z[..., :CONV_WIDTH]
    c_gate = z[..., CONV_WIDTH:2 * CONV_WIDTH]
    v = z[..., 2 * CONV_WIDTH:3 * CONV_WIDTH]
    u = z[..., 3 * CONV_WIDTH:]
    y_conv = b_gate * short_conv3(c_gate * v, conv_w)
    uf = u.astype(jnp.float32).reshape(bsz, s, FFT_GROUPS, GROUP_DIM)
    y_fft = jnp.fft.fftn(uf, axes=(1, 3), norm="ortho").real
    y_fft = y_fft.reshape(bsz, s, FFT_WIDTH).astype(h.dtype)
    y = jnp.concatenate([rmsnorm(y_conv, g_conv_out), rmsnorm(y_fft, g_fft_out)], axis=-1)
    return y @ w_out


def memory_cross_attention(h, mem_n, w_q, w_k, w_v, w_o):
    bsz, s, d = h.shape
    m = mem_n.shape[1]
    q = (h @ w_q).reshape(bsz, s, XATTN_HEADS, XATTN_HEAD_DIM)
    k = (mem_n @ w_k).reshape(bsz, m, XATTN_HEADS, XATTN_HEAD_DIM)
    v = (mem_n @ w_v).reshape(bsz, m, XATTN_HEADS, XATTN_HEAD_DIM)
    scores = jnp.einsum('bshd,bmhd->bhsm', q, k).astype(jnp.float32) * (XATTN_HEAD_DIM ** -0.5)
    p = jax.nn.softmax(scores, axis=-1).astype(v.dtype)
    o = jnp.einsum('bhsm,bmhd->bshd', p, v).reshape(bsz, s, d)
    return o @ w_o


def moe_ffn(h, w_router, b_router, w_gate_up, b_gate_up, w_down, b_down):
    bsz, s, d = h.shape
    t = bsz * s
    hf = h.reshape(t, d)
    logits = (hf @ w_router).astype(jnp.float32) + b_router.astype(jnp.float32)
    top_v, top_i = lax.top_k(logits, TOP_K)
    top_w = jax.nn.softmax(top_v, axis=-1)
    n_assign = t * TOP_K
    flat_e = top_i.reshape(n_assign)
    flat_tok = jnp.repeat(jnp.arange(t, dtype=jnp.int32), TOP_K)
    flat_w = top_w.reshape(n_assign)
    order = jnp.argsort(flat_e)
    sorted_e = flat_e[order]
    counts = jnp.bincount(flat_e, length=N_EXPERTS)
    padded = ((counts + ROW_BLOCK - 1) // ROW_BLOCK) * ROW_BLOCK
    start = jnp.cumsum(counts) - counts
    pad_end = jnp.cumsum(padded)
    pad_start = pad_end - padded
    dest = pad_start[sorted_e] + jnp.arange(n_assign) - start[sorted_e]
    n_blocks = -(-n_assign // ROW_BLOCK) + N_EXPERTS
    n_rows = n_blocks * ROW_BLOCK
    row_tok = jnp.full((n_rows,), t, dtype=jnp.int32).at[dest].set(flat_tok[order])
    row_w = jnp.zeros((n_rows,), jnp.float32).at[dest].set(flat_w[order])
    block_e = jnp.minimum(
        jnp.searchsorted(pad_end, jnp.arange(n_blocks) * ROW_BLOCK, side='right'), N_EXPERTS - 1)
    h_pad = jnp.concatenate([hf, jnp.zeros((1, d), hf.dtype)], axis=0)
    xs = h_pad[row_tok].reshape(n_blocks, ROW_BLOCK, d)

    def expert_block(args):
        xb, e = args
        gu = xb @ w_gate_up[e] + b_gate_up[e]
        gate = jnp.minimum(gu[:, :D_EXPERT], SWIGLU_LIMIT)
        up = jnp.clip(gu[:, D_EXPERT:], -SWIGLU_LIMIT, SWIGLU_LIMIT)
        glu = gate * jax.nn.sigmoid(SWIGLU_ALPHA * gate)
        return ((up + 1.0) * glu) @ w_down[e] + b_down[e]

    out = lax.map(expert_block, (xs, block_e)).reshape(n_rows, d)
    y = jnp.zeros((t + 1, d), out.dtype).at[row_tok].add(out * row_w[:, None].astype(out.dtype))
    return y[:t].reshape(bsz, s, d)


def setup_inputs(seed: int = 0) -> dict:
    key = jax.random.key(seed)
    ks = jax.random.split(key, 24)
    f32 = jnp.float32

    def nrm(k, shape, scale):
        return jax.random.normal(k, shape, f32) * scale

    def gain(k, shape):
        return 1.0 + 0.02 * jax.random.normal(k, shape, f32)

    L = DEPTH
    return {
        "x": nrm(ks[0], (BATCH, SEQ, D_MODEL), 1.0),
        "mem": nrm(ks[1], (BATCH, MEM_LEN, D_MODEL), 1.0),
        "norm_mix": gain(ks[2], (L, D_MODEL)),
        "w_in": nrm(ks[3], (L, D_MODEL, IN_PROJ_WIDTH), D_MODEL ** -0.5),
        "conv_w": nrm(ks[4], (L, CONV_K, CONV_WIDTH), CONV_K ** -0.5),
        "g_conv_out": gain(ks[5], (L, CONV_WIDTH)),
        "g_fft_out": gain(ks[6], (L, FFT_WIDTH)),
        "w_out": nrm(ks[7], (L, MIX_WIDTH, D_MODEL), MIX_WIDTH ** -0.5),
        "norm_xattn": gain(ks[8], (L, D_MODEL)),
        "norm_mem": gain(ks[9], (L, D_MODEL)),
        "w_q": nrm(ks[10], (L, D_MODEL, D_MODEL), D_MODEL ** -0.5),
        "w_k": nrm(ks[11], (L, D_MODEL, D_MODEL), D_MODEL ** -0.5),
        "w_v": nrm(ks[12], (L, D_MODEL, D_MODEL), D_MODEL ** -0.5),
        "w_o": nrm(ks[13], (L, D_MODEL, D_MODEL), D_MODEL ** -0.5),
        "norm_ffn": gain(ks[14], (L, D_MODEL)),
        "w_router": nrm(ks[15], (L, D_MODEL, N_EXPERTS), D_MODEL ** -0.5),
        "b_router": nrm(ks[16], (L, N_EXPERTS), 0.01),
        "w_gate_up": nrm(ks[17], (L, N_EXPERTS, D_MODEL, 2 * D_EXPERT), D_MODEL ** -0.5),
        "b_gate_up": nrm(ks[18], (L, N_EXPERTS, 2 * D_EXPERT), 0.02),
        "w_down": nrm(ks[19], (L, N_EXPERTS, D_EXPERT, D_MODEL), D_EXPERT ** -0.5),
        "b_down": nrm(ks[20], (L, N_EXPERTS, D_MODEL), 0.02),
        "norm_final": gain(ks[21], (D_MODEL,)),
    }


def reference(x, mem, norm_mix, w_in, conv_w, g_conv_out, g_fft_out, w_out,
              norm_xattn, norm_mem, w_q, w_k, w_v, w_o, norm_ffn, w_router, b_router,
              w_gate_up, b_gate_up, w_down, b_down, norm_final):
    for l in range(DEPTH):
        h = rmsnorm(x, norm_mix[l])
        x = x + hybrid_mixer(h, w_in[l], conv_w[l], g_conv_out[l], g_fft_out[l], w_out[l])
        h = rmsnorm(x, norm_xattn[l])
        mem_n = rmsnorm(mem, norm_mem[l])
        x = x + memory_cross_attention(h, mem_n, w_q[l], w_k[l], w_v[l], w_o[l])
        h = rmsnorm(x, norm_ffn[l])
        x = x + moe_ffn(h, w_router[l], b_router[l], w_gate_up[l], b_gate_up[l], w_down[l], b_down[l])
    return rmsnorm(x, norm_final)
```

```python
import numpy as np
from contextlib import ExitStack
import concourse.bass as bass
import concourse.mybir as mybir
from concourse.bass_utils import run_bass_kernel_spmd

F32 = mybir.dt.float32
BF16 = mybir.dt.bfloat16
I32 = mybir.dt.int32
ALU = mybir.AluOpType
AF = mybir.ActivationFunctionType
AX = mybir.AxisListType

D = 1024
SEQ = 8192
TOK = 2048
NT = TOK // 128
NE = 32
CAP = 384
NSLOT = NE * CAP
EPS = 1e-5


class Sched:
    def __init__(self, nc, es):
        self.nc = nc
        self.es = es
        self.eng = {"pe": nc.tensor, "act": nc.scalar, "dve": nc.vector, "pool": nc.gpsimd, "sp": nc.sync}
        self.sem = {k: es.enter_context(nc.semaphore("c_" + k)) for k in self.eng}
        self.cnt = {k: 0 for k in self.eng}
        self.waited = {k: {} for k in self.eng}
        self.dsem = {}
        self.dcnt = {}
        self.res = {}
        self.nobar = set()
        self.semname = {}

    def _wait(self, e, ev):
        if ev is None:
            return
        s, v, owner = ev
        if owner == "pe" and e == "pe":
            return
        w = self.waited[e]
        if w.get(id(s), 0) >= v:
            return
        self.eng[e].wait_ge(s, v)
        w[id(s)] = v

    def _deps(self, e, reads, writes):
        for r in reads:
            st = self.res.get(r)
            if st:
                self._wait(e, st[0])
        for wname in writes:
            st = self.res.get(wname)
            if st:
                self._wait(e, st[0])
                for ev in list(st[1].values()):
                    self._wait(e, ev)

    def _record(self, ev, reads, writes):
        for r in reads:
            st = self.res.setdefault(r, [None, {}])
            old = st[1].get(id(ev[0]))
            if old is None or old[1] < ev[1]:
                st[1][id(ev[0])] = ev
        for wname in writes:
            self.res[wname] = [ev, {}]

    def op(self, e, fn, reads=(), writes=(), inc=True):
        self._deps(e, reads, writes)
        ins = fn(self.eng[e])
        if inc:
            self.cnt[e] += 1
            ins.then_inc(self.sem[e], 1)
            ev = (self.sem[e], self.cnt[e], e)
        else:
            ev = (self.sem[e], self.cnt[e] + 1, e)
        self._record(ev, reads, writes)
        return ins

    def dma(self, q, key, fn, reads=(), writes=()):
        self._deps(q, reads, writes)
        if key not in self.dsem:
            self.dsem[key] = self.es.enter_context(self.nc.semaphore("d_" + key))
            self.dcnt[key] = 0
        ins = fn(self.eng[q])
        self.dcnt[key] += 16
        ins.then_inc(self.dsem[key], 16)
        ev = (self.dsem[key], self.dcnt[key], "dma")
        self._record(ev, reads, writes)
        return ins

    def barrier(self):
        evs = [(self.sem[o], self.cnt[o], o) for o in self.eng if self.cnt[o] > 0]
        evs += [(self.dsem[k], self.dcnt[k], "dma") for k in self.dsem if k not in self.nobar]
        for e in self.eng:
            for ev in evs:
                if ev[2] == e and e != "pe":
                    continue
                if ev[2] == "pe" and e == "pe":
                    continue
                self._wait(e, ev)
        self.res = {k: v for k, v in self.res.items()}

    def finish(self, q, names):
        for n in names:
            st = self.res.get(n)
            if st:
                self._wait(q, st[0])
                for ev in list(st[1].values()):
                    self._wait(q, ev)


def build(stage="full", dbg=False):
    nc = bass.Bass("TRN2", target_bir_lowering=False)
    es = ExitStack()

    def din(name, shape, dt=F32):
        return nc.dram_tensor(name, list(shape), dt, kind="ExternalInput").ap()

    def dscr(name, shape, dt):
        return nc.dram_tensor(name, list(shape), dt, kind="Internal").ap()

    xrot = din("xrot", [SEQ, D])
    xhalo = din("xhalo", [128, D])
    memb = din("memb", [256, D])
    gvec = din("gvec", [5, D])
    w_in = din("w_in", [D, 2048])
    cw = din("cw", [128, 4, 3])
    gcf = din("gcf", [128, 8])
    w_out = din("w_out", [D, D])
    w_q = din("w_q", [D, D]); w_k = din("w_k", [D, D]); w_v = din("w_v", [D, D]); w_o = din("w_o", [D, D])
    w_r = din("w_r", [D, NE])
    b_r = din("b_r", [1, NE])
    f1c = din("f1c", [128, 128]); f1s = din("f1s", [128, 128])
    f3 = din("f3", [128, 128, 32])
    bdc = din("bdc", [128, 128]); bds = din("bds", [128, 128])
    ebase = din("ebase", [128, NE])
    full = stage == "full"
    if full:
        w_gu = din("w_gu", [NE, D, 2048])
        b_gu = din("b_gu", [128, NE, 16])
        w_dn = din("w_dn", [NE, D, D])
        b_dn = din("b_dn", [NE, D])
    out = nc.dram_tensor("out", [TOK, D], F32, kind="ExternalOutput").ap()

    U_d = dscr("U_d", [SEQ, 512], BF16)
    A_d = dscr("A_d", [2, 64, 128, 512], BF16)
    X1_d = dscr("X1_d", [TOK, D], F32)
    X2_d = dscr("X2_d", [TOK, D], F32)
    Xd = dscr("Xd", [NSLOT, D], BF16)
    O_d = dscr("O_d", [NSLOT, D], F32)

    S = Sched(nc, es)
    op, dma = S.op, S.dma

    def sb(name, shape, dt, stack):
        return stack.enter_context(nc.sbuf_tensor(name, list(shape), dt))

    def ps(name, shape, dt, stack):
        return stack.enter_context(nc.psum_tensor(name, list(shape), dt))

    identf = sb("identf", [128, 128], F32, es)
    identb = sb("identb", [128, 128], BF16, es)
    onesb = sb("onesb", [128, 128], BF16, es)
    ltri = sb("ltri", [128, 128], BF16, es)
    ltrif = sb("ltrif", [128, 128], F32, es)
    epsb = sb("epsb", [128, 1], F32, es)
    gv = sb("gv", [128, 2, D], F32, es)
    GSLOT = {0: 0, 2: 0, 1: 1, 3: 0, 4: 1}

    def load_gain(g):
        sl = GSLOT[g]
        dma("sp", f"gv{sl}", lambda e: e.dma_start(out=gv[:, sl, :], in_=gvec[g:g + 1, :].partition_broadcast(128)), writes=[f"gv{sl}"])
    op("pool", lambda e: e.memset(identf[:], 0.0), writes=["identf"])
    op("pool", lambda e: e.affine_select(out=identf[:], in_=identf[:], pattern=[[-1, 128]], compare_op=ALU.not_equal,
                                          fill=1.0, base=0, channel_multiplier=1), reads=["identf"], writes=["identf"])
    op("dve", lambda e: e.tensor_copy(out=identb[:], in_=identf[:]), reads=["identf"], writes=["identb"])
    op("dve", lambda e: e.memset(onesb[:], 1.0), writes=["onesb"])
    op("dve", lambda e: e.memset(epsb[:], EPS), writes=["epsb"])
    op("pool", lambda e: e.memset(ltrif[:], 1.0), writes=["ltrif"])
    op("pool", lambda e: e.affine_select(out=ltrif[:], in_=ltrif[:], pattern=[[1, 128]], compare_op=ALU.is_gt,
                                          fill=0.0, base=0, channel_multiplier=-1), reads=["ltrif"], writes=["ltrif"])
    op("dve", lambda e: e.tensor_copy(out=ltri[:], in_=ltrif[:]), reads=["ltrif"], writes=["ltri"])
    load_gain(0)

    zt = sb("zt", [128, 2, D], BF16, es)
    op("pool", lambda e: e.memset(zt[:], 0.0), writes=["zt"])
    dbg_outs = {}

    def dump(name, shape, dt, src_ap, reads):
        if not dbg:
            return
        t = nc.dram_tensor("dbg_" + name, list(shape), dt, kind="ExternalOutput").ap()
        dbg_outs[name] = t
        dma("sp", "dbg_" + name, lambda e: e.dma_start(out=t, in_=src_ap), reads=reads, writes=["dbg_" + name])

    def rmsnorm_tile(xt, xname, gi, hb, hname, ss, rs, junk, hf=None, hfname=None):
        op("act", lambda e: e.activation(out=junk[:], in_=xt, func=AF.Square, accum_out=ss[:, 0:1]),
           reads=[xname], writes=["junk", "ss"])
        op("act", lambda e: e.activation(out=rs[:, 0:1], in_=ss[:, 0:1], func=AF.Sqrt, bias=epsb[:, 0:1], scale=1.0 / D),
           reads=["ss", "epsb"], writes=["rs"])
        op("dve", lambda e: e.reciprocal(out=rs[:, 0:1], in_=rs[:, 0:1]), reads=["rs"], writes=["rs"])
        if hf is not None:
            op("dve", lambda e: e.scalar_tensor_tensor(out=hf, in0=xt, scalar=rs[:, 0:1], in1=gv[:, GSLOT[gi], :], op0=ALU.mult, op1=ALU.mult),
               reads=[xname, "rs", f"gv{GSLOT[gi]}"], writes=[hfname])
            op("pool", lambda e: e.tensor_copy(out=hb, in_=hf), reads=[hfname], writes=[hname])
        else:
            op("dve", lambda e: e.scalar_tensor_tensor(out=hb, in0=xt, scalar=rs[:, 0:1], in1=gv[:, GSLOT[gi], :], op0=ALU.mult, op1=ALU.mult),
               reads=[xname, "rs", f"gv{GSLOT[gi]}"], writes=[hname])

    def load_w(name, src2d, dst, ncols, q="pool"):
        dma(q, name, lambda e: e.dma_start(out=dst, in_=src2d.rearrange("(dc p) f -> p dc f", p=128)), writes=[name])

    with ExitStack() as sA:
        ynT = sb("ynT", [128, 8, TOK], BF16, sA)
        gcf_sb = sb("gcf_sb", [128, 8], F32, sA)
        dma("sp", "gcf", lambda e: e.dma_start(out=gcf_sb[:], in_=gcf), writes=["gcf"])
        S.barrier()
        with ExitStack() as sA0:
            BT = sb("BT", [128, 4, TOK], F32, sA0)
            CVx = sb("CVx", [128, 4, TOK + 2], F32, sA0)
            S.barrier()
            with ExitStack() as s1:
                Win = sb("Win", [128, 8, 2048], BF16, s1)
                load_w("Win", w_in, Win[:], 2048)
                if full:
                    for r in range(NSLOT // 256):
                        dma("pool", "zfill", lambda e, r=r: e.dma_start(out=Xd[r * 256:(r + 1) * 256, :].rearrange("(p n) d -> p n d", n=2), in_=zt[:]),
                            reads=["zt"], writes=[])
                    S.res["Xd"] = [(S.dsem["zfill"], S.dcnt["zfill"], "dma"), {}]
                hT4 = [sb(f"hT4_{i}", [128, 8, 512], BF16, s1) for i in range(2)]
                xb = [sb(f"xb_{i}", [128, D], F32, s1) for i in range(3)]
                hb = [sb(f"hb_{i}", [128, D], BF16, s1) for i in range(2)]
                ub = [sb(f"ub_{i}", [128, 512], BF16, s1) for i in range(2)]
                junk = sb("junk", [128, D], BF16, s1)
                ss = sb("ss", [128, 1], F32, s1)
                rs = sb("rs", [128, 1], F32, s1)
                Ctmp = sb("Ctmp", [128, 4, 512], F32, s1)
                hTh = sb("hTh", [128, 8, 128], BF16, s1)
                Chs = sb("Chs", [128, 8], F32, s1)
                CVh = sb("CVh", [128, 8], F32, s1)
                psT = [ps(f"psT_{i}", [128, 8, 128], BF16, s1) for i in range(2)]
                psU = [ps(f"psU_{i}", [128, 512], F32, s1) for i in range(2)]
                psZ = [ps(f"psZ_{i}", [128, 512], F32, s1) for i in range(3)]
                psH = ps("psH", [128, 16], F32, s1)

                def norm_and_transpose(src_ap, i, dstT, dstname, dst_cols):
                    k3, k2 = i % 3, i % 2
                    dma("sp", f"xb{k3}", lambda e: e.dma_start(out=xb[k3][:], in_=src_ap), writes=[f"xb{k3}"])
                    rmsnorm_tile(xb[k3][:], f"xb{k3}", 0, hb[k2][:], f"hb{k2}", ss, rs, junk)
                    for dc in range(8):
                        op("pe", lambda e, dc=dc: e.transpose(out=psT[k2][:, dc, :], in_=hb[k2][:, dc * 128:(dc + 1) * 128], identity=identb[:]),
                           reads=[f"hb{k2}", "identb"], writes=[f"psT{k2}"], inc=(dc == 7))
                    op("act", lambda e: e.copy(out=dstT[:, :, dst_cols], in_=psT[k2][:]), reads=[f"psT{k2}"], writes=[dstname])

                norm_and_transpose(xhalo, 0, hTh, "hTh", slice(0, 128))
                for j in range(8):
                    for dc in range(8):
                        op("pe", lambda e, j=j, dc=dc: e.matmul(out=psH[:, 2 * j:2 * j + 2], lhsT=Win[:, dc, 512 + j * 128:512 + (j + 1) * 128],
                                                               rhs=hTh[:, dc, 0:2], start=(dc == 0), stop=(dc == 7)),
                           reads=["Win", "hTh"], writes=["psH"], inc=(dc == 7))
                op("act", lambda e: e.copy(out=Chs[:], in_=psH[:, 0:8]), reads=["psH"], writes=["Chs"])
                op("dve", lambda e: e.tensor_tensor(out=CVh[:], in0=psH[:, 8:16], in1=Chs[:], op=ALU.mult), reads=["psH", "Chs"], writes=["CVh"])
                CVh3 = CVh[:].rearrange("p (c t) -> p c t", t=2)
                op("dve", lambda e: e.tensor_copy(out=CVx[:, :, 0:1], in_=CVh3[:, :, 0:1]), reads=["CVh"], writes=["CVxh0"])
                op("dve", lambda e: e.tensor_copy(out=CVx[:, :, TOK + 1:TOK + 2], in_=CVh3[:, :, 1:2]), reads=["CVh"], writes=["CVxh1"])

                def a1_s0(i):
                    k3, k2 = (i + 1) % 3, (i + 1) % 2
                    dma("sp", f"xb{k3}", lambda e: e.dma_start(out=xb[k3][:], in_=xrot[i * 128:(i + 1) * 128, :]), writes=[f"xb{k3}"])
                    rmsnorm_tile(xb[k3][:], f"xb{k3}", 0, hb[k2][:], f"hb{k2}", ss, rs, junk)

                def a1_s1(i):
                    k2 = (i + 1) % 2
                    grp, t = i // 4, i % 4
                    g2 = grp % 2
                    for dc in range(8):
                        op("pe", lambda e, dc=dc: e.transpose(out=psT[k2][:, dc, :], in_=hb[k2][:, dc * 128:(dc + 1) * 128], identity=identb[:]),
                           reads=[f"hb{k2}", "identb"], writes=[f"psT{k2}"], inc=(dc == 7))
                    op("act", lambda e: e.copy(out=hT4[g2][:, :, t * 128:(t + 1) * 128], in_=psT[k2][:]), reads=[f"psT{k2}"], writes=[f"hT4_{g2}"])

                def a1_s2(i):
                    grp, t = i // 4, i % 4
                    g2 = grp % 2
                    u2 = i % 2
                    for dc in range(8):
                        op("pe", lambda e, dc=dc: e.matmul(out=psU[u2][:], lhsT=hT4[g2][:, dc, t * 128:(t + 1) * 128], rhs=Win[:, dc, 1536:2048],
                                                          start=(dc == 0), stop=(dc == 7)),
                           reads=["Win", f"hT4_{g2}"], writes=[f"psU{u2}"], inc=(dc == 7))
                    op("dve", lambda e: e.tensor_copy(out=ub[u2][:], in_=psU[u2][:]), reads=[f"psU{u2}"], writes=[f"ub{u2}"])
                    dma("pool", f"ubst{u2}", lambda e: e.dma_start(out=U_d[i * 128:(i + 1) * 128, :], in_=ub[u2][:]), reads=[f"ub{u2}"], writes=[f"U_d{i}"])
                    if t == 3 and grp < 4:
                        for j in range(12):
                            z3 = j % 3
                            for dc in range(8):
                                op("pe", lambda e, j=j, dc=dc: e.matmul(out=psZ[z3][:], lhsT=Win[:, dc, j * 128:(j + 1) * 128], rhs=hT4[g2][:, dc, :],
                                                                       start=(dc == 0), stop=(dc == 7)),
                                   reads=["Win", f"hT4_{g2}"], writes=[f"psZ{z3}"], inc=(dc == 7))
                            cols = slice(grp * 512, (grp + 1) * 512)
                            if j < 4:
                                op("act", lambda e, j=j: e.copy(out=BT[:, j, cols], in_=psZ[z3][:]), reads=[f"psZ{z3}"], writes=[f"BT{j}"])
                            elif j < 8:
                                op("act", lambda e, j=j: e.copy(out=Ctmp[:, j - 4, :], in_=psZ[z3][:]), reads=[f"psZ{z3}"], writes=[f"Ctmp{j - 4}"])
                            else:
                                op("dve", lambda e, j=j: e.tensor_tensor(out=CVx[:, j - 8, 1 + grp * 512:1 + (grp + 1) * 512], in0=psZ[z3][:], in1=Ctmp[:, j - 8, :], op=ALU.mult),
                                   reads=[f"psZ{z3}", f"Ctmp{j - 8}"], writes=[f"CVx{j - 8}"])

                for step in range(64 + 2):
                    if step < 64:
                        a1_s0(step)
                    if 0 <= step - 1 < 64:
                        a1_s1(step - 1)
                    if 0 <= step - 2 < 64:
                        a1_s2(step - 2)
            S.barrier()
            with ExitStack() as s2:
                cw_sb = sb("cw_sb", [128, 4, 3], F32, s2)
                dma("sp", "cw", lambda e: e.dma_start(out=cw_sb[:], in_=cw), writes=["cw"])
                T1 = sb("T1", [128, TOK], F32, s2)
                sq = [sb(f"sq_{i}", [128, 512], BF16, s2) for i in range(2)]
                rstd = sb("rstd", [128, TOK], F32, s2)
                psN = [ps(f"psN_{i}", [128, 512], F32, s2) for i in range(2)]
                for c in range(4):
                    rd = [f"CVx{c}", "CVxh0", "CVxh1", "cw"]
                    op("dve", lambda e, c=c: e.tensor_scalar(out=T1[:], in0=CVx[:, c, 0:TOK], scalar1=cw_sb[:, c, 0:1], scalar2=None, op0=ALU.mult),
                       reads=rd, writes=["T1"])
                    op("dve", lambda e, c=c: e.scalar_tensor_tensor(out=T1[:], in0=CVx[:, c, 1:TOK + 1], scalar=cw_sb[:, c, 1:2], in1=T1[:], op0=ALU.mult, op1=ALU.add),
                       reads=rd + ["T1"], writes=["T1"])
                    op("dve", lambda e, c=c: e.scalar_tensor_tensor(out=T1[:], in0=CVx[:, c, 2:TOK + 2], scalar=cw_sb[:, c, 2:3], in1=T1[:], op0=ALU.mult, op1=ALU.add),
                       reads=rd + ["T1"], writes=["T1"])
                    op("dve", lambda e, c=c: e.tensor_tensor(out=BT[:, c, :], in0=T1[:], in1=BT[:, c, :], op=ALU.mult), reads=["T1", f"BT{c}"], writes=[f"BT{c}"])

                def branch_norm(Y, ynames, goff):
                    for tb in range(4):
                        cols = slice(tb * 512, (tb + 1) * 512)
                        n2 = tb % 2
                        for c in range(4):
                            s2i = (tb * 4 + c) % 2
                            op("act", lambda e, c=c: e.activation(out=sq[s2i][:], in_=Y[:, c, cols], func=AF.Square), reads=[ynames[c]], writes=[f"sq{s2i}"])
                            op("pe", lambda e, c=c: e.matmul(out=psN[n2][:], lhsT=onesb[:], rhs=sq[s2i][:], start=(c == 0), stop=(c == 3)),
                               reads=["onesb", f"sq{s2i}"], writes=[f"psN{n2}"])
                        op("act", lambda e: e.activation(out=rstd[:, cols], in_=psN[n2][:], func=AF.Sqrt, bias=epsb[:, 0:1], scale=1.0 / 512),
                           reads=[f"psN{n2}", "epsb"], writes=[f"rstd{tb}"])
                        op("dve", lambda e: e.reciprocal(out=rstd[:, cols], in_=rstd[:, cols]), reads=[f"rstd{tb}"], writes=[f"rstd{tb}"])
                        for c in range(4):
                            op("dve", lambda e, c=c: e.scalar_tensor_tensor(out=ynT[:, goff + c, cols], in0=Y[:, c, cols], scalar=gcf_sb[:, goff + c:goff + c + 1],
                                                                          in1=rstd[:, cols], op0=ALU.mult, op1=ALU.mult),
                               reads=[ynames[c], "gcf", f"rstd{tb}"], writes=[f"ynT{goff + c}"])

                branch_norm(BT, [f"BT{c}" for c in range(4)], 0)
                if dbg:
                    dump("yc", [128, 4, TOK], F32, BT[:], [f"BT{c}" for c in range(4)])
        S.barrier()
        with ExitStack() as s3:
            Us = sb("Us", [128, 64, 512], BF16, s3)
            f1 = sb("f1", [128, 2, 128], BF16, s3)
            dma("pool", "f1", lambda e: e.dma_start(out=f1[:, 0, :], in_=f1c), writes=["f1"])
            dma("pool", "f1", lambda e: e.dma_start(out=f1[:, 1, :], in_=f1s), writes=["f1"])
            Ast = [sb(f"Ast_{i}", [128, 2, 4, 512], BF16, s3) for i in range(2)]
            psA = [ps(f"psA_{i}", [128, 512], F32, s3) for i in range(4)]
            for h in range(4):
                dma("sp", f"Us{h}", lambda e, h=h: e.dma_start(out=Us[:, h * 16:(h + 1) * 16, :],
                                                          in_=U_d.rearrange("(s1 s2) c -> s1 s2 c", s2=64)[:, h * 16:(h + 1) * 16, :]),
                    reads=[f"U_d{i}" for i in range(64)], writes=[f"Us{h}"])
            A_v = A_d.rearrange("r s k c -> k r s c")
            for sblk in range(16):
                a2 = sblk % 2
                for sl in range(4):
                    s2_ = sblk * 4 + sl
                    for ri in range(2):
                        p4 = (s2_ * 2 + ri) % 4
                        op("pe", lambda e, ri=ri, s2_=s2_: e.matmul(out=psA[p4][:], lhsT=f1[:, ri, :], rhs=Us[:, s2_, :], start=True, stop=True),
                           reads=["f1", f"Us{s2_ // 16}"], writes=[f"psA{p4}"])
                        if ri == 0:
                            op("act", lambda e, sl=sl: e.copy(out=Ast[a2][:, 0, sl, :], in_=psA[p4][:]), reads=[f"psA{p4}"], writes=[f"Ast{a2}"])
                        else:
                            op("dve", lambda e, sl=sl: e.tensor_copy(out=Ast[a2][:, 1, sl, :], in_=psA[p4][:]), reads=[f"psA{p4}"], writes=[f"Ast{a2}"])
                for ri in range(2):
                    dma("pool", f"Ast{a2}_{ri}", lambda e, ri=ri, sblk=sblk: e.dma_start(out=A_v[:, ri, sblk * 4:(sblk + 1) * 4, :], in_=Ast[a2][:, ri, :, :]),
                        reads=[f"Ast{a2}"], writes=[f"A_d{sblk}_{ri}"])
        S.barrier()
        with ExitStack() as s4:
            f3_sb = sb("f3_sb", [128, 128, 32], BF16, s4)
            dma("pool", "f3", lambda e: e.dma_start(out=f3_sb[:], in_=f3), writes=["f3"])
            bd_sb = sb("bd_sb", [128, 2, 128], BF16, s4)
            dma("pool", "bd", lambda e: e.dma_start(out=bd_sb[:, 0, :], in_=bdc), writes=["bd"])
            dma("pool", "bd", lambda e: e.dma_start(out=bd_sb[:, 1, :], in_=bds), writes=["bd"])
            Ach = [sb(f"Ach_{i}", [128, 16, 512], BF16, s4) for i in range(2)]
            XT = sb("XT", [128, 4, 128, 32], BF16, s4)
            yf = sb("yf", [128, 4, TOK], F32, s4)
            sq = [sb(f"sqf_{i}", [128, 512], BF16, s4) for i in range(2)]
            rstd = sb("rstdf", [128, TOK], F32, s4)
            psX = [ps(f"psX_{i}", [128, 16, 32], F32, s4) for i in range(4)]
            psY = [ps(f"psY_{i}", [128, 32, 16], F32, s4) for i in range(2)]
            psN = [ps(f"psNf_{i}", [128, 512], F32, s4) for i in range(2)]
            A_r = A_d.rearrange("r s k c -> (r s) k c")
            for kc in range(8):
                a2 = kc % 2
                dma("sp", f"Ach{a2}", lambda e, kc=kc: e.dma_start(out=Ach[a2][:], in_=A_r[:, kc * 16:(kc + 1) * 16, :]), reads=[f"A_d{sb_}_{ri_}" for sb_ in range(16) for ri_ in range(2)], writes=[f"Ach{a2}"])
                for cc in range(4):
                    for kl in range(16):
                        k1 = kc * 16 + kl
                        op("pe", lambda e, cc=cc, kl=kl, k1=k1: e.matmul(out=psX[cc][:, kl, :], lhsT=Ach[a2][:, kl, cc * 128:(cc + 1) * 128], rhs=f3_sb[:, k1, :],
                                                                       start=True, stop=True),
                           reads=[f"Ach{a2}", "f3"], writes=[f"psX{cc}"], inc=(kl == 15))
                    if cc % 2 == 0:
                        op("act", lambda e, cc=cc, kc=kc: e.copy(out=XT[:, cc, kc * 16:(kc + 1) * 16, :], in_=psX[cc][:]), reads=[f"psX{cc}"], writes=[f"XT{cc}"])
                    else:
                        op("dve", lambda e, cc=cc, kc=kc: e.tensor_copy(out=XT[:, cc, kc * 16:(kc + 1) * 16, :], in_=psX[cc][:]), reads=[f"psX{cc}"], writes=[f"XT{cc}"])
            scale = 1.0 / float(np.sqrt(8192.0 * 64.0))
            for cc in range(4):
                yv = yf[:, cc, :].rearrange("p (k2 k1) -> p k1 k2", k1=128)
                for kq in range(4):
                    y2 = (cc * 4 + kq) % 2
                    op("pe", lambda e, cc=cc, kq=kq: e.matmul(out=psY[y2][:], lhsT=bd_sb[:, 0, :], rhs=XT[:, cc, kq * 32:(kq + 1) * 32, 0:16], start=True, stop=False),
                       reads=["bd", f"XT{cc}"], writes=[f"psY{y2}"], inc=False)
                    op("pe", lambda e, cc=cc, kq=kq: e.matmul(out=psY[y2][:], lhsT=bd_sb[:, 1, :], rhs=XT[:, cc, kq * 32:(kq + 1) * 32, 16:32], start=False, stop=True),
                       reads=["bd", f"XT{cc}"], writes=[f"psY{y2}"])
                    op("act", lambda e, kq=kq, yv=yv: e.activation(out=yv[:, kq * 32:(kq + 1) * 32, :], in_=psY[y2][:], func=AF.Copy, scale=scale),
                       reads=[f"psY{y2}"], writes=[f"yf{cc}"])
            branch_norm_names = [f"yf{c}" for c in range(4)]
            for tb in range(4):
                cols = slice(tb * 512, (tb + 1) * 512)
                n2 = tb % 2
                for c in range(4):
                    s2i = (tb * 4 + c) % 2
                    op("act", lambda e, c=c: e.activation(out=sq[s2i][:], in_=yf[:, c, cols], func=AF.Square), reads=[f"yf{c}"], writes=[f"sqf{s2i}"])
                    op("pe", lambda e, c=c: e.matmul(out=psN[n2][:], lhsT=onesb[:], rhs=sq[s2i][:], start=(c == 0), stop=(c == 3)),
                       reads=["onesb", f"sqf{s2i}"], writes=[f"psNf{n2}"])
                op("act", lambda e: e.activation(out=rstd[:, cols], in_=psN[n2][:], func=AF.Sqrt, bias=epsb[:, 0:1], scale=1.0 / 512),
                   reads=[f"psNf{n2}", "epsb"], writes=[f"rstdf{tb}"])
                op("dve", lambda e: e.reciprocal(out=rstd[:, cols], in_=rstd[:, cols]), reads=[f"rstdf{tb}"], writes=[f"rstdf{tb}"])
                for c in range(4):
                    op("dve", lambda e, c=c: e.scalar_tensor_tensor(out=ynT[:, 4 + c, cols], in0=yf[:, c, cols], scalar=gcf_sb[:, 4 + c:5 + c],
                                                                  in1=rstd[:, cols], op0=ALU.mult, op1=ALU.mult),
                       reads=[f"yf{c}", "gcf", f"rstdf{tb}"], writes=[f"ynT{4 + c}"])
            if dbg:
                dump("yf", [128, 4, TOK], F32, yf[:], branch_norm_names)
        S.barrier()
        with ExitStack() as s5:
            Wo_ = sb("Wout", [128, 8, D], BF16, s5)
            load_w("Wout", w_out, Wo_[:], D)
            xb = [sb(f"xr_{i}", [128, D], F32, s5) for i in range(2)]
            psW = [ps(f"psW_{i}", [128, 512], F32, s5) for i in range(4)]
            yn_names = [f"ynT{c}" for c in range(8)]
            for i in range(NT):
                k2 = i % 2
                dma("sp", f"xr{k2}", lambda e, i=i: e.dma_start(out=xb[k2][:], in_=xrot[i * 128:(i + 1) * 128, :]), writes=[f"xr{k2}"])
                for dh in range(2):
                    p4 = (i * 2 + dh) % 4
                    for c in range(8):
                        op("pe", lambda e, c=c, dh=dh, i=i: e.matmul(out=psW[p4][:], lhsT=ynT[:, c, i * 128:(i + 1) * 128], rhs=Wo_[:, c, dh * 512:(dh + 1) * 512],
                                                                   start=(c == 0), stop=(c == 7)),
                           reads=yn_names + ["Wout"], writes=[f"psW{p4}"], inc=(c == 7))
                    op("dve", lambda e, dh=dh: e.tensor_tensor(out=xb[k2][:, dh * 512:(dh + 1) * 512], in0=psW[p4][:], in1=xb[k2][:, dh * 512:(dh + 1) * 512], op=ALU.add),
                       reads=[f"psW{p4}", f"xr{k2}"], writes=[f"xr{k2}"])
                dma("pool", f"x1st{k2}", lambda e, i=i: e.dma_start(out=X1_d[i * 128:(i + 1) * 128, :], in_=xb[k2][:]), reads=[f"xr{k2}"], writes=[f"X1_d{i}"])

    if stage == "A":
        dma("sp", "fin", lambda e: e.dma_start(out=out, in_=X1_d), reads=[f"X1_d{i}" for i in range(NT)], writes=["out"])
        S.finish("sp", ["out"] + ["dbg_" + n for n in dbg_outs])
        return nc, es, dbg_outs

    S.barrier()
    with ExitStack() as sM:
        bc_reg = nc.gpsimd.to_reg(NSLOT - 1)
        IDX = sb("IDX", [128, NT, 4], I32, sM)
        WK = sb("WK", [128, NT, 4], F32, sM)
        S.nobar.update(["Wgu0", "Wgu1", "Wdn0", "Wdn1", "zfill", "bgu"])
        if full:
            Wgu = [sb(f"Wgu_{i}", [128, 8, 2048], BF16, sM) for i in range(2)]
            Wdn = [sb(f"Wdn_{i}", [128, 8, D], BF16, sM) for i in range(2)]
            bgu = sb("bgu", [128, NE, 16], F32, sM)
            dma("sp", "bgu", lambda e: e.dma_start(out=bgu[:], in_=b_gu), writes=["bgu"])

        def load_expert(e_):
            k = e_ % 2
            load_w(f"Wgu{k}", w_gu[e_], Wgu[k][:], 2048)
            load_w(f"Wdn{k}", w_dn[e_], Wdn[k][:], D)

        if full:
            load_expert(0)
            load_expert(1)
        S.barrier()
        with ExitStack() as sB:
            KT = sb("KT", [128, 8, 256], BF16, sB)
            Vb = sb("Vb", [128, 2, D], BF16, sB)
            junk = sb("junkB", [128, D], BF16, sB)
            ss = sb("ssB", [128, 1], F32, sB)
            load_gain(2)
            load_gain(1)
            rs = sb("rsB", [128, 1], F32, sB)
            S.barrier()
            with ExitStack() as sK:
                Wk = sb("Wk", [128, 8, D], BF16, sK)
                Wv = sb("Wv", [128, 8, D], BF16, sK)
                load_w("Wk", w_k, Wk[:], D)
                load_w("Wv", w_v, Wv[:], D)
                mt_ = [sb(f"mt_{i}", [128, D], F32, sK) for i in range(2)]
                mb_ = [sb(f"mb_{i}", [128, D], BF16, sK) for i in range(2)]
                memT = sb("memT", [128, 8, 256], BF16, sK)
                psT = [ps(f"psTk_{i}", [128, 8, 128], BF16, sK) for i in range(2)]
                psK = [ps(f"psK_{i}", [128, 512], F32, sK) for i in range(2)]
                for m in range(2):
                    dma("sp", f"mt{m}", lambda e, m=m: e.dma_start(out=mt_[m][:], in_=memb[m * 128:(m + 1) * 128, :]), writes=[f"mt{m}"])
                    rmsnorm_tile(mt_[m][:], f"mt{m}", 2, mb_[m][:], f"mb{m}", ss, rs, junk)
                    for dc in range(8):
                        op("pe", lambda e, dc=dc, m=m: e.transpose(out=psT[m][:, dc, :], in_=mb_[m][:, dc * 128:(dc + 1) * 128], identity=identb[:]),
                           reads=[f"mb{m}", "identb"], writes=[f"psTk{m}"], inc=(dc == 7))
                    op("act", lambda e, m=m: e.copy(out=memT[:, :, m * 128:(m + 1) * 128], in_=psT[m][:]), reads=[f"psTk{m}"], writes=["memT"])
                for j in range(8):
                    k2 = j % 2
                    for dc in range(8):
                        op("pe", lambda e, j=j, dc=dc: e.matmul(out=psK[k2][:, 0:256], lhsT=Wk[:, dc, j * 128:(j + 1) * 128], rhs=memT[:, dc, :], start=(dc == 0), stop=(dc == 7)),
                           reads=["Wk", "memT"], writes=[f"psK{k2}"], inc=(dc == 7))
                    op("act", lambda e, j=j: e.copy(out=KT[:, j, :], in_=psK[k2][:, 0:256]), reads=[f"psK{k2}"], writes=["KT"])
                for m in range(2):
                    for dh in range(2):
                        k2 = (m * 2 + dh) % 2
                        for dc in range(8):
                            op("pe", lambda e, m=m, dh=dh, dc=dc: e.matmul(out=psK[k2][:], lhsT=memT[:, dc, m * 128:(m + 1) * 128], rhs=Wv[:, dc, dh * 512:(dh + 1) * 512],
                                                                         start=(dc == 0), stop=(dc == 7)),
                               reads=["Wv", "memT"], writes=[f"psK{k2}"], inc=(dc == 7))
                        op("dve", lambda e, m=m, dh=dh: e.tensor_copy(out=Vb[:, m, dh * 512:(dh + 1) * 512], in_=psK[k2][:]), reads=[f"psK{k2}"], writes=["Vb"])
            S.barrier()
            with ExitStack() as sQ:
                Wq = sb("Wq", [128, 8, D], BF16, sQ)
                Wo = sb("Wo", [128, 8, D], BF16, sQ)
                load_w("Wq", w_q, Wq[:], D)
                load_w("Wo", w_o, Wo[:], D)
                xt = [sb(f"x1_{i}", [128, D], F32, sQ) for i in range(4)]
                hb = [sb(f"h2b_{i}", [128, D], BF16, sQ) for i in range(2)]
                hT4 = sb("h2T4", [128, 8, 512], BF16, sQ)
                QT4 = sb("QT4", [128, 8, 512], BF16, sQ)
                E = sb("E", [128, 4, 256], F32, sQ)
                Pb = sb("Pb", [128, 4, 256], BF16, sQ)
                PT = sb("PT", [128, 8, 128], BF16, sQ)
                OT = sb("OT", [128, 8, 128], BF16, sQ)
                mx = sb("mx", [128, 4], F32, sQ)
                nmx = sb("nmx", [128, 4], F32, sQ)
                sm = sb("sm", [128, 4], F32, sQ)
                rsm = sb("rsm", [128, 4], F32, sQ)
                psT = ps("psTq", [128, 8, 128], BF16, sQ)
                psQ = ps("psQ", [128, 512], F32, sQ)
                psS = ps("psS", [128, 4, 256], F32, sQ)
                psPT = ps("psPT", [128, 8, 128], BF16, sQ)
                psO = ps("psO", [128, 8, 128], F32, sQ)
                psW = ps("psWo", [128, 512], F32, sQ)
                Pb2 = [Pb, sb("Pb_1", [128, 4, 256], BF16, sQ)]

                def b_pro(grp):
                    for t in range(4):
                        i = grp * 4 + t
                        k2 = i % 2
                        dma("sp", f"x1l{t}", lambda e, i=i, t=t: e.dma_start(out=xt[t][:], in_=X1_d[i * 128:(i + 1) * 128, :]), reads=[f"X1_d{i}"], writes=[f"x1_{t}"])
                        rmsnorm_tile(xt[t][:], f"x1_{t}", 1, hb[k2][:], f"h2b{k2}", ss, rs, junk)
                        for dc in range(8):
                            op("pe", lambda e, dc=dc, k2=k2: e.transpose(out=psT[:, dc, :], in_=hb[k2][:, dc * 128:(dc + 1) * 128], identity=identb[:]),
                               reads=[f"h2b{k2}", "identb"], writes=["psTq"], inc=(dc == 7))
                        op("act", lambda e, t=t: e.copy(out=hT4[:, :, t * 128:(t + 1) * 128], in_=psT[:]), reads=["psTq"], writes=["h2T4"])
                    for j in range(8):
                        for dc in range(8):
                            op("pe", lambda e, j=j, dc=dc: e.matmul(out=psQ[:], lhsT=Wq[:, dc, j * 128:(j + 1) * 128], rhs=hT4[:, dc, :], start=(dc == 0), stop=(dc == 7)),
                               reads=["Wq", "h2T4"], writes=["psQ"], inc=(dc == 7))
                        op("act", lambda e, j=j: e.copy(out=QT4[:, j, :], in_=psQ[:]), reads=["psQ"], writes=["QT4"])

                def b_s1(i):
                    t = i % 4
                    p2 = i % 2
                    tc_ = slice(t * 128, (t + 1) * 128)
                    for hh in range(4):
                        for hf in range(2):
                            op("pe", lambda e, hh=hh, hf=hf: e.matmul(out=psS[:, hh, :], lhsT=QT4[:, hh * 2 + hf, tc_], rhs=KT[:, hh * 2 + hf, :], start=(hf == 0), stop=(hf == 1)),
                               reads=["QT4", "KT"], writes=["psS"], inc=(hf == 1))
                    op("dve", lambda e: e.tensor_reduce(out=mx[:], in_=psS[:], axis=AX.X, op=ALU.max), reads=["psS"], writes=["mx"])
                    op("dve", lambda e: e.tensor_scalar(out=nmx[:], in0=mx[:], scalar1=-1.0 / 16.0, scalar2=None, op0=ALU.mult), reads=["mx"], writes=["nmx"])
                    for hh in range(4):
                        op("act", lambda e, hh=hh: e.activation(out=E[:, hh, :], in_=psS[:, hh, :], func=AF.Exp, bias=nmx[:, hh:hh + 1], scale=1.0 / 16.0,
                                                               accum_out=sm[:, hh:hh + 1]),
                           reads=["psS", "nmx"], writes=["E", "sm"])
                    op("dve", lambda e: e.reciprocal(out=rsm[:], in_=sm[:]), reads=["sm"], writes=["rsm"])
                    for hh in range(4):
                        op("dve", lambda e, hh=hh: e.tensor_scalar(out=Pb2[p2][:, hh, :], in0=E[:, hh, :], scalar1=rsm[:, hh:hh + 1], scalar2=None, op0=ALU.mult),
                           reads=["E", "rsm"], writes=[f"Pb{p2}"])

                def b_s2(i):
                    t = i % 4
                    p2 = i % 2
                    for hh in range(4):
                        for m in range(2):
                            op("pe", lambda e, hh=hh, m=m: e.transpose(out=psPT[:, hh * 2 + m, :], in_=Pb2[p2][:, hh, m * 128:(m + 1) * 128], identity=identb[:]),
                               reads=[f"Pb{p2}", "identb"], writes=["psPT"], inc=(hh == 3 and m == 1))
                    op("act", lambda e: e.copy(out=PT[:], in_=psPT[:]), reads=["psPT"], writes=["PT"])
                    for hh in range(4):
                        for hf in range(2):
                            c = hh * 2 + hf
                            for m in range(2):
                                op("pe", lambda e, hh=hh, m=m, c=c: e.matmul(out=psO[:, c, :], lhsT=Vb[:, m, c * 128:(c + 1) * 128], rhs=PT[:, hh * 2 + m, :],
                                                                           start=(m == 0), stop=(m == 1)),
                                   reads=["Vb", "PT"], writes=["psO"], inc=(c == 7 and m == 1))
                    op("act", lambda e: e.copy(out=OT[:], in_=psO[:]), reads=["psO"], writes=["OT"])
                    for dh in range(2):
                        for c in range(8):
                            op("pe", lambda e, c=c, dh=dh: e.matmul(out=psW[:], lhsT=OT[:, c, :], rhs=Wo[:, c, dh * 512:(dh + 1) * 512], start=(c == 0), stop=(c == 7)),
                               reads=["OT", "Wo"], writes=["psWo"], inc=(c == 7))
                        op("dve", lambda e, dh=dh: e.tensor_tensor(out=xt[t][:, dh * 512:(dh + 1) * 512], in0=psW[:], in1=xt[t][:, dh * 512:(dh + 1) * 512], op=ALU.add),
                           reads=["psWo", f"x1_{t}"], writes=[f"x1_{t}"])
                    dma("pool", f"x2st{t}", lambda e: e.dma_start(out=X2_d[i * 128:(i + 1) * 128, :], in_=xt[t][:]), reads=[f"x1_{t}"], writes=[f"X2_d{i}"])

                for grp in range(4):
                    b_pro(grp)
                    for t in range(4):
                        b_s1(grp * 4 + t)
                        if t > 0:
                            b_s2(grp * 4 + t - 1)
                    b_s2(grp * 4 + 3)

        if stage == "B":
            dma("sp", "fin", lambda e: e.dma_start(out=out, in_=X2_d), reads=[f"X2_d{i}" for i in range(NT)], writes=["out"])
            S.finish("sp", ["out"] + ["dbg_" + n for n in dbg_outs])
            return nc, es, dbg_outs


        S.barrier()
        with ExitStack() as sC:
            Wr = sb("Wr", [128, 8, NE], F32, sC)
            dma("sp", "Wr", lambda e: e.dma_start(out=Wr[:], in_=w_r.rearrange("(dc p) f -> p dc f", p=128)), writes=["Wr"])
            brb = sb("brb", [128, NE], F32, sC)
            dma("sp", "brb", lambda e: e.dma_start(out=brb[:], in_=b_r.partition_broadcast(128)), writes=["brb"])
            eb1 = sb("eb1", [128, NE], F32, sC)
            dma("sp", "eb1", lambda e: e.dma_start(out=eb1[:], in_=ebase), writes=["eb1"])
            load_gain(3)
            masks = sb("masks", [128, NT, NE], BF16, sC)
            xt = [sb(f"x2_{i}", [128, D], F32, sC) for i in range(2)]
            hf = [sb(f"h3f_{i}", [128, D], F32, sC) for i in range(2)]
            hb = [sb(f"h3b_{i}", [128, D], BF16, sC) for i in range(2)]
            hT = sb("h3T", [128, 8, 128], F32, sC)
            junk = sb("junkC", [128, D], BF16, sC)
            ss = sb("ssC", [128, 1], F32, sC)
            rs = sb("rsC", [128, 1], F32, sC)
            lg = sb("lg", [128, NE], F32, sC)
            m8 = sb("m8", [128, 8], F32, sC)
            nm = sb("nm", [128, 1], F32, sC)
            mk = sb("mk", [128, NE], F32, sC)
            ex = sb("ex", [128, NE], F32, sC)
            em = sb("em", [128, NE], F32, sC)
            sme = sb("sme", [128, 1], F32, sC)
            wt = sb("wt", [128, NE], F32, sC)
            key = sb("key", [128, NE], F32, sC)
            k8 = sb("k8", [128, 8], F32, sC)
            eq = sb("eq", [128, NE], F32, sC)
            psT = [ps(f"psTr_{i}", [128, 4, 128], F32, sC) for i in range(2)]
            psL = ps("psL", [128, NE], F32, sC)
            psP = ps("psP", [128, NE], F32, sC)
            def c_s0(i):
                k2 = i % 2
                dma("sp", f"x2l{k2}", lambda e: e.dma_start(out=xt[k2][:], in_=X2_d[i * 128:(i + 1) * 128, :]), reads=[f"X2_d{i}"], writes=[f"x2_{k2}"])
                rmsnorm_tile(xt[k2][:], f"x2_{k2}", 3, hb[k2][:], f"h3b{k2}", ss, rs, junk, hf=hf[k2][:], hfname=f"h3f{k2}")

            def c_s1(i):
                k2 = i % 2
                for dc in range(8):
                    op("pe", lambda e, dc=dc: e.transpose(out=psT[dc // 4][:, dc % 4, :], in_=hf[k2][:, dc * 128:(dc + 1) * 128], identity=identf[:]),
                       reads=[f"h3f{k2}", "identf"], writes=[f"psTr{dc // 4}"], inc=(dc % 4 == 3))
                op("act", lambda e: e.copy(out=hT[:, 0:4, :], in_=psT[0][:]), reads=["psTr0"], writes=["h3Ta"])
                op("dve", lambda e: e.tensor_copy(out=hT[:, 4:8, :], in_=psT[1][:]), reads=["psTr1"], writes=["h3Tb"])
                for dc in range(8):
                    op("pe", lambda e, dc=dc: e.matmul(out=psL[:], lhsT=hT[:, dc, :], rhs=Wr[:, dc, :], start=(dc == 0), stop=(dc == 7)),
                       reads=["h3Ta", "h3Tb", "Wr"], writes=["psL"], inc=(dc == 7))
                op("dve", lambda e: e.tensor_tensor(out=lg[:], in0=psL[:], in1=brb[:], op=ALU.add), reads=["psL", "brb"], writes=["lg"])
                op("dve", lambda e: e.max(out=m8[:], in_=lg[:]), reads=["lg"], writes=["m8"])
                op("dve", lambda e: e.tensor_scalar(out=mk[:], in0=lg[:], scalar1=m8[:, 3:4], scalar2=None, op0=ALU.is_ge), reads=["lg", "m8"], writes=["mk"])
                op("dve", lambda e, i=i: e.tensor_copy(out=masks[:, i, :], in_=mk[:]), reads=["mk"], writes=[f"masks{i}"])
                op("dve", lambda e: e.tensor_scalar(out=nm[:], in0=m8[:, 0:1], scalar1=-1.0, scalar2=None, op0=ALU.mult), reads=["m8"], writes=["nm"])
                op("act", lambda e: e.activation(out=ex[:], in_=lg[:], func=AF.Exp, bias=nm[:, 0:1], scale=1.0), reads=["lg", "nm"], writes=["ex"])
                op("dve", lambda e: e.tensor_tensor(out=em[:], in0=ex[:], in1=mk[:], op=ALU.mult), reads=["ex", "mk"], writes=["em"])
                op("dve", lambda e: e.reduce_sum(out=sme[:], in_=em[:], axis=AX.X), reads=["em"], writes=["sme"])
                op("dve", lambda e: e.reciprocal(out=sme[:], in_=sme[:]), reads=["sme"], writes=["sme"])
                op("dve", lambda e: e.tensor_scalar(out=wt[:], in0=em[:], scalar1=sme[:, 0:1], scalar2=None, op0=ALU.mult), reads=["em", "sme"], writes=["wt"])
                op("pe", lambda e, i=i: e.matmul(out=psP[:], lhsT=ltri[:], rhs=masks[:, i, :], start=True, stop=(i == 0)),
                   reads=["ltri", f"masks{i}"], writes=["psP"], inc=(i == 0))
                for j in range(i):
                    op("pe", lambda e, j=j, i=i: e.matmul(out=psP[:], lhsT=onesb[:], rhs=masks[:, j, :], start=False, stop=(j == i - 1)),
                       reads=["onesb", f"masks{j}"], writes=["psP"], inc=(j == i - 1))
                op("dve", lambda e: e.tensor_tensor(out=key[:], in0=psP[:], in1=eb1[:], op=ALU.add), reads=["psP", "eb1"], writes=["key"])
                op("dve", lambda e: e.tensor_tensor(out=key[:], in0=key[:], in1=mk[:], op=ALU.mult), reads=["key", "mk"], writes=["key"])
                op("dve", lambda e: e.max(out=k8[:], in_=key[:]), reads=["key"], writes=["k8"])
                op("dve", lambda e, i=i: e.tensor_scalar(out=IDX[:, i, :], in0=k8[:, 0:4], scalar1=-1.0, scalar2=None, op0=ALU.add), reads=["k8"], writes=[f"IDX{i}"])
                for k in range(4):
                    op("dve", lambda e, k=k: e.tensor_scalar(out=eq[:], in0=key[:], scalar1=k8[:, k:k + 1], scalar2=None, op0=ALU.is_equal), reads=["key", "k8"], writes=["eq"])
                    op("dve", lambda e: e.tensor_tensor(out=eq[:], in0=eq[:], in1=wt[:], op=ALU.mult), reads=["eq", "wt"], writes=["eq"])
                    op("dve", lambda e, k=k, i=i: e.reduce_sum(out=WK[:, i, k:k + 1], in_=eq[:], axis=AX.X), reads=["eq"], writes=[f"WK{i}"])
                for k in range(4):
                    dma("pool", f"disp{k2}", lambda e, k=k, i=i: e.indirect_dma_start(out=Xd, out_offset=bass.IndirectOffsetOnAxis(ap=IDX[:, i, k:k + 1], axis=0),
                                                                                  in_=hb[k2][:, :], in_offset=None, bounds_check=bc_reg, oob_is_err=False),
                        reads=[f"h3b{k2}", f"IDX{i}"], writes=["Xd"])

            c_s0(0)
            for i in range(NT):
                if i + 1 < NT:
                    c_s0(i + 1)
                c_s1(i)
        S.barrier()
        with ExitStack() as sD:
            Xe = [sb(f"Xe_{i}", [128, 3, D], BF16, sD) for i in range(2)]
            XTe = [sb(f"XTe_{i}", [128, 8, CAP], BF16, sD) for i in range(2)]
            actT = [sb(f"actT_{i}", [128, 8, CAP], BF16, sD) for i in range(2)]
            bdb = [sb(f"bdb_{i}", [128, D], F32, sD) for i in range(2)]
            Oe = [sb(f"Oe_{i}", [128, 3, D], F32, sD) for i in range(2)]
            g_ = [sb(f"g_{i}", [128, CAP], F32, sD) for i in range(2)]
            sg_ = [sb(f"sg_{i}", [128, CAP], F32, sD) for i in range(2)]
            u_ = [sb(f"u_{i}", [128, CAP], F32, sD) for i in range(2)]
            psXT = [ps(f"psXT_{i}", [128, 3, 128], BF16, sD) for i in range(2)]
            psG = [ps(f"psG_{i}", [128, 512], F32, sD) for i in range(2)]
            psUp = [ps(f"psUp_{i}", [128, 512], F32, sD) for i in range(2)]
            psD = [ps(f"psD_{i}", [128, 512], F32, sD) for i in range(2)]
            def ex_load(e_):
                k = e_ % 2
                dma("sp", f"Xe{k}", lambda e: e.dma_start(out=Xe[k][:], in_=Xd[e_ * CAP:(e_ + 1) * CAP, :].rearrange("(b p) d -> p b d", p=128)),
                    reads=["Xd"], writes=[f"Xe{k}"])
                dma("sp", f"bdb{k}", lambda e: e.dma_start(out=bdb[k][:], in_=b_dn[e_:e_ + 1, :].partition_broadcast(128)), writes=[f"bdb{k}"])

            def ex_tr(e_):
                k = e_ % 2
                for dc in range(8):
                    x2 = dc % 2
                    for b in range(3):
                        op("pe", lambda e, dc=dc, b=b: e.transpose(out=psXT[x2][:, b, :], in_=Xe[k][:, b, dc * 128:(dc + 1) * 128], identity=identb[:]),
                           reads=[f"Xe{k}", "identb"], writes=[f"psXT{x2}"], inc=(b == 2))
                    if dc % 2 == 0:
                        op("act", lambda e, dc=dc: e.copy(out=XTe[k][:, dc, :], in_=psXT[x2][:]), reads=[f"psXT{x2}"], writes=[f"XTe{k}"])
                    else:
                        op("dve", lambda e, dc=dc: e.tensor_copy(out=XTe[k][:, dc, :], in_=psXT[x2][:]), reads=[f"psXT{x2}"], writes=[f"XTe{k}"])

            def ex_gu(e_):
                k = e_ % 2
                for j in range(8):
                    j2 = j % 2
                    for dc in range(8):
                        op("pe", lambda e, j=j, dc=dc: e.matmul(out=psG[j2][:, 0:CAP], lhsT=Wgu[k][:, dc, j * 128:(j + 1) * 128], rhs=XTe[k][:, dc, :], start=(dc == 0), stop=(dc == 7)),
                           reads=[f"Wgu{k}", f"XTe{k}"], writes=[f"psG{j2}"], inc=(dc == 7))
                    for dc in range(8):
                        op("pe", lambda e, j=j, dc=dc: e.matmul(out=psUp[j2][:, 0:CAP], lhsT=Wgu[k][:, dc, 1024 + j * 128:1024 + (j + 1) * 128], rhs=XTe[k][:, dc, :],
                                                               start=(dc == 0), stop=(dc == 7)),
                           reads=[f"Wgu{k}", f"XTe{k}"], writes=[f"psUp{j2}"], inc=(dc == 7))
                    op("dve", lambda e, j=j: e.tensor_scalar(out=g_[j2][:], in0=psG[j2][:, 0:CAP], scalar1=bgu[:, e_, j:j + 1], scalar2=7.0, op0=ALU.add, op1=ALU.min),
                       reads=[f"psG{j2}", "bgu"], writes=[f"g{j2}"])
                    op("act", lambda e: e.activation(out=sg_[j2][:], in_=g_[j2][:], func=AF.Silu, scale=1.702), reads=[f"g{j2}"], writes=[f"sg{j2}"])
                    op("act", lambda e, j=j: e.activation(out=u_[j2][:], in_=psUp[j2][:, 0:CAP], func=AF.Identity, bias=bgu[:, e_, 8 + j:9 + j], scale=1.0),
                       reads=[f"psUp{j2}", "bgu"], writes=[f"u{j2}"])
                    op("dve", lambda e: e.tensor_scalar(out=u_[j2][:], in0=u_[j2][:], scalar1=7.0, scalar2=-7.0, op0=ALU.min, op1=ALU.max), reads=[f"u{j2}"], writes=[f"u{j2}"])
                    op("dve", lambda e, j=j: e.scalar_tensor_tensor(out=actT[k][:, j, :], in0=u_[j2][:], scalar=1.0, in1=sg_[j2][:], op0=ALU.add, op1=ALU.mult),
                       reads=[f"sg{j2}", f"u{j2}"], writes=[f"actT{k}"])

            def ex_dn(e_):
                k = e_ % 2
                for b in range(3):
                    for dh in range(2):
                        d2 = (b * 2 + dh) % 2
                        for j in range(8):
                            op("pe", lambda e, b=b, dh=dh, j=j: e.matmul(out=psD[d2][:], lhsT=actT[k][:, j, b * 128:(b + 1) * 128], rhs=Wdn[k][:, j, dh * 512:(dh + 1) * 512],
                                                                       start=(j == 0), stop=(j == 7)),
                               reads=[f"actT{k}", f"Wdn{k}"], writes=[f"psD{d2}"], inc=(j == 7))
                        op("dve", lambda e, b=b, dh=dh: e.scalar_tensor_tensor(out=Oe[k][:, b, dh * 512:(dh + 1) * 512], in0=psD[d2][:], scalar=1.0 / 1.702,
                                                                                in1=bdb[k][:, dh * 512:(dh + 1) * 512], op0=ALU.mult, op1=ALU.add),
                           reads=[f"psD{d2}", f"bdb{k}"], writes=[f"Oe{k}"])
                dma("sp", f"Oest{k}", lambda e: e.dma_start(out=O_d[e_ * CAP:(e_ + 1) * CAP, :].rearrange("(b p) d -> p b d", p=128), in_=Oe[k][:]),
                    reads=[f"Oe{k}"], writes=["O_d"])

            ex_load(0)
            ex_tr(0)
            for e_ in range(NE):
                if e_ + 1 < NE:
                    ex_load(e_ + 1)
                ex_gu(e_)
                if e_ + 1 < NE:
                    ex_tr(e_ + 1)
                ex_dn(e_)
                if e_ + 2 < NE:
                    load_expert(e_ + 2)
        S.barrier()
        with ExitStack() as sE:
            xt = [sb(f"x2c_{i}", [128, D], F32, sE) for i in range(2)]
            G = [sb(f"G_{i}", [128, D], F32, sE) for i in range(4)]
            ob = [sb(f"ob_{i}", [128, D], F32, sE) for i in range(2)]
            load_gain(4)
            junk = sb("junkE", [128, D], BF16, sE)
            ss = sb("ssE", [128, 1], F32, sE)
            rs = sb("rsE", [128, 1], F32, sE)
            G8 = G + [sb(f"G_{i}", [128, D], F32, sE) for i in range(4, 8)]

            def e_s0(i):
                k2 = i % 2
                dma("sp", f"x2c{k2}", lambda e: e.dma_start(out=xt[k2][:], in_=X2_d[i * 128:(i + 1) * 128, :]), reads=[f"X2_d{i}"], writes=[f"x2c{k2}"])
                for k in range(4):
                    gi = k2 * 4 + k
                    dma("pool", f"G{gi}", lambda e, k=k, gi=gi: e.indirect_dma_start(out=G8[gi][:, :], out_offset=None, in_=O_d,
                                                                                  in_offset=bass.IndirectOffsetOnAxis(ap=IDX[:, i, k:k + 1], axis=0),
                                                                                  bounds_check=bc_reg, oob_is_err=False),
                        reads=["O_d", f"IDX{i}"], writes=[f"G{gi}"])

            def e_s1(i):
                k2 = i % 2
                for k in range(4):
                    gi = k2 * 4 + k
                    op("dve", lambda e, k=k, gi=gi: e.scalar_tensor_tensor(out=xt[k2][:], in0=G8[gi][:], scalar=WK[:, i, k:k + 1], in1=xt[k2][:], op0=ALU.mult, op1=ALU.add),
                       reads=[f"G{gi}", f"WK{i}", f"x2c{k2}"], writes=[f"x2c{k2}"])
                op("act", lambda e: e.activation(out=junk[:], in_=xt[k2][:], func=AF.Square, accum_out=ss[:, 0:1]), reads=[f"x2c{k2}"], writes=["junkE", "ssE"])
                op("act", lambda e: e.activation(out=rs[:, 0:1], in_=ss[:, 0:1], func=AF.Sqrt, bias=epsb[:, 0:1], scale=1.0 / D), reads=["ssE", "epsb"], writes=["rsE"])
                op("dve", lambda e: e.reciprocal(out=rs[:, 0:1], in_=rs[:, 0:1]), reads=["rsE"], writes=["rsE"])
                op("dve", lambda e: e.scalar_tensor_tensor(out=ob[k2][:], in0=xt[k2][:], scalar=rs[:, 0:1], in1=gv[:, GSLOT[4], :], op0=ALU.mult, op1=ALU.mult),
                   reads=[f"x2c{k2}", "rsE", f"gv{GSLOT[4]}"], writes=[f"ob{k2}"])
                dma("sp", f"ost{k2}", lambda e: e.dma_start(out=out[i * 128:(i + 1) * 128, :], in_=ob[k2][:]), reads=[f"ob{k2}"], writes=[f"out{i}"])

            e_s0(0)
            for i in range(NT):
                if i + 1 < NT:
                    e_s0(i + 1)
                e_s1(i)
    S.finish("sp", [f"out{i}" for i in range(NT)] + ["dbg_" + n for n in dbg_outs])
    return nc, es, dbg_outs


def host_inputs(inputs, stage="full", cores=range(8)):
    f = np.float32
    x = np.asarray(inputs["x"], f)
    mem = np.asarray(inputs["mem"], f)
    gvec = np.stack([inputs["norm_mix"][0], inputs["norm_xattn"][0], inputs["norm_mem"][0], inputs["norm_ffn"][0], inputs["norm_final"]]).astype(f)
    cwv = np.asarray(inputs["conv_w"][0], f)
    cw = np.ascontiguousarray(cwv.reshape(3, 4, 128).transpose(2, 1, 0))
    gc = np.asarray(inputs["g_conv_out"][0], f).reshape(4, 128).T
    gf = np.asarray(inputs["g_fft_out"][0], f).reshape(4, 128).T
    gcf = np.ascontiguousarray(np.concatenate([gc, gf], axis=1))
    a = np.arange(128)
    f1c = np.cos(2 * np.pi * np.outer(a, a) / 128).astype(f)
    f1s = (-np.sin(2 * np.pi * np.outer(a, a) / 128)).astype(f)
    c64 = np.arange(64)
    C64 = np.cos(2 * np.pi * np.outer(c64, c64) / 64)
    S64 = np.sin(2 * np.pi * np.outer(c64, c64) / 64)
    bdc = np.zeros((128, 128)); bds = np.zeros((128, 128))
    for g in range(2):
        bdc[g * 64:(g + 1) * 64, g * 64:(g + 1) * 64] = C64
        bds[g * 64:(g + 1) * 64, g * 64:(g + 1) * 64] = S64
    ebase = np.broadcast_to((np.arange(NE) * CAP + 1).astype(f), (128, NE)).copy()
    common = {
        "gvec": gvec, "w_in": np.asarray(inputs["w_in"][0], f), "cw": cw, "gcf": gcf,
        "w_out": np.asarray(inputs["w_out"][0], f), "w_q": np.asarray(inputs["w_q"][0], f), "w_k": np.asarray(inputs["w_k"][0], f),
        "w_v": np.asarray(inputs["w_v"][0], f), "w_o": np.asarray(inputs["w_o"][0], f), "w_r": np.asarray(inputs["w_router"][0], f),
        "b_r": np.asarray(inputs["b_router"], f).reshape(1, NE), "f1c": f1c, "f1s": f1s, "bdc": bdc.astype(f), "bds": bds.astype(f), "ebase": ebase,
    }
    if stage == "full":
        common["w_gu"] = np.asarray(inputs["w_gate_up"][0], f)
        common["b_gu"] = np.ascontiguousarray(np.asarray(inputs["b_gate_up"][0], f).reshape(NE, 16, 128).transpose(2, 0, 1))
        common["w_dn"] = np.asarray(inputs["w_down"][0], f)
        common["b_dn"] = np.asarray(inputs["b_down"][0], f)
    maps = []
    s2 = np.arange(64)
    k1 = np.arange(128)
    for c in cores:
        b, q = c // 4, c % 4
        xr = np.roll(x[b], -TOK * q, axis=0)
        xh = np.zeros((128, D), f)
        if q > 0:
            xh[0] = x[b, TOK * q - 1]
        if q < 3:
            xh[1] = x[b, TOK * (q + 1)]
        k2 = 16 * q + np.arange(16)
        kk = k1[:, None] + 128 * k2[None, :]
        phi = 2 * np.pi * s2[:, None, None] * kk[None] / 8192.0 + np.pi * kk[None] * q / 2.0
        gcos, gsin = np.cos(phi), np.sin(phi)
        t = np.zeros((2, 64, 128, 32))
        t[0, :, :, 0:16] = gcos; t[1, :, :, 0:16] = gsin
        t[0, :, :, 16:32] = -gsin; t[1, :, :, 16:32] = gcos
        m = dict(common)
        m.update({"xrot": np.ascontiguousarray(xr), "xhalo": xh, "memb": np.ascontiguousarray(mem[b]), "f3": t.reshape(128, 128, 32).astype(f)})
        maps.append(m)
    return maps


def kernel(**inputs):
    nc, es, _ = build("full")
    maps = host_inputs(inputs)
    res = run_bass_kernel_spmd(nc, maps, core_ids=list(range(8)))
    outs = [np.asarray(r["out"], np.float32) for r in res.results]
    y = np.stack(outs).reshape(2, 4 * TOK, D)
    return y
```

```python
import numpy as np
from contextlib import ExitStack
import concourse.bass as bass
import concourse.mybir as mybir
from concourse.bass_utils import run_bass_kernel_spmd

F32 = mybir.dt.float32
BF16 = mybir.dt.bfloat16
I32 = mybir.dt.int32
ALU = mybir.AluOpType
AF = mybir.ActivationFunctionType
AX = mybir.AxisListType

D = 1024
SEQ = 8192
TOK = 2048
NT = TOK // 128
NE = 32
CAP = 384
NSLOT = NE * CAP
EPS = 1e-5


class Sched:
    def __init__(self, nc, es):
        self.nc = nc
        self.es = es
        self.eng = {"pe": nc.tensor, "act": nc.scalar, "dve": nc.vector, "pool": nc.gpsimd, "sp": nc.sync}
        self.sem = {k: es.enter_context(nc.semaphore("c_" + k)) for k in self.eng}
        self.cnt = {k: 0 for k in self.eng}
        self.waited = {k: {} for k in self.eng}
        self.dsem = {}
        self.dcnt = {}
        self.res = {}
        self.nobar = set()
        self.semname = {}

    def _wait(self, e, ev):
        if ev is None:
            return
        s, v, owner = ev
        if owner == "pe" and e == "pe":
            return
        w = self.waited[e]
        if w.get(id(s), 0) >= v:
            return
        self.eng[e].wait_ge(s, v)
        w[id(s)] = v

    def _deps(self, e, reads, writes):
        for r in reads:
            st = self.res.get(r)
            if st:
                self._wait(e, st[0])
        for wname in writes:
            st = self.res.get(wname)
            if st:
                self._wait(e, st[0])
                for ev in list(st[1].values()):
                    self._wait(e, ev)

    def _record(self, ev, reads, writes):
        for r in reads:
            st = self.res.setdefault(r, [None, {}])
            old = st[1].get(id(ev[0]))
            if old is None or old[1] < ev[1]:
                st[1][id(ev[0])] = ev
        for wname in writes:
            self.res[wname] = [ev, {}]

    def op(self, e, fn, reads=(), writes=(), inc=True):
        self._deps(e, reads, writes)
        ins = fn(self.eng[e])
        if inc:
            self.cnt[e] += 1
            ins.then_inc(self.sem[e], 1)
            ev = (self.sem[e], self.cnt[e], e)
        else:
            ev = (self.sem[e], self.cnt[e] + 1, e)
        self._record(ev, reads, writes)
        return ins

    def dma(self, q, key, fn, reads=(), writes=()):
        self._deps(q, reads, writes)
        if key not in self.dsem:
            self.dsem[key] = self.es.enter_context(self.nc.semaphore("d_" + key))
            self.dcnt[key] = 0
        ins = fn(self.eng[q])
        self.dcnt[key] += 16
        ins.then_inc(self.dsem[key], 16)
        ev = (self.dsem[key], self.dcnt[key], "dma")
        self._record(ev, reads, writes)
        return ins

    def barrier(self):
        evs = [(self.sem[o], self.cnt[o], o) for o in self.eng if self.cnt[o] > 0]
        evs += [(self.dsem[k], self.dcnt[k], "dma") for k in self.dsem if k not in self.nobar]
        for e in self.eng:
            for ev in evs:
                if ev[2] == e and e != "pe":
                    continue
                if ev[2] == "pe" and e == "pe":
                    continue
                self._wait(e, ev)
        self.res = {k: v for k, v in self.res.items()}

    def finish(self, q, names):
        for n in names:
            st = self.res.get(n)
            if st:
                self._wait(q, st[0])
                for ev in list(st[1].values()):
                    self._wait(q, ev)


def build(stage="full", dbg=False):
    nc = bass.Bass("TRN2", target_bir_lowering=False)
    es = ExitStack()

    def din(name, shape, dt=F32):
        return nc.dram_tensor(name, list(shape), dt, kind="ExternalInput").ap()

    def dscr(name, shape, dt):
        return nc.dram_tensor(name, list(shape), dt, kind="Internal").ap()

    xrot = din("xrot", [SEQ, D])
    xhalo = din("xhalo", [128, D])
    memb = din("memb", [256, D])
    gvec = din("gvec", [5, D])
    w_in = din("w_in", [D, 2048])
    cw = din("cw", [128, 4, 3])
    gcf = din("gcf", [128, 8])
    w_out = din("w_out", [D, D])
    w_q = din("w_q", [D, D]); w_k = din("w_k", [D, D]); w_v = din("w_v", [D, D]); w_o = din("w_o", [D, D])
    w_r = din("w_r", [D, NE])
    b_r = din("b_r", [1, NE])
    f1c = din("f1c", [128, 128]); f1s = din("f1s", [128, 128])
    f3 = din("f3", [128, 128, 32])
    bdc = din("bdc", [128, 128]); bds = din("bds", [128, 128])
    ebase = din("ebase", [128, NE])
    full = stage == "full"
    if full:
        w_gu = din("w_gu", [NE, D, 2048])
        b_gu = din("b_gu", [128, NE, 16])
        w_dn = din("w_dn", [NE, D, D])
        b_dn = din("b_dn", [NE, D])
    out = nc.dram_tensor("out", [TOK, D], F32, kind="ExternalOutput").ap()

    U_d = dscr("U_d", [SEQ, 512], BF16)
    A_d = dscr("A_d", [2, 64, 128, 512], BF16)
    X1_d = dscr("X1_d", [TOK, D], F32)
    X2_d = dscr("X2_d", [TOK, D], F32)
    Xd = dscr("Xd", [NSLOT, D], BF16)
    O_d = dscr("O_d", [NSLOT, D], F32)

    S = Sched(nc, es)
    op, dma = S.op, S.dma

    def sb(name, shape, dt, stack):
        return stack.enter_context(nc.sbuf_tensor(name, list(shape), dt))

    def ps(name, shape, dt, stack):
        return stack.enter_context(nc.psum_tensor(name, list(shape), dt))

    identf = sb("identf", [128, 128], F32, es)
    identb = sb("identb", [128, 128], BF16, es)
    onesb = sb("onesb", [128, 128], BF16, es)
    ltri = sb("ltri", [128, 128], BF16, es)
    ltrif = sb("ltrif", [128, 128], F32, es)
    epsb = sb("epsb", [128, 1], F32, es)
    gv = sb("gv", [128, 2, D], F32, es)
    GSLOT = {0: 0, 2: 0, 1: 1, 3: 0, 4: 1}

    def load_gain(g):
        sl = GSLOT[g]
        dma("sp", f"gv{sl}", lambda e: e.dma_start(out=gv[:, sl, :], in_=gvec[g:g + 1, :].partition_broadcast(128)), writes=[f"gv{sl}"])
    op("pool", lambda e: e.memset(identf[:], 0.0), writes=["identf"])
    op("pool", lambda e: e.affine_select(out=identf[:], in_=identf[:], pattern=[[-1, 128]], compare_op=ALU.not_equal,
                                          fill=1.0, base=0, channel_multiplier=1), reads=["identf"], writes=["identf"])
    op("dve", lambda e: e.tensor_copy(out=identb[:], in_=identf[:]), reads=["identf"], writes=["identb"])
    op("dve", lambda e: e.memset(onesb[:], 1.0), writes=["onesb"])
    op("dve", lambda e: e.memset(epsb[:], EPS), writes=["epsb"])
    op("pool", lambda e: e.memset(ltrif[:], 1.0), writes=["ltrif"])
    op("pool", lambda e: e.affine_select(out=ltrif[:], in_=ltrif[:], pattern=[[1, 128]], compare_op=ALU.is_gt,
                                          fill=0.0, base=0, channel_multiplier=-1), reads=["ltrif"], writes=["ltrif"])
    op("dve", lambda e: e.tensor_copy(out=ltri[:], in_=ltrif[:]), reads=["ltrif"], writes=["ltri"])
    load_gain(0)

    zt = sb("zt", [128, 2, D], BF16, es)
    op("pool", lambda e: e.memset(zt[:], 0.0), writes=["zt"])
    dbg_outs = {}

    def dump(name, shape, dt, src_ap, reads):
        if not dbg:
            return
        t = nc.dram_tensor("dbg_" + name, list(shape), dt, kind="ExternalOutput").ap()
        dbg_outs[name] = t
        dma("sp", "dbg_" + name, lambda e: e.dma_start(out=t, in_=src_ap), reads=reads, writes=["dbg_" + name])

    def rmsnorm_tile(xt, xname, gi, hb, hname, ss, rs, junk, hf=None, hfname=None):
        op("act", lambda e: e.activation(out=junk[:], in_=xt, func=AF.Square, accum_out=ss[:, 0:1]),
           reads=[xname], writes=["junk", "ss"])
        op("act", lambda e: e.activation(out=rs[:, 0:1], in_=ss[:, 0:1], func=AF.Sqrt, bias=epsb[:, 0:1], scale=1.0 / D),
           reads=["ss", "epsb"], writes=["rs"])
        op("dve", lambda e: e.reciprocal(out=rs[:, 0:1], in_=rs[:, 0:1]), reads=["rs"], writes=["rs"])
        if hf is not None:
            op("dve", lambda e: e.scalar_tensor_tensor(out=hf, in0=xt, scalar=rs[:, 0:1], in1=gv[:, GSLOT[gi], :], op0=ALU.mult, op1=ALU.mult),
               reads=[xname, "rs", f"gv{GSLOT[gi]}"], writes=[hfname])
            op("act", lambda e: e.copy(out=hb, in_=hf), reads=[hfname], writes=[hname])
        else:
            op("dve", lambda e: e.scalar_tensor_tensor(out=hb, in0=xt, scalar=rs[:, 0:1], in1=gv[:, GSLOT[gi], :], op0=ALU.mult, op1=ALU.mult),
               reads=[xname, "rs", f"gv{GSLOT[gi]}"], writes=[hname])

    def load_w(name, src2d, dst, ncols, q="pool"):
        dma(q, name, lambda e: e.dma_start(out=dst, in_=src2d.rearrange("(dc p) f -> p dc f", p=128)), writes=[name])

    with ExitStack() as sA:
        ynT = sb("ynT", [128, 8, TOK], BF16, sA)
        gcf_sb = sb("gcf_sb", [128, 8], F32, sA)
        dma("sp", "gcf", lambda e: e.dma_start(out=gcf_sb[:], in_=gcf), writes=["gcf"])
        S.barrier()
        with ExitStack() as sA0:
            BT = sb("BT", [128, 4, TOK], F32, sA0)
            CVx = sb("CVx", [128, 4, TOK + 2], F32, sA0)
            S.barrier()
            with ExitStack() as s1:
                Win = sb("Win", [128, 8, 2048], BF16, s1)
                load_w("Win", w_in, Win[:], 2048)
                if full:
                    for r in range(NSLOT // 256):
                        dma("pool", "zfill", lambda e, r=r: e.dma_start(out=Xd[r * 256:(r + 1) * 256, :].rearrange("(p n) d -> p n d", n=2), in_=zt[:]),
                            reads=["zt"], writes=[])
                    S.res["Xd"] = [(S.dsem["zfill"], S.dcnt["zfill"], "dma"), {}]
                hT4 = [sb(f"hT4_{i}", [128, 8, 512], BF16, s1) for i in range(2)]
                xb = [sb(f"xb_{i}", [128, D], F32, s1) for i in range(3)]
                hb = [sb(f"hb_{i}", [128, D], BF16, s1) for i in range(2)]
                ub = [sb(f"ub_{i}", [128, 512], BF16, s1) for i in range(2)]
                junk = sb("junk", [128, D], BF16, s1)
                ss = sb("ss", [128, 1], F32, s1)
                rs = sb("rs", [128, 1], F32, s1)
                Ctmp = sb("Ctmp", [128, 4, 512], F32, s1)
                hTh = sb("hTh", [128, 8, 128], BF16, s1)
                Chs = sb("Chs", [128, 8], F32, s1)
                CVh = sb("CVh", [128, 8], F32, s1)
                psT = [ps(f"psT_{i}", [128, 8, 128], BF16, s1) for i in range(2)]
                psU = [ps(f"psU_{i}", [128, 512], F32, s1) for i in range(2)]
                psZ = [ps(f"psZ_{i}", [128, 512], F32, s1) for i in range(3)]
                psH = ps("psH", [128, 16], F32, s1)

                def norm_and_transpose(src_ap, i, dstT, dstname, dst_cols):
                    k3, k2 = i % 3, i % 2
                    dma("sp", f"xb{k3}", lambda e: e.dma_start(out=xb[k3][:], in_=src_ap), writes=[f"xb{k3}"])
                    rmsnorm_tile(xb[k3][:], f"xb{k3}", 0, hb[k2][:], f"hb{k2}", ss, rs, junk)
                    for dc in range(8):
                        op("pe", lambda e, dc=dc: e.transpose(out=psT[k2][:, dc, :], in_=hb[k2][:, dc * 128:(dc + 1) * 128], identity=identb[:]),
                           reads=[f"hb{k2}", "identb"], writes=[f"psT{k2}"], inc=(dc == 7))
                    op("act", lambda e: e.copy(out=dstT[:, :, dst_cols], in_=psT[k2][:]), reads=[f"psT{k2}"], writes=[dstname])

                norm_and_transpose(xhalo, 0, hTh, "hTh", slice(0, 128))
                for j in range(8):
                    for dc in range(8):
                        op("pe", lambda e, j=j, dc=dc: e.matmul(out=psH[:, 2 * j:2 * j + 2], lhsT=Win[:, dc, 512 + j * 128:512 + (j + 1) * 128],
                                                               rhs=hTh[:, dc, 0:2], start=(dc == 0), stop=(dc == 7)),
                           reads=["Win", "hTh"], writes=["psH"], inc=(dc == 7))
                op("act", lambda e: e.copy(out=Chs[:], in_=psH[:, 0:8]), reads=["psH"], writes=["Chs"])
                op("dve", lambda e: e.tensor_tensor(out=CVh[:], in0=psH[:, 8:16], in1=Chs[:], op=ALU.mult), reads=["psH", "Chs"], writes=["CVh"])
                CVh3 = CVh[:].rearrange("p (c t) -> p c t", t=2)
                op("dve", lambda e: e.tensor_copy(out=CVx[:, :, 0:1], in_=CVh3[:, :, 0:1]), reads=["CVh"], writes=["CVxh0"])
                op("dve", lambda e: e.tensor_copy(out=CVx[:, :, TOK + 1:TOK + 2], in_=CVh3[:, :, 1:2]), reads=["CVh"], writes=["CVxh1"])

                def a1_s0(i):
                    k3, k2 = (i + 1) % 3, (i + 1) % 2
                    dma("sp", f"xb{k3}", lambda e: e.dma_start(out=xb[k3][:], in_=xrot[i * 128:(i + 1) * 128, :]), writes=[f"xb{k3}"])
                    rmsnorm_tile(xb[k3][:], f"xb{k3}", 0, hb[k2][:], f"hb{k2}", ss, rs, junk)

                def a1_s1(i):
                    k2 = (i + 1) % 2
                    grp, t = i // 4, i % 4
                    g2 = grp % 2
                    for dc in range(8):
                        op("pe", lambda e, dc=dc: e.transpose(out=psT[k2][:, dc, :], in_=hb[k2][:, dc * 128:(dc + 1) * 128], identity=identb[:]),
                           reads=[f"hb{k2}", "identb"], writes=[f"psT{k2}"], inc=(dc == 7))
                    op("act", lambda e: e.copy(out=hT4[g2][:, :, t * 128:(t + 1) * 128], in_=psT[k2][:]), reads=[f"psT{k2}"], writes=[f"hT4_{g2}"])

                def a1_s2(i):
                    grp, t = i // 4, i % 4
                    g2 = grp % 2
                    u2 = i % 2
                    for dc in range(8):
                        op("pe", lambda e, dc=dc: e.matmul(out=psU[u2][:], lhsT=hT4[g2][:, dc, t * 128:(t + 1) * 128], rhs=Win[:, dc, 1536:2048],
                                                          start=(dc == 0), stop=(dc == 7)),
                           reads=["Win", f"hT4_{g2}"], writes=[f"psU{u2}"], inc=(dc == 7))
                    op("dve", lambda e: e.tensor_copy(out=ub[u2][:], in_=psU[u2][:]), reads=[f"psU{u2}"], writes=[f"ub{u2}"])
                    dma("pool", f"ubst{u2}", lambda e: e.dma_start(out=U_d[i * 128:(i + 1) * 128, :], in_=ub[u2][:]), reads=[f"ub{u2}"], writes=[f"U_d{i}"])
                    if t == 3 and grp < 4:
                        for j in range(12):
                            z3 = j % 3
                            for dc in range(8):
                                op("pe", lambda e, j=j, dc=dc: e.matmul(out=psZ[z3][:], lhsT=Win[:, dc, j * 128:(j + 1) * 128], rhs=hT4[g2][:, dc, :],
                                                                       start=(dc == 0), stop=(dc == 7)),
                                   reads=["Win", f"hT4_{g2}"], writes=[f"psZ{z3}"], inc=(dc == 7))
                            cols = slice(grp * 512, (grp + 1) * 512)
                            if j < 4:
                                op("act", lambda e, j=j: e.copy(out=BT[:, j, cols], in_=psZ[z3][:]), reads=[f"psZ{z3}"], writes=[f"BT{j}"])
                            elif j < 8:
                                op("act", lambda e, j=j: e.copy(out=Ctmp[:, j - 4, :], in_=psZ[z3][:]), reads=[f"psZ{z3}"], writes=[f"Ctmp{j - 4}"])
                            else:
                                op("dve", lambda e, j=j: e.tensor_tensor(out=CVx[:, j - 8, 1 + grp * 512:1 + (grp + 1) * 512], in0=psZ[z3][:], in1=Ctmp[:, j - 8, :], op=ALU.mult),
                                   reads=[f"psZ{z3}", f"Ctmp{j - 8}"], writes=[f"CVx{j - 8}"])

                for step in range(64 + 2):
                    if step < 64:
                        a1_s0(step)
                    if 0 <= step - 1 < 64:
                        a1_s1(step - 1)
                    if 0 <= step - 2 < 64:
                        a1_s2(step - 2)
            S.barrier()
            with ExitStack() as s2:
                cw_sb = sb("cw_sb", [128, 4, 3], F32, s2)
                dma("sp", "cw", lambda e: e.dma_start(out=cw_sb[:], in_=cw), writes=["cw"])
                T1 = sb("T1", [128, TOK], F32, s2)
                sq = [sb(f"sq_{i}", [128, 512], BF16, s2) for i in range(2)]
                rstd = sb("rstd", [128, TOK], F32, s2)
                psN = [ps(f"psN_{i}", [128, 512], F32, s2) for i in range(2)]
                for c in range(4):
                    rd = [f"CVx{c}", "CVxh0", "CVxh1", "cw"]
                    op("dve", lambda e, c=c: e.tensor_scalar(out=T1[:], in0=CVx[:, c, 0:TOK], scalar1=cw_sb[:, c, 0:1], scalar2=None, op0=ALU.mult),
                       reads=rd, writes=["T1"])
                    op("dve", lambda e, c=c: e.scalar_tensor_tensor(out=T1[:], in0=CVx[:, c, 1:TOK + 1], scalar=cw_sb[:, c, 1:2], in1=T1[:], op0=ALU.mult, op1=ALU.add),
                       reads=rd + ["T1"], writes=["T1"])
                    op("dve", lambda e, c=c: e.scalar_tensor_tensor(out=T1[:], in0=CVx[:, c, 2:TOK + 2], scalar=cw_sb[:, c, 2:3], in1=T1[:], op0=ALU.mult, op1=ALU.add),
                       reads=rd + ["T1"], writes=["T1"])
                    op("dve", lambda e, c=c: e.tensor_tensor(out=BT[:, c, :], in0=T1[:], in1=BT[:, c, :], op=ALU.mult), reads=["T1", f"BT{c}"], writes=[f"BT{c}"])

                def branch_norm(Y, ynames, goff):
                    for tb in range(4):
                        cols = slice(tb * 512, (tb + 1) * 512)
                        n2 = tb % 2
                        for c in range(4):
                            s2i = (tb * 4 + c) % 2
                            op("act", lambda e, c=c: e.activation(out=sq[s2i][:], in_=Y[:, c, cols], func=AF.Square), reads=[ynames[c]], writes=[f"sq{s2i}"])
                            op("pe", lambda e, c=c: e.matmul(out=psN[n2][:], lhsT=onesb[:], rhs=sq[s2i][:], start=(c == 0), stop=(c == 3)),
                               reads=["onesb", f"sq{s2i}"], writes=[f"psN{n2}"])
                        op("act", lambda e: e.activation(out=rstd[:, cols], in_=psN[n2][:], func=AF.Sqrt, bias=epsb[:, 0:1], scale=1.0 / 512),
                           reads=[f"psN{n2}", "epsb"], writes=[f"rstd{tb}"])
                        op("dve", lambda e: e.reciprocal(out=rstd[:, cols], in_=rstd[:, cols]), reads=[f"rstd{tb}"], writes=[f"rstd{tb}"])
                        for c in range(4):
                            op("dve", lambda e, c=c: e.scalar_tensor_tensor(out=ynT[:, goff + c, cols], in0=Y[:, c, cols], scalar=gcf_sb[:, goff + c:goff + c + 1],
                                                                          in1=rstd[:, cols], op0=ALU.mult, op1=ALU.mult),
                               reads=[ynames[c], "gcf", f"rstd{tb}"], writes=[f"ynT{goff + c}"])

                branch_norm(BT, [f"BT{c}" for c in range(4)], 0)
                if dbg:
                    dump("yc", [128, 4, TOK], F32, BT[:], [f"BT{c}" for c in range(4)])
        S.barrier()
        Wo_ = sb("Wout", [128, 8, D], BF16, sA)
        load_w("Wout", w_out, Wo_[:], D)
        S.barrier()
        with ExitStack() as s3:
            Us = sb("Us", [128, 64, 512], BF16, s3)
            f1 = sb("f1", [128, 2, 128], BF16, s3)
            dma("pool", "f1", lambda e: e.dma_start(out=f1[:, 0, :], in_=f1c), writes=["f1"])
            dma("pool", "f1", lambda e: e.dma_start(out=f1[:, 1, :], in_=f1s), writes=["f1"])
            Ast = [sb(f"Ast_{i}", [128, 2, 4, 512], BF16, s3) for i in range(2)]
            psA = [ps(f"psA_{i}", [128, 512], F32, s3) for i in range(4)]
            for h in range(4):
                dma("sp", f"Us{h}", lambda e, h=h: e.dma_start(out=Us[:, h * 16:(h + 1) * 16, :],
                                                          in_=U_d.rearrange("(s1 s2) c -> s1 s2 c", s2=64)[:, h * 16:(h + 1) * 16, :]),
                    reads=[f"U_d{i}" for i in range(64)], writes=[f"Us{h}"])
            A_v = A_d.rearrange("r s k c -> k r s c")
            for sblk in range(16):
                a2 = sblk % 2
                for sl in range(4):
                    s2_ = sblk * 4 + sl
                    for ri in range(2):
                        p4 = (s2_ * 2 + ri) % 4
                        op("pe", lambda e, ri=ri, s2_=s2_: e.matmul(out=psA[p4][:], lhsT=f1[:, ri, :], rhs=Us[:, s2_, :], start=True, stop=True),
                           reads=["f1", f"Us{s2_ // 16}"], writes=[f"psA{p4}"])
                        if ri == 0:
                            op("act", lambda e, sl=sl: e.copy(out=Ast[a2][:, 0, sl, :], in_=psA[p4][:]), reads=[f"psA{p4}"], writes=[f"Ast{a2}"])
                        else:
                            op("dve", lambda e, sl=sl: e.tensor_copy(out=Ast[a2][:, 1, sl, :], in_=psA[p4][:]), reads=[f"psA{p4}"], writes=[f"Ast{a2}"])
                for ri in range(2):
                    dma("pool", f"Ast{a2}_{ri}", lambda e, ri=ri, sblk=sblk: e.dma_start(out=A_v[:, ri, sblk * 4:(sblk + 1) * 4, :], in_=Ast[a2][:, ri, :, :]),
                        reads=[f"Ast{a2}"], writes=[f"A_d{sblk}_{ri}"])
        S.barrier()
        with ExitStack() as s4:
            f3_sb = sb("f3_sb", [128, 128, 32], BF16, s4)
            dma("pool", "f3", lambda e: e.dma_start(out=f3_sb[:], in_=f3), writes=["f3"])
            bd_sb = sb("bd_sb", [128, 2, 128], BF16, s4)
            dma("pool", "bd", lambda e: e.dma_start(out=bd_sb[:, 0, :], in_=bdc), writes=["bd"])
            dma("pool", "bd", lambda e: e.dma_start(out=bd_sb[:, 1, :], in_=bds), writes=["bd"])
            Ach = [sb(f"Ach_{i}", [128, 16, 512], BF16, s4) for i in range(2)]
            XT = sb("XT", [128, 4, 128, 32], BF16, s4)
            yf = sb("yf", [128, 4, TOK], F32, s4)
            sq = [sb(f"sqf_{i}", [128, 512], BF16, s4) for i in range(2)]
            rstd = sb("rstdf", [128, TOK], F32, s4)
            psX = [ps(f"psX_{i}", [128, 16, 32], F32, s4) for i in range(4)]
            psY = [ps(f"psY_{i}", [128, 32, 16], F32, s4) for i in range(2)]
            psN = [ps(f"psNf_{i}", [128, 512], F32, s4) for i in range(2)]
            A_r = A_d.rearrange("r s k c -> (r s) k c")
            for kc in range(8):
                a2 = kc % 2
                dma("sp", f"Ach{a2}", lambda e, kc=kc: e.dma_start(out=Ach[a2][:], in_=A_r[:, kc * 16:(kc + 1) * 16, :]), reads=[f"A_d{sb_}_{ri_}" for sb_ in range(16) for ri_ in range(2)], writes=[f"Ach{a2}"])
                for cc in range(4):
                    for kl in range(16):
                        k1 = kc * 16 + kl
                        op("pe", lambda e, cc=cc, kl=kl, k1=k1: e.matmul(out=psX[cc][:, kl, :], lhsT=Ach[a2][:, kl, cc * 128:(cc + 1) * 128], rhs=f3_sb[:, k1, :],
                                                                       start=True, stop=True),
                           reads=[f"Ach{a2}", "f3"], writes=[f"psX{cc}"], inc=(kl == 15))
                    if cc % 2 == 0:
                        op("act", lambda e, cc=cc, kc=kc: e.copy(out=XT[:, cc, kc * 16:(kc + 1) * 16, :], in_=psX[cc][:]), reads=[f"psX{cc}"], writes=[f"XT{cc}"])
                    else:
                        op("dve", lambda e, cc=cc, kc=kc: e.tensor_copy(out=XT[:, cc, kc * 16:(kc + 1) * 16, :], in_=psX[cc][:]), reads=[f"psX{cc}"], writes=[f"XT{cc}"])
            scale = 1.0 / float(np.sqrt(8192.0 * 64.0))
            for cc in range(4):
                yv = yf[:, cc, :].rearrange("p (k2 k1) -> p k1 k2", k1=128)
                for kq in range(4):
                    y2 = (cc * 4 + kq) % 2
                    op("pe", lambda e, cc=cc, kq=kq: e.matmul(out=psY[y2][:], lhsT=bd_sb[:, 0, :], rhs=XT[:, cc, kq * 32:(kq + 1) * 32, 0:16], start=True, stop=False),
                       reads=["bd", f"XT{cc}"], writes=[f"psY{y2}"], inc=False)
                    op("pe", lambda e, cc=cc, kq=kq: e.matmul(out=psY[y2][:], lhsT=bd_sb[:, 1, :], rhs=XT[:, cc, kq * 32:(kq + 1) * 32, 16:32], start=False, stop=True),
                       reads=["bd", f"XT{cc}"], writes=[f"psY{y2}"])
                    op("act", lambda e, kq=kq, yv=yv: e.activation(out=yv[:, kq * 32:(kq + 1) * 32, :], in_=psY[y2][:], func=AF.Copy, scale=scale),
                       reads=[f"psY{y2}"], writes=[f"yf{cc}"])
            branch_norm_names = [f"yf{c}" for c in range(4)]
            for tb in range(4):
                cols = slice(tb * 512, (tb + 1) * 512)
                n2 = tb % 2
                for c in range(4):
                    s2i = (tb * 4 + c) % 2
                    op("act", lambda e, c=c: e.activation(out=sq[s2i][:], in_=yf[:, c, cols], func=AF.Square), reads=[f"yf{c}"], writes=[f"sqf{s2i}"])
                    op("pe", lambda e, c=c: e.matmul(out=psN[n2][:], lhsT=onesb[:], rhs=sq[s2i][:], start=(c == 0), stop=(c == 3)),
                       reads=["onesb", f"sqf{s2i}"], writes=[f"psNf{n2}"])
                op("act", lambda e: e.activation(out=rstd[:, cols], in_=psN[n2][:], func=AF.Sqrt, bias=epsb[:, 0:1], scale=1.0 / 512),
                   reads=[f"psNf{n2}", "epsb"], writes=[f"rstdf{tb}"])
                op("dve", lambda e: e.reciprocal(out=rstd[:, cols], in_=rstd[:, cols]), reads=[f"rstdf{tb}"], writes=[f"rstdf{tb}"])
                for c in range(4):
                    op("dve", lambda e, c=c: e.scalar_tensor_tensor(out=ynT[:, 4 + c, cols], in0=yf[:, c, cols], scalar=gcf_sb[:, 4 + c:5 + c],
                                                                  in1=rstd[:, cols], op0=ALU.mult, op1=ALU.mult),
                       reads=[f"yf{c}", "gcf", f"rstdf{tb}"], writes=[f"ynT{4 + c}"])
            if dbg:
                dump("yf", [128, 4, TOK], F32, yf[:], branch_norm_names)
        S.barrier()
        with ExitStack() as s5:
            xb = [sb(f"xr_{i}", [128, D], F32, s5) for i in range(2)]
            psW = [ps(f"psW_{i}", [128, 512], F32, s5) for i in range(4)]
            yn_names = [f"ynT{c}" for c in range(8)]
            for i in range(NT):
                k2 = i % 2
                dma("sp", f"xr{k2}", lambda e, i=i: e.dma_start(out=xb[k2][:], in_=xrot[i * 128:(i + 1) * 128, :]), writes=[f"xr{k2}"])
                for dh in range(2):
                    p4 = (i * 2 + dh) % 4
                    for c in range(8):
                        op("pe", lambda e, c=c, dh=dh, i=i: e.matmul(out=psW[p4][:], lhsT=ynT[:, c, i * 128:(i + 1) * 128], rhs=Wo_[:, c, dh * 512:(dh + 1) * 512],
                                                                   start=(c == 0), stop=(c == 7)),
                           reads=yn_names + ["Wout"], writes=[f"psW{p4}"], inc=(c == 7))
                    op("dve", lambda e, dh=dh: e.tensor_tensor(out=xb[k2][:, dh * 512:(dh + 1) * 512], in0=psW[p4][:], in1=xb[k2][:, dh * 512:(dh + 1) * 512], op=ALU.add),
                       reads=[f"psW{p4}", f"xr{k2}"], writes=[f"xr{k2}"])
                dma("pool", f"x1st{k2}", lambda e, i=i: e.dma_start(out=X1_d[i * 128:(i + 1) * 128, :], in_=xb[k2][:]), reads=[f"xr{k2}"], writes=[f"X1_d{i}"])

    if stage == "A":
        dma("sp", "fin", lambda e: e.dma_start(out=out, in_=X1_d), reads=[f"X1_d{i}" for i in range(NT)], writes=["out"])
        S.finish("sp", ["out"] + ["dbg_" + n for n in dbg_outs])
        return nc, es, dbg_outs

    S.barrier()
    with ExitStack() as sM:
        bc_reg = nc.gpsimd.to_reg(NSLOT - 1)
        IDX = sb("IDX", [128, NT, 4], I32, sM)
        WK = sb("WK", [128, NT, 4], F32, sM)
        S.nobar.update(["Wgu0", "Wgu1", "Wdn0", "Wdn1", "zfill", "bgu"])
        if full:
            Wgu = [sb(f"Wgu_{i}", [128, 8, 2048], BF16, sM) for i in range(2)]
            Wdn = [sb(f"Wdn_{i}", [128, 8, D], BF16, sM) for i in range(2)]
            bgu = sb("bgu", [128, NE, 16], F32, sM)
            dma("sp", "bgu", lambda e: e.dma_start(out=bgu[:], in_=b_gu), writes=["bgu"])

        def load_expert(e_):
            k = e_ % 2
            load_w(f"Wgu{k}", w_gu[e_], Wgu[k][:], 2048)
            load_w(f"Wdn{k}", w_dn[e_], Wdn[k][:], D)

        S.barrier()
        with ExitStack() as sB:
            KT = sb("KT", [128, 8, 256], BF16, sB)
            Vb = sb("Vb", [128, 2, D], BF16, sB)
            junk = sb("junkB", [128, D], BF16, sB)
            ss = sb("ssB", [128, 1], F32, sB)
            load_gain(2)
            load_gain(1)
            rs = sb("rsB", [128, 1], F32, sB)
            S.barrier()
            with ExitStack() as sK:
                Wk = sb("Wk", [128, 8, D], BF16, sK)
                Wv = sb("Wv", [128, 8, D], BF16, sK)
                load_w("Wk", w_k, Wk[:], D)
                load_w("Wv", w_v, Wv[:], D)
                mt_ = [sb(f"mt_{i}", [128, D], F32, sK) for i in range(2)]
                mb_ = [sb(f"mb_{i}", [128, D], BF16, sK) for i in range(2)]
                memT = sb("memT", [128, 8, 256], BF16, sK)
                psT = [ps(f"psTk_{i}", [128, 8, 128], BF16, sK) for i in range(2)]
                psK = [ps(f"psK_{i}", [128, 512], F32, sK) for i in range(2)]
                for m in range(2):
                    dma("sp", f"mt{m}", lambda e, m=m: e.dma_start(out=mt_[m][:], in_=memb[m * 128:(m + 1) * 128, :]), writes=[f"mt{m}"])
                    rmsnorm_tile(mt_[m][:], f"mt{m}", 2, mb_[m][:], f"mb{m}", ss, rs, junk)
                    for dc in range(8):
                        op("pe", lambda e, dc=dc, m=m: e.transpose(out=psT[m][:, dc, :], in_=mb_[m][:, dc * 128:(dc + 1) * 128], identity=identb[:]),
                           reads=[f"mb{m}", "identb"], writes=[f"psTk{m}"], inc=(dc == 7))
                    op("act", lambda e, m=m: e.copy(out=memT[:, :, m * 128:(m + 1) * 128], in_=psT[m][:]), reads=[f"psTk{m}"], writes=["memT"])
                for j in range(8):
                    k2 = j % 2
                    for dc in range(8):
                        op("pe", lambda e, j=j, dc=dc: e.matmul(out=psK[k2][:, 0:256], lhsT=Wk[:, dc, j * 128:(j + 1) * 128], rhs=memT[:, dc, :], start=(dc == 0), stop=(dc == 7)),
                           reads=["Wk", "memT"], writes=[f"psK{k2}"], inc=(dc == 7))
                    op("act", lambda e, j=j: e.copy(out=KT[:, j, :], in_=psK[k2][:, 0:256]), reads=[f"psK{k2}"], writes=["KT"])
                for m in range(2):
                    for dh in range(2):
                        k2 = (m * 2 + dh) % 2
                        for dc in range(8):
                            op("pe", lambda e, m=m, dh=dh, dc=dc: e.matmul(out=psK[k2][:], lhsT=memT[:, dc, m * 128:(m + 1) * 128], rhs=Wv[:, dc, dh * 512:(dh + 1) * 512],
                                                                         start=(dc == 0), stop=(dc == 7)),
                               reads=["Wv", "memT"], writes=[f"psK{k2}"], inc=(dc == 7))
                        op("dve", lambda e, m=m, dh=dh: e.tensor_copy(out=Vb[:, m, dh * 512:(dh + 1) * 512], in_=psK[k2][:]), reads=[f"psK{k2}"], writes=["Vb"])
            S.barrier()
            with ExitStack() as sQ:
                Wq = sb("Wq", [128, 8, D], BF16, sQ)
                Wo = sb("Wo", [128, 8, D], BF16, sQ)
                load_w("Wq", w_q, Wq[:], D)
                load_w("Wo", w_o, Wo[:], D)
                if full:
                    load_expert(0)
                    load_expert(1)
                xt = [sb(f"x1_{i}", [128, D], F32, sQ) for i in range(4)]
                hb = [sb(f"h2b_{i}", [128, D], BF16, sQ) for i in range(4)]
                hT4 = sb("h2T4", [128, 8, 512], BF16, sQ)
                QT4 = sb("QT4", [128, 8, 512], BF16, sQ)
                E = sb("E", [128, 4, 256], F32, sQ)
                Pb = sb("Pb", [128, 4, 256], BF16, sQ)
                PT = sb("PT", [128, 8, 128], BF16, sQ)
                OT = sb("OT", [128, 8, 128], BF16, sQ)
                mx = sb("mx", [128, 4], F32, sQ)
                nmx = sb("nmx", [128, 4], F32, sQ)
                sm = sb("sm", [128, 4], F32, sQ)
                rsm = sb("rsm", [128, 4], F32, sQ)
                psT = ps("psTq", [128, 8, 128], BF16, sQ)
                psQ = ps("psQ", [128, 512], F32, sQ)
                psS = ps("psS", [128, 4, 256], F32, sQ)
                psPT = ps("psPT", [128, 8, 128], BF16, sQ)
                psO = ps("psO", [128, 8, 128], F32, sQ)
                psW = ps("psWo", [128, 512], F32, sQ)
                Pb2 = [Pb, sb("Pb_1", [128, 4, 256], BF16, sQ)]

                def b_pro(grp):
                    for t in range(4):
                        i = grp * 4 + t
                        dma("sp", f"x1l{t}", lambda e, i=i, t=t: e.dma_start(out=xt[t][:], in_=X1_d[i * 128:(i + 1) * 128, :]), reads=[f"X1_d{i}"], writes=[f"x1_{t}"])
                        rmsnorm_tile(xt[t][:], f"x1_{t}", 1, hb[t][:], f"h2b{t}", ss, rs, junk)
                    for t in range(4):
                        k2 = t
                        for dc in range(8):
                            op("pe", lambda e, dc=dc, k2=k2: e.transpose(out=psT[:, dc, :], in_=hb[k2][:, dc * 128:(dc + 1) * 128], identity=identb[:]),
                               reads=[f"h2b{k2}", "identb"], writes=["psTq"], inc=(dc == 7))
                        op("act", lambda e, t=t: e.copy(out=hT4[:, :, t * 128:(t + 1) * 128], in_=psT[:]), reads=["psTq"], writes=["h2T4"])
                    for j in range(8):
                        pq, pqn = (psQ, "psQ") if j % 2 == 0 else (psW, "psWo")
                        for dc in range(8):
                            op("pe", lambda e, j=j, dc=dc, pq=pq: e.matmul(out=pq[:], lhsT=Wq[:, dc, j * 128:(j + 1) * 128], rhs=hT4[:, dc, :], start=(dc == 0), stop=(dc == 7)),
                               reads=["Wq", "h2T4"], writes=[pqn], inc=(dc == 7))
                        if j % 2 == 0:
                            op("act", lambda e, j=j, pq=pq: e.copy(out=QT4[:, j, :], in_=pq[:]), reads=[pqn], writes=["QT4"])
                        else:
                            op("dve", lambda e, j=j, pq=pq: e.tensor_copy(out=QT4[:, j, :], in_=pq[:]), reads=[pqn], writes=["QT4"])

                def b_s1(i):
                    t = i % 4
                    p2 = i % 2
                    tc_ = slice(t * 128, (t + 1) * 128)
                    for hh in range(4):
                        for hf in range(2):
                            op("pe", lambda e, hh=hh, hf=hf: e.matmul(out=psS[:, hh, :], lhsT=QT4[:, hh * 2 + hf, tc_], rhs=KT[:, hh * 2 + hf, :], start=(hf == 0), stop=(hf == 1)),
                               reads=["QT4", "KT"], writes=["psS"], inc=(hf == 1))
                    op("dve", lambda e: e.tensor_reduce(out=mx[:], in_=psS[:], axis=AX.X, op=ALU.max), reads=["psS"], writes=["mx"])
                    op("dve", lambda e: e.tensor_scalar(out=nmx[:], in0=mx[:], scalar1=-1.0 / 16.0, scalar2=None, op0=ALU.mult), reads=["mx"], writes=["nmx"])
                    for hh in range(4):
                        op("act", lambda e, hh=hh: e.activation(out=E[:, hh, :], in_=psS[:, hh, :], func=AF.Exp, bias=nmx[:, hh:hh + 1], scale=1.0 / 16.0,
                                                               accum_out=sm[:, hh:hh + 1]),
                           reads=["psS", "nmx"], writes=["E", "sm"])
                    op("dve", lambda e: e.reciprocal(out=rsm[:], in_=sm[:]), reads=["sm"], writes=["rsm"])
                    for hh in range(4):
                        op("dve", lambda e, hh=hh: e.tensor_scalar(out=Pb2[p2][:, hh, :], in0=E[:, hh, :], scalar1=rsm[:, hh:hh + 1], scalar2=None, op0=ALU.mult),
                           reads=["E", "rsm"], writes=[f"Pb{p2}"])

                def b_s2(i):
                    t = i % 4
                    p2 = i % 2
                    for hh in range(4):
                        for m in range(2):
                            op("pe", lambda e, hh=hh, m=m: e.transpose(out=psPT[:, hh * 2 + m, :], in_=Pb2[p2][:, hh, m * 128:(m + 1) * 128], identity=identb[:]),
                               reads=[f"Pb{p2}", "identb"], writes=["psPT"], inc=(hh == 3 and m == 1))
                    op("act", lambda e: e.copy(out=PT[:], in_=psPT[:]), reads=["psPT"], writes=["PT"])
                    for hh in range(4):
                        for hf in range(2):
                            c = hh * 2 + hf
                            for m in range(2):
                                op("pe", lambda e, hh=hh, m=m, c=c: e.matmul(out=psO[:, c, :], lhsT=Vb[:, m, c * 128:(c + 1) * 128], rhs=PT[:, hh * 2 + m, :],
                                                                           start=(m == 0), stop=(m == 1)),
                                   reads=["Vb", "PT"], writes=["psO"], inc=(c == 7 and m == 1))
                    op("act", lambda e: e.copy(out=OT[:], in_=psO[:]), reads=["psO"], writes=["OT"])
                    for dh in range(2):
                        for c in range(8):
                            op("pe", lambda e, c=c, dh=dh: e.matmul(out=psW[:], lhsT=OT[:, c, :], rhs=Wo[:, c, dh * 512:(dh + 1) * 512], start=(c == 0), stop=(c == 7)),
                               reads=["OT", "Wo"], writes=["psWo"], inc=(c == 7))
                        op("dve", lambda e, dh=dh: e.tensor_tensor(out=xt[t][:, dh * 512:(dh + 1) * 512], in0=psW[:], in1=xt[t][:, dh * 512:(dh + 1) * 512], op=ALU.add),
                           reads=["psWo", f"x1_{t}"], writes=[f"x1_{t}"])
                    dma("pool", f"x2st{t}", lambda e: e.dma_start(out=X2_d[i * 128:(i + 1) * 128, :], in_=xt[t][:]), reads=[f"x1_{t}"], writes=[f"X2_d{i}"])

                for grp in range(4):
                    b_pro(grp)
                    for t in range(4):
                        b_s1(grp * 4 + t)
                        if t > 0:
                            b_s2(grp * 4 + t - 1)
                    b_s2(grp * 4 + 3)

        if stage == "B":
            dma("sp", "fin", lambda e: e.dma_start(out=out, in_=X2_d), reads=[f"X2_d{i}" for i in range(NT)], writes=["out"])
            S.finish("sp", ["out"] + ["dbg_" + n for n in dbg_outs])
            return nc, es, dbg_outs


        S.barrier()
        with ExitStack() as sC:
            Wr = sb("Wr", [128, 8, NE], F32, sC)
            dma("sp", "Wr", lambda e: e.dma_start(out=Wr[:], in_=w_r.rearrange("(dc p) f -> p dc f", p=128)), writes=["Wr"])
            brb = sb("brb", [128, NE], F32, sC)
            dma("sp", "brb", lambda e: e.dma_start(out=brb[:], in_=b_r.partition_broadcast(128)), writes=["brb"])
            eb1 = sb("eb1", [128, NE], F32, sC)
            dma("sp", "eb1", lambda e: e.dma_start(out=eb1[:], in_=ebase), writes=["eb1"])
            load_gain(3)
            masks = sb("masks", [128, NT, NE], BF16, sC)
            xt = [sb(f"x2_{i}", [128, D], F32, sC) for i in range(2)]
            hf = [sb(f"h3f_{i}", [128, D], F32, sC) for i in range(2)]
            hb = [sb(f"h3b_{i}", [128, D], BF16, sC) for i in range(2)]
            hT = sb("h3T", [128, 8, 128], F32, sC)
            junk = sb("junkC", [128, D], BF16, sC)
            ss = sb("ssC", [128, 1], F32, sC)
            rs = sb("rsC", [128, 1], F32, sC)
            lg = sb("lg", [128, NE], F32, sC)
            m8 = sb("m8", [128, 8], F32, sC)
            nm = sb("nm", [128, 1], F32, sC)
            mk = sb("mk", [128, NE], F32, sC)
            ex = sb("ex", [128, NE], F32, sC)
            em = sb("em", [128, NE], F32, sC)
            sme = sb("sme", [128, 1], F32, sC)
            wt = sb("wt", [128, NE], F32, sC)
            key = sb("key", [128, NE], F32, sC)
            k8 = sb("k8", [128, 8], F32, sC)
            eq = sb("eq", [128, NE], F32, sC)
            psT = [ps(f"psTr_{i}", [128, 4, 128], F32, sC) for i in range(2)]
            psL = ps("psL", [128, NE], F32, sC)
            psP = ps("psP", [128, NE], F32, sC)
            def c_s0(i):
                k2 = i % 2
                dma("sp", f"x2l{k2}", lambda e: e.dma_start(out=xt[k2][:], in_=X2_d[i * 128:(i + 1) * 128, :]), reads=[f"X2_d{i}"], writes=[f"x2_{k2}"])
                rmsnorm_tile(xt[k2][:], f"x2_{k2}", 3, hb[k2][:], f"h3b{k2}", ss, rs, junk, hf=hf[k2][:], hfname=f"h3f{k2}")

            def c_s1(i):
                k2 = i % 2
                for dc in range(8):
                    op("pe", lambda e, dc=dc: e.transpose(out=psT[dc // 4][:, dc % 4, :], in_=hf[k2][:, dc * 128:(dc + 1) * 128], identity=identf[:]),
                       reads=[f"h3f{k2}", "identf"], writes=[f"psTr{dc // 4}"], inc=(dc % 4 == 3))
                op("act", lambda e: e.copy(out=hT[:, 0:4, :], in_=psT[0][:]), reads=["psTr0"], writes=["h3Ta"])
                op("dve", lambda e: e.tensor_copy(out=hT[:, 4:8, :], in_=psT[1][:]), reads=["psTr1"], writes=["h3Tb"])
                for dc in range(8):
                    op("pe", lambda e, dc=dc: e.matmul(out=psL[:], lhsT=hT[:, dc, :], rhs=Wr[:, dc, :], start=(dc == 0), stop=(dc == 7)),
                       reads=["h3Ta", "h3Tb", "Wr"], writes=["psL"], inc=(dc == 7))
                op("dve", lambda e: e.tensor_tensor(out=lg[:], in0=psL[:], in1=brb[:], op=ALU.add), reads=["psL", "brb"], writes=["lg"])
                op("dve", lambda e: e.max(out=m8[:], in_=lg[:]), reads=["lg"], writes=["m8"])
                op("dve", lambda e: e.tensor_scalar(out=mk[:], in0=lg[:], scalar1=m8[:, 3:4], scalar2=None, op0=ALU.is_ge), reads=["lg", "m8"], writes=["mk"])
                op("dve", lambda e, i=i: e.tensor_copy(out=masks[:, i, :], in_=mk[:]), reads=["mk"], writes=[f"masks{i}"])
                op("dve", lambda e: e.tensor_scalar(out=nm[:], in0=m8[:, 0:1], scalar1=-1.0, scalar2=None, op0=ALU.mult), reads=["m8"], writes=["nm"])
                op("act", lambda e: e.activation(out=ex[:], in_=lg[:], func=AF.Exp, bias=nm[:, 0:1], scale=1.0), reads=["lg", "nm"], writes=["ex"])
                op("dve", lambda e: e.tensor_tensor(out=em[:], in0=ex[:], in1=mk[:], op=ALU.mult), reads=["ex", "mk"], writes=["em"])
                op("dve", lambda e: e.reduce_sum(out=sme[:], in_=em[:], axis=AX.X), reads=["em"], writes=["sme"])
                op("dve", lambda e: e.reciprocal(out=sme[:], in_=sme[:]), reads=["sme"], writes=["sme"])
                op("dve", lambda e: e.tensor_scalar(out=wt[:], in0=em[:], scalar1=sme[:, 0:1], scalar2=None, op0=ALU.mult), reads=["em", "sme"], writes=["wt"])
                op("pe", lambda e, i=i: e.matmul(out=psP[:], lhsT=ltri[:], rhs=masks[:, i, :], start=True, stop=(i == 0)),
                   reads=["ltri", f"masks{i}"], writes=["psP"], inc=(i == 0))
                for j in range(i):
                    op("pe", lambda e, j=j, i=i: e.matmul(out=psP[:], lhsT=onesb[:], rhs=masks[:, j, :], start=False, stop=(j == i - 1)),
                       reads=["onesb", f"masks{j}"], writes=["psP"], inc=(j == i - 1))
                op("dve", lambda e: e.tensor_tensor(out=key[:], in0=psP[:], in1=eb1[:], op=ALU.add), reads=["psP", "eb1"], writes=["key"])
                op("dve", lambda e: e.tensor_tensor(out=key[:], in0=key[:], in1=mk[:], op=ALU.mult), reads=["key", "mk"], writes=["key"])
                op("dve", lambda e: e.max(out=k8[:], in_=key[:]), reads=["key"], writes=["k8"])
                op("dve", lambda e, i=i: e.tensor_scalar(out=IDX[:, i, :], in0=k8[:, 0:4], scalar1=-1.0, scalar2=None, op0=ALU.add), reads=["k8"], writes=[f"IDX{i}"])
                for k in range(4):
                    op("dve", lambda e, k=k: e.tensor_scalar(out=eq[:], in0=key[:], scalar1=k8[:, k:k + 1], scalar2=None, op0=ALU.is_equal), reads=["key", "k8"], writes=["eq"])
                    op("dve", lambda e: e.tensor_tensor(out=eq[:], in0=eq[:], in1=wt[:], op=ALU.mult), reads=["eq", "wt"], writes=["eq"])
                    op("dve", lambda e, k=k, i=i: e.reduce_sum(out=WK[:, i, k:k + 1], in_=eq[:], axis=AX.X), reads=["eq"], writes=[f"WK{i}"])
                for k in range(4):
                    S._deps("pool", ["Xd"], [])
                    dma("pool", f"disp{k2}_{k}", lambda e, k=k, i=i: e.indirect_dma_start(out=Xd, out_offset=bass.IndirectOffsetOnAxis(ap=IDX[:, i, k:k + 1], axis=0),
                                                                                      in_=hb[k2][:, :], in_offset=None, bounds_check=bc_reg, oob_is_err=False),
                        reads=[f"h3b{k2}", f"IDX{i}"], writes=[f"Xd_disp{k2}_{k}"])

            c_s0(0)
            for i in range(NT):
                if i + 1 < NT:
                    c_s0(i + 1)
                c_s1(i)
        S.barrier()
        with ExitStack() as sD:
            Xe = [sb(f"Xe_{i}", [128, 3, D], BF16, sD) for i in range(2)]
            XTe = [sb(f"XTe_{i}", [128, 8, CAP], BF16, sD) for i in range(2)]
            actT = [sb(f"actT_{i}", [128, 8, CAP], BF16, sD) for i in range(2)]
            bdb = [sb(f"bdb_{i}", [128, D], F32, sD) for i in range(2)]
            Oe = [sb(f"Oe_{i}", [128, 3, D], F32, sD) for i in range(2)]
            g_ = [sb(f"g_{i}", [128, CAP], F32, sD) for i in range(2)]
            sg_ = [sb(f"sg_{i}", [128, CAP], F32, sD) for i in range(2)]
            u_ = [sb(f"u_{i}", [128, CAP], F32, sD) for i in range(2)]
            psXT = [ps(f"psXT_{i}", [128, 3, 128], BF16, sD) for i in range(2)]
            psG = [ps(f"psG_{i}", [128, 512], F32, sD) for i in range(2)]
            psUp = [ps(f"psUp_{i}", [128, 512], F32, sD) for i in range(2)]
            psD = [ps(f"psD_{i}", [128, 512], F32, sD) for i in range(2)]
            def ex_load(e_):
                k = e_ % 2
                dma("sp", f"Xe{k}", lambda e: e.dma_start(out=Xe[k][:], in_=Xd[e_ * CAP:(e_ + 1) * CAP, :].rearrange("(b p) d -> p b d", p=128)),
                    reads=["Xd"] + [f"Xd_disp{a_}_{b_}" for a_ in range(2) for b_ in range(4)], writes=[f"Xe{k}"])
                dma("sp", f"bdb{k}", lambda e: e.dma_start(out=bdb[k][:], in_=b_dn[e_:e_ + 1, :].partition_broadcast(128)), writes=[f"bdb{k}"])

            def ex_tr(e_):
                k = e_ % 2
                for dc in range(8):
                    x2 = dc % 2
                    for b in range(3):
                        op("pe", lambda e, dc=dc, b=b: e.transpose(out=psXT[x2][:, b, :], in_=Xe[k][:, b, dc * 128:(dc + 1) * 128], identity=identb[:]),
                           reads=[f"Xe{k}", "identb"], writes=[f"psXT{x2}"], inc=(b == 2))
                    if dc % 2 == 0:
                        op("act", lambda e, dc=dc: e.copy(out=XTe[k][:, dc, :], in_=psXT[x2][:]), reads=[f"psXT{x2}"], writes=[f"XTe{k}"])
                    else:
                        op("dve", lambda e, dc=dc: e.tensor_copy(out=XTe[k][:, dc, :], in_=psXT[x2][:]), reads=[f"psXT{x2}"], writes=[f"XTe{k}"])

            def ex_gu(e_):
                k = e_ % 2
                for j in range(8):
                    j2 = j % 2
                    for dc in range(8):
                        op("pe", lambda e, j=j, dc=dc: e.matmul(out=psG[j2][:, 0:CAP], lhsT=Wgu[k][:, dc, j * 128:(j + 1) * 128], rhs=XTe[k][:, dc, :], start=(dc == 0), stop=(dc == 7)),
                           reads=[f"Wgu{k}", f"XTe{k}"], writes=[f"psG{j2}"], inc=(dc == 7))
                    for dc in range(8):
                        op("pe", lambda e, j=j, dc=dc: e.matmul(out=psUp[j2][:, 0:CAP], lhsT=Wgu[k][:, dc, 1024 + j * 128:1024 + (j + 1) * 128], rhs=XTe[k][:, dc, :],
                                                               start=(dc == 0), stop=(dc == 7)),
                           reads=[f"Wgu{k}", f"XTe{k}"], writes=[f"psUp{j2}"], inc=(dc == 7))
                    op("dve", lambda e, j=j: e.tensor_scalar(out=g_[j2][:], in0=psG[j2][:, 0:CAP], scalar1=bgu[:, e_, j:j + 1], scalar2=7.0, op0=ALU.add, op1=ALU.min),
                       reads=[f"psG{j2}", "bgu"], writes=[f"g{j2}"])
                    op("act", lambda e: e.activation(out=sg_[j2][:], in_=g_[j2][:], func=AF.Silu, scale=1.702), reads=[f"g{j2}"], writes=[f"sg{j2}"])
                    op("act", lambda e, j=j: e.activation(out=u_[j2][:], in_=psUp[j2][:, 0:CAP], func=AF.Identity, bias=bgu[:, e_, 8 + j:9 + j], scale=1.0),
                       reads=[f"psUp{j2}", "bgu"], writes=[f"u{j2}"])
                    op("dve", lambda e: e.tensor_scalar(out=u_[j2][:], in0=u_[j2][:], scalar1=7.0, scalar2=-7.0, op0=ALU.min, op1=ALU.max), reads=[f"u{j2}"], writes=[f"u{j2}"])
                    op("dve", lambda e, j=j: e.scalar_tensor_tensor(out=actT[k][:, j, :], in0=u_[j2][:], scalar=1.0, in1=sg_[j2][:], op0=ALU.add, op1=ALU.mult),
                       reads=[f"sg{j2}", f"u{j2}"], writes=[f"actT{k}"])

            def ex_dn(e_):
                k = e_ % 2
                for b in range(3):
                    for dh in range(2):
                        d2 = (b * 2 + dh) % 2
                        for j in range(8):
                            op("pe", lambda e, b=b, dh=dh, j=j: e.matmul(out=psD[d2][:], lhsT=actT[k][:, j, b * 128:(b + 1) * 128], rhs=Wdn[k][:, j, dh * 512:(dh + 1) * 512],
                                                                       start=(j == 0), stop=(j == 7)),
                               reads=[f"actT{k}", f"Wdn{k}"], writes=[f"psD{d2}"], inc=(j == 7))
                        op("dve", lambda e, b=b, dh=dh: e.scalar_tensor_tensor(out=Oe[k][:, b, dh * 512:(dh + 1) * 512], in0=psD[d2][:], scalar=1.0 / 1.702,
                                                                                in1=bdb[k][:, dh * 512:(dh + 1) * 512], op0=ALU.mult, op1=ALU.add),
                           reads=[f"psD{d2}", f"bdb{k}"], writes=[f"Oe{k}"])
                dma("sp", f"Oest{k}", lambda e: e.dma_start(out=O_d[e_ * CAP:(e_ + 1) * CAP, :].rearrange("(b p) d -> p b d", p=128), in_=Oe[k][:]),
                    reads=[f"Oe{k}"], writes=["O_d"])

            ex_load(0)
            ex_tr(0)
            for e_ in range(NE):
                if e_ + 1 < NE:
                    ex_load(e_ + 1)
                ex_gu(e_)
                if e_ + 1 < NE:
                    ex_tr(e_ + 1)
                ex_dn(e_)
                if e_ + 2 < NE:
                    load_expert(e_ + 2)
        S.barrier()
        with ExitStack() as sE:
            xt = [sb(f"x2c_{i}", [128, D], F32, sE) for i in range(2)]
            G = [sb(f"G_{i}", [128, D], F32, sE) for i in range(4)]
            ob = [sb(f"ob_{i}", [128, D], F32, sE) for i in range(2)]
            load_gain(4)
            junk = sb("junkE", [128, D], BF16, sE)
            ss = sb("ssE", [128, 1], F32, sE)
            rs = sb("rsE", [128, 1], F32, sE)
            G8 = G + [sb(f"G_{i}", [128, D], F32, sE) for i in range(4, 8)]

            def e_s0(i):
                k2 = i % 2
                dma("sp", f"x2c{k2}", lambda e: e.dma_start(out=xt[k2][:], in_=X2_d[i * 128:(i + 1) * 128, :]), reads=[f"X2_d{i}"], writes=[f"x2c{k2}"])
                for k in range(4):
                    gi = k2 * 4 + k
                    dma("pool", f"G{gi}", lambda e, k=k, gi=gi: e.indirect_dma_start(out=G8[gi][:, :], out_offset=None, in_=O_d,
                                                                                  in_offset=bass.IndirectOffsetOnAxis(ap=IDX[:, i, k:k + 1], axis=0),
                                                                                  bounds_check=bc_reg, oob_is_err=False),
                        reads=["O_d", f"IDX{i}"], writes=[f"G{gi}"])

            def e_s1(i):
                k2 = i % 2
                for k in range(4):
                    gi = k2 * 4 + k
                    op("dve", lambda e, k=k, gi=gi: e.scalar_tensor_tensor(out=xt[k2][:], in0=G8[gi][:], scalar=WK[:, i, k:k + 1], in1=xt[k2][:], op0=ALU.mult, op1=ALU.add),
                       reads=[f"G{gi}", f"WK{i}", f"x2c{k2}"], writes=[f"x2c{k2}"])
                op("act", lambda e: e.activation(out=junk[:], in_=xt[k2][:], func=AF.Square, accum_out=ss[:, 0:1]), reads=[f"x2c{k2}"], writes=["junkE", "ssE"])
                op("act", lambda e: e.activation(out=rs[:, 0:1], in_=ss[:, 0:1], func=AF.Sqrt, bias=epsb[:, 0:1], scale=1.0 / D), reads=["ssE", "epsb"], writes=["rsE"])
                op("dve", lambda e: e.reciprocal(out=rs[:, 0:1], in_=rs[:, 0:1]), reads=["rsE"], writes=["rsE"])
                op("dve", lambda e: e.scalar_tensor_tensor(out=ob[k2][:], in0=xt[k2][:], scalar=rs[:, 0:1], in1=gv[:, GSLOT[4], :], op0=ALU.mult, op1=ALU.mult),
                   reads=[f"x2c{k2}", "rsE", f"gv{GSLOT[4]}"], writes=[f"ob{k2}"])
                dma("sp", f"ost{k2}", lambda e: e.dma_start(out=out[i * 128:(i + 1) * 128, :], in_=ob[k2][:]), reads=[f"ob{k2}"], writes=[f"out{i}"])

            e_s0(0)
            for i in range(NT):
                if i + 1 < NT:
                    e_s0(i + 1)
                e_s1(i)
    S.finish("sp", [f"out{i}" for i in range(NT)] + ["dbg_" + n for n in dbg_outs])
    return nc, es, dbg_outs


def host_inputs(inputs, stage="full", cores=range(8)):
    f = np.float32
    x = np.asarray(inputs["x"], f)
    mem = np.asarray(inputs["mem"], f)
    gvec = np.stack([inputs["norm_mix"][0], inputs["norm_xattn"][0], inputs["norm_mem"][0], inputs["norm_ffn"][0], inputs["norm_final"]]).astype(f)
    cwv = np.asarray(inputs["conv_w"][0], f)
    cw = np.ascontiguousarray(cwv.reshape(3, 4, 128).transpose(2, 1, 0))
    gc = np.asarray(inputs["g_conv_out"][0], f).reshape(4, 128).T
    gf = np.asarray(inputs["g_fft_out"][0], f).reshape(4, 128).T
    gcf = np.ascontiguousarray(np.concatenate([gc, gf], axis=1))
    a = np.arange(128)
    f1c = np.cos(2 * np.pi * np.outer(a, a) / 128).astype(f)
    f1s = (-np.sin(2 * np.pi * np.outer(a, a) / 128)).astype(f)
    c64 = np.arange(64)
    C64 = np.cos(2 * np.pi * np.outer(c64, c64) / 64)
    S64 = np.sin(2 * np.pi * np.outer(c64, c64) / 64)
    bdc = np.zeros((128, 128)); bds = np.zeros((128, 128))
    for g in range(2):
        bdc[g * 64:(g + 1) * 64, g * 64:(g + 1) * 64] = C64
        bds[g * 64:(g + 1) * 64, g * 64:(g + 1) * 64] = S64
    ebase = np.broadcast_to((np.arange(NE) * CAP + 1).astype(f), (128, NE)).copy()
    common = {
        "gvec": gvec, "w_in": np.asarray(inputs["w_in"][0], f), "cw": cw, "gcf": gcf,
        "w_out": np.asarray(inputs["w_out"][0], f), "w_q": np.asarray(inputs["w_q"][0], f), "w_k": np.asarray(inputs["w_k"][0], f),
        "w_v": np.asarray(inputs["w_v"][0], f), "w_o": np.asarray(inputs["w_o"][0], f), "w_r": np.asarray(inputs["w_router"][0], f),
        "b_r": np.asarray(inputs["b_router"], f).reshape(1, NE), "f1c": f1c, "f1s": f1s, "bdc": bdc.astype(f), "bds": bds.astype(f), "ebase": ebase,
    }
    if stage == "full":
        common["w_gu"] = np.asarray(inputs["w_gate_up"][0], f)
        common["b_gu"] = np.ascontiguousarray(np.asarray(inputs["b_gate_up"][0], f).reshape(NE, 16, 128).transpose(2, 0, 1))
        common["w_dn"] = np.asarray(inputs["w_down"][0], f)
        common["b_dn"] = np.asarray(inputs["b_down"][0], f)
    maps = []
    s2 = np.arange(64)
    k1 = np.arange(128)
    for c in cores:
        b, q = c // 4, c % 4
        xr = np.roll(x[b], -TOK * q, axis=0)
        xh = np.zeros((128, D), f)
        if q > 0:
            xh[0] = x[b, TOK * q - 1]
        if q < 3:
            xh[1] = x[b, TOK * (q + 1)]
        k2 = 16 * q + np.arange(16)
        kk = k1[:, None] + 128 * k2[None, :]
        phi = 2 * np.pi * s2[:, None, None] * kk[None] / 8192.0 + np.pi * kk[None] * q / 2.0
        gcos, gsin = np.cos(phi), np.sin(phi)
        t = np.zeros((2, 64, 128, 32))
        t[0, :, :, 0:16] = gcos; t[1, :, :, 0:16] = gsin
        t[0, :, :, 16:32] = -gsin; t[1, :, :, 16:32] = gcos
        m = dict(common)
        m.update({"xrot": np.ascontiguousarray(xr), "xhalo": xh, "memb": np.ascontiguousarray(mem[b]), "f3": t.reshape(128, 128, 32).astype(f)})
        maps.append(m)
    return maps


def kernel(**inputs):
    nc, es, _ = build("full")
    maps = host_inputs(inputs)
    res = run_bass_kernel_spmd(nc, maps, core_ids=list(range(8)))
    outs = [np.asarray(r["out"], np.float32) for r in res.results]
    y = np.stack(outs).reshape(2, 4 * TOK, D)
    return y
```

```python
import numpy as np
from contextlib import ExitStack
import concourse.bass as bass
import concourse.mybir as mybir
from concourse.bass_utils import run_bass_kernel_spmd

F32 = mybir.dt.float32
BF16 = mybir.dt.bfloat16
I32 = mybir.dt.int32
ALU = mybir.AluOpType
AF = mybir.ActivationFunctionType
AX = mybir.AxisListType

D = 1024
SEQ = 8192
TOK = 2048
NT = TOK // 128
NE = 32
CAP = 384
NSLOT = NE * CAP
EPS = 1e-5


class Sched:
    def __init__(self, nc, es):
        self.nc = nc
        self.es = es
        self.eng = {"pe": nc.tensor, "act": nc.scalar, "dve": nc.vector, "pool": nc.gpsimd, "sp": nc.sync}
        self.sem = {k: es.enter_context(nc.semaphore("c_" + k)) for k in self.eng}
        self.cnt = {k: 0 for k in self.eng}
        self.waited = {k: {} for k in self.eng}
        self.dsem = {}
        self.dcnt = {}
        self.res = {}
        self.nobar = set()
        self.semname = {}

    def _wait(self, e, ev):
        if ev is None:
            return
        s, v, owner = ev
        if owner == "pe" and e == "pe":
            return
        w = self.waited[e]
        if w.get(id(s), 0) >= v:
            return
        self.eng[e].wait_ge(s, v)
        w[id(s)] = v

    def _deps(self, e, reads, writes):
        for r in reads:
            st = self.res.get(r)
            if st:
                self._wait(e, st[0])
        for wname in writes:
            st = self.res.get(wname)
            if st:
                self._wait(e, st[0])
                for ev in list(st[1].values()):
                    self._wait(e, ev)

    def _record(self, ev, reads, writes):
        for r in reads:
            st = self.res.setdefault(r, [None, {}])
            old = st[1].get(id(ev[0]))
            if old is None or old[1] < ev[1]:
                st[1][id(ev[0])] = ev
        for wname in writes:
            self.res[wname] = [ev, {}]

    def op(self, e, fn, reads=(), writes=(), inc=True):
        self._deps(e, reads, writes)
        ins = fn(self.eng[e])
        if inc:
            self.cnt[e] += 1
            ins.then_inc(self.sem[e], 1)
            ev = (self.sem[e], self.cnt[e], e)
        else:
            ev = (self.sem[e], self.cnt[e] + 1, e)
        self._record(ev, reads, writes)
        return ins

    def dma(self, q, key, fn, reads=(), writes=()):
        self._deps(q, reads, writes)
        if key not in self.dsem:
            self.dsem[key] = self.es.enter_context(self.nc.semaphore("d_" + key))
            self.dcnt[key] = 0
        ins = fn(self.eng[q])
        self.dcnt[key] += 16
        ins.then_inc(self.dsem[key], 16)
        ev = (self.dsem[key], self.dcnt[key], "dma")
        self._record(ev, reads, writes)
        return ins

    def barrier(self):
        evs = [(self.sem[o], self.cnt[o], o) for o in self.eng if self.cnt[o] > 0]
        evs += [(self.dsem[k], self.dcnt[k], "dma") for k in self.dsem if k not in self.nobar]
        for e in self.eng:
            for ev in evs:
                if ev[2] == e and e != "pe":
                    continue
                if ev[2] == "pe" and e == "pe":
                    continue
                self._wait(e, ev)
        self.res = {k: v for k, v in self.res.items()}

    def finish(self, q, names):
        for n in names:
            st = self.res.get(n)
            if st:
                self._wait(q, st[0])
                for ev in list(st[1].values()):
                    self._wait(q, ev)


def build(stage="full", dbg=False):
    nc = bass.Bass("TRN2", target_bir_lowering=False)
    es = ExitStack()

    def din(name, shape, dt=F32):
        return nc.dram_tensor(name, list(shape), dt, kind="ExternalInput").ap()

    def dscr(name, shape, dt):
        return nc.dram_tensor(name, list(shape), dt, kind="Internal").ap()

    xrot = din("xrot", [SEQ, D])
    xhalo = din("xhalo", [128, D])
    memb = din("memb", [256, D])
    gvec = din("gvec", [5, D])
    w_in = din("w_in", [D, 2048])
    cw = din("cw", [128, 4, 3])
    gcf = din("gcf", [128, 8])
    w_out = din("w_out", [D, D])
    w_q = din("w_q", [D, D]); w_k = din("w_k", [D, D]); w_v = din("w_v", [D, D]); w_o = din("w_o", [D, D])
    w_r = din("w_r", [D, NE])
    b_r = din("b_r", [1, NE])
    f1c = din("f1c", [128, 128]); f1s = din("f1s", [128, 128])
    f3 = din("f3", [128, 128, 32])
    bdc = din("bdc", [128, 128]); bds = din("bds", [128, 128])
    ebase = din("ebase", [128, NE])
    full = stage == "full"
    if full:
        w_gu = din("w_gu", [NE, D, 2048])
        b_gu = din("b_gu", [128, NE, 16])
        w_dn = din("w_dn", [NE, D, D])
        b_dn = din("b_dn", [NE, D])
    out = nc.dram_tensor("out", [TOK, D], F32, kind="ExternalOutput").ap()

    U_d = dscr("U_d", [SEQ, 512], BF16)
    A_d = dscr("A_d", [2, 64, 128, 512], BF16)
    X1_d = dscr("X1_d", [TOK, D], F32)
    X2_d = dscr("X2_d", [TOK, D], F32)
    Xd = dscr("Xd", [NSLOT, D], BF16)
    O_d = dscr("O_d", [NSLOT, D], F32)

    S = Sched(nc, es)
    op, dma = S.op, S.dma

    def sb(name, shape, dt, stack):
        return stack.enter_context(nc.sbuf_tensor(name, list(shape), dt))

    def ps(name, shape, dt, stack):
        return stack.enter_context(nc.psum_tensor(name, list(shape), dt))

    identf = sb("identf", [128, 128], F32, es)
    identb = sb("identb", [128, 128], BF16, es)
    onesb = sb("onesb", [128, 128], BF16, es)
    ltri = sb("ltri", [128, 128], BF16, es)
    ltrif = sb("ltrif", [128, 128], F32, es)
    epsb = sb("epsb", [128, 1], F32, es)
    gv = sb("gv", [128, 2, D], F32, es)
    GSLOT = {0: 0, 2: 0, 1: 1, 3: 0, 4: 1}

    def load_gain(g):
        sl = GSLOT[g]
        dma("sp", f"gv{sl}", lambda e: e.dma_start(out=gv[:, sl, :], in_=gvec[g:g + 1, :].partition_broadcast(128)), writes=[f"gv{sl}"])
    op("pool", lambda e: e.memset(identf[:], 0.0), writes=["identf"])
    op("pool", lambda e: e.affine_select(out=identf[:], in_=identf[:], pattern=[[-1, 128]], compare_op=ALU.not_equal,
                                          fill=1.0, base=0, channel_multiplier=1), reads=["identf"], writes=["identf"])
    op("dve", lambda e: e.tensor_copy(out=identb[:], in_=identf[:]), reads=["identf"], writes=["identb"])
    op("dve", lambda e: e.memset(onesb[:], 1.0), writes=["onesb"])
    op("dve", lambda e: e.memset(epsb[:], EPS), writes=["epsb"])
    op("pool", lambda e: e.memset(ltrif[:], 1.0), writes=["ltrif"])
    op("pool", lambda e: e.affine_select(out=ltrif[:], in_=ltrif[:], pattern=[[1, 128]], compare_op=ALU.is_gt,
                                          fill=0.0, base=0, channel_multiplier=-1), reads=["ltrif"], writes=["ltrif"])
    op("dve", lambda e: e.tensor_copy(out=ltri[:], in_=ltrif[:]), reads=["ltrif"], writes=["ltri"])
    load_gain(0)

    zt = sb("zt", [128, 2, D], BF16, es)
    op("pool", lambda e: e.memset(zt[:], 0.0), writes=["zt"])
    ZB = NSLOT // 256 // 4

    def zfill_burst(bi):
        if not full:
            return
        for r in range(bi * ZB, (bi + 1) * ZB):
            dma("pool", "zfill", lambda e, r=r: e.dma_start(out=Xd[r * 256:(r + 1) * 256, :].rearrange("(p n) d -> p n d", n=2), in_=zt[:]),
                reads=["zt"], writes=[])
        if bi == 3:
            S.res["Xd"] = [(S.dsem["zfill"], S.dcnt["zfill"], "dma"), {}]

    dbg_outs = {}

    def dump(name, shape, dt, src_ap, reads):
        if not dbg:
            return
        t = nc.dram_tensor("dbg_" + name, list(shape), dt, kind="ExternalOutput").ap()
        dbg_outs[name] = t
        dma("sp", "dbg_" + name, lambda e: e.dma_start(out=t, in_=src_ap), reads=reads, writes=["dbg_" + name])

    def rmsnorm_tile(xt, xname, gi, hb, hname, ss, rs, junk, hf=None, hfname=None):
        op("act", lambda e: e.activation(out=junk[:], in_=xt, func=AF.Square, accum_out=ss[:, 0:1]),
           reads=[xname], writes=["junk", "ss"])
        op("act", lambda e: e.activation(out=rs[:, 0:1], in_=ss[:, 0:1], func=AF.Sqrt, bias=epsb[:, 0:1], scale=1.0 / D),
           reads=["ss", "epsb"], writes=["rs"])
        op("dve", lambda e: e.reciprocal(out=rs[:, 0:1], in_=rs[:, 0:1]), reads=["rs"], writes=["rs"])
        if hf is not None:
            op("dve", lambda e: e.scalar_tensor_tensor(out=hf, in0=xt, scalar=rs[:, 0:1], in1=gv[:, GSLOT[gi], :], op0=ALU.mult, op1=ALU.mult),
               reads=[xname, "rs", f"gv{GSLOT[gi]}"], writes=[hfname])
            op("act", lambda e: e.copy(out=hb, in_=hf), reads=[hfname], writes=[hname])
        else:
            op("dve", lambda e: e.scalar_tensor_tensor(out=hb, in0=xt, scalar=rs[:, 0:1], in1=gv[:, GSLOT[gi], :], op0=ALU.mult, op1=ALU.mult),
               reads=[xname, "rs", f"gv{GSLOT[gi]}"], writes=[hname])

    def load_w(name, src2d, dst, ncols, q="pool"):
        dma(q, name, lambda e: e.dma_start(out=dst, in_=src2d.rearrange("(dc p) f -> p dc f", p=128)), writes=[name])

    with ExitStack() as sA:
        ynT = sb("ynT", [128, 8, TOK], BF16, sA)
        gcf_sb = sb("gcf_sb", [128, 8], F32, sA)
        dma("sp", "gcf", lambda e: e.dma_start(out=gcf_sb[:], in_=gcf), writes=["gcf"])
        S.barrier()
        with ExitStack() as sA0:
            BT = sb("BT", [128, 4, TOK], F32, sA0)
            CVx = sb("CVx", [128, 4, TOK + 2], F32, sA0)
            S.barrier()
            with ExitStack() as s1:
                Win = sb("Win", [128, 8, 2048], BF16, s1)
                load_w("Win", w_in, Win[:], 2048)
                hT4 = [sb(f"hT4_{i}", [128, 8, 512], BF16, s1) for i in range(2)]
                xb = [sb(f"xb_{i}", [128, D], F32, s1) for i in range(3)]
                hb = [sb(f"hb_{i}", [128, D], BF16, s1) for i in range(2)]
                ub = [sb(f"ub_{i}", [128, 512], BF16, s1) for i in range(2)]
                junk = sb("junk", [128, D], BF16, s1)
                ss = sb("ss", [128, 1], F32, s1)
                rs = sb("rs", [128, 1], F32, s1)
                Ctmp = sb("Ctmp", [128, 4, 512], F32, s1)
                hTh = sb("hTh", [128, 8, 128], BF16, s1)
                Chs = sb("Chs", [128, 8], F32, s1)
                CVh = sb("CVh", [128, 8], F32, s1)
                psT = [ps(f"psT_{i}", [128, 8, 128], BF16, s1) for i in range(2)]
                psU = [ps(f"psU_{i}", [128, 512], F32, s1) for i in range(2)]
                psZ = [ps(f"psZ_{i}", [128, 512], F32, s1) for i in range(3)]
                psH = ps("psH", [128, 16], F32, s1)

                def norm_and_transpose(src_ap, i, dstT, dstname, dst_cols):
                    k3, k2 = i % 3, i % 2
                    dma("sp", f"xb{k3}", lambda e: e.dma_start(out=xb[k3][:], in_=src_ap), writes=[f"xb{k3}"])
                    rmsnorm_tile(xb[k3][:], f"xb{k3}", 0, hb[k2][:], f"hb{k2}", ss, rs, junk)
                    for dc in range(8):
                        op("pe", lambda e, dc=dc: e.transpose(out=psT[k2][:, dc, :], in_=hb[k2][:, dc * 128:(dc + 1) * 128], identity=identb[:]),
                           reads=[f"hb{k2}", "identb"], writes=[f"psT{k2}"], inc=(dc == 7))
                    op("act", lambda e: e.copy(out=dstT[:, :, dst_cols], in_=psT[k2][:]), reads=[f"psT{k2}"], writes=[dstname])

                norm_and_transpose(xhalo, 0, hTh, "hTh", slice(0, 128))
                for j in range(8):
                    for dc in range(8):
                        op("pe", lambda e, j=j, dc=dc: e.matmul(out=psH[:, 2 * j:2 * j + 2], lhsT=Win[:, dc, 512 + j * 128:512 + (j + 1) * 128],
                                                               rhs=hTh[:, dc, 0:2], start=(dc == 0), stop=(dc == 7)),
                           reads=["Win", "hTh"], writes=["psH"], inc=(dc == 7))
                op("act", lambda e: e.copy(out=Chs[:], in_=psH[:, 0:8]), reads=["psH"], writes=["Chs"])
                op("dve", lambda e: e.tensor_tensor(out=CVh[:], in0=psH[:, 8:16], in1=Chs[:], op=ALU.mult), reads=["psH", "Chs"], writes=["CVh"])
                CVh3 = CVh[:].rearrange("p (c t) -> p c t", t=2)
                op("dve", lambda e: e.tensor_copy(out=CVx[:, :, 0:1], in_=CVh3[:, :, 0:1]), reads=["CVh"], writes=["CVxh0"])
                op("dve", lambda e: e.tensor_copy(out=CVx[:, :, TOK + 1:TOK + 2], in_=CVh3[:, :, 1:2]), reads=["CVh"], writes=["CVxh1"])

                def a1_s0(i):
                    k3, k2 = (i + 1) % 3, (i + 1) % 2
                    dma("sp", f"xb{k3}", lambda e: e.dma_start(out=xb[k3][:], in_=xrot[i * 128:(i + 1) * 128, :]), writes=[f"xb{k3}"])
                    rmsnorm_tile(xb[k3][:], f"xb{k3}", 0, hb[k2][:], f"hb{k2}", ss, rs, junk)

                def a1_s1(i):
                    k2 = (i + 1) % 2
                    grp, t = i // 4, i % 4
                    g2 = grp % 2
                    for dc in range(8):
                        op("pe", lambda e, dc=dc: e.transpose(out=psT[k2][:, dc, :], in_=hb[k2][:, dc * 128:(dc + 1) * 128], identity=identb[:]),
                           reads=[f"hb{k2}", "identb"], writes=[f"psT{k2}"], inc=(dc == 7))
                    op("act", lambda e: e.copy(out=hT4[g2][:, :, t * 128:(t + 1) * 128], in_=psT[k2][:]), reads=[f"psT{k2}"], writes=[f"hT4_{g2}"])

                def a1_s2(i):
                    grp, t = i // 4, i % 4
                    g2 = grp % 2
                    u2 = i % 2
                    for dc in range(8):
                        op("pe", lambda e, dc=dc: e.matmul(out=psU[u2][:], lhsT=hT4[g2][:, dc, t * 128:(t + 1) * 128], rhs=Win[:, dc, 1536:2048],
                                                          start=(dc == 0), stop=(dc == 7)),
                           reads=["Win", f"hT4_{g2}"], writes=[f"psU{u2}"], inc=(dc == 7))
                    op("dve", lambda e: e.tensor_copy(out=ub[u2][:], in_=psU[u2][:]), reads=[f"psU{u2}"], writes=[f"ub{u2}"])
                    dma("pool", f"ubst{u2}", lambda e: e.dma_start(out=U_d[i * 128:(i + 1) * 128, :], in_=ub[u2][:]), reads=[f"ub{u2}"], writes=[f"U_d{i}"])
                    if t == 3 and grp < 4:
                        for j in range(12):
                            z3 = j % 3
                            for dc in range(8):
                                op("pe", lambda e, j=j, dc=dc: e.matmul(out=psZ[z3][:], lhsT=Win[:, dc, j * 128:(j + 1) * 128], rhs=hT4[g2][:, dc, :],
                                                                       start=(dc == 0), stop=(dc == 7)),
                                   reads=["Win", f"hT4_{g2}"], writes=[f"psZ{z3}"], inc=(dc == 7))
                            cols = slice(grp * 512, (grp + 1) * 512)
                            if j < 4:
                                op("act", lambda e, j=j: e.copy(out=BT[:, j, cols], in_=psZ[z3][:]), reads=[f"psZ{z3}"], writes=[f"BT{j}"])
                            elif j < 8:
                                op("act", lambda e, j=j: e.copy(out=Ctmp[:, j - 4, :], in_=psZ[z3][:]), reads=[f"psZ{z3}"], writes=[f"Ctmp{j - 4}"])
                            else:
                                op("dve", lambda e, j=j: e.tensor_tensor(out=CVx[:, j - 8, 1 + grp * 512:1 + (grp + 1) * 512], in0=psZ[z3][:], in1=Ctmp[:, j - 8, :], op=ALU.mult),
                                   reads=[f"psZ{z3}", f"Ctmp{j - 8}"], writes=[f"CVx{j - 8}"])

                for step in range(64 + 2):
                    if step < 64:
                        a1_s0(step)
                    if 0 <= step - 1 < 64:
                        a1_s1(step - 1)
                    if 0 <= step - 2 < 64:
                        a1_s2(step - 2)
            S.barrier()
            with ExitStack() as s2:
                cw_sb = sb("cw_sb", [128, 4, 3], F32, s2)
                dma("sp", "cw", lambda e: e.dma_start(out=cw_sb[:], in_=cw), writes=["cw"])
                zfill_burst(0)
                T1 = sb("T1", [128, TOK], F32, s2)
                sq = [sb(f"sq_{i}", [128, 512], BF16, s2) for i in range(2)]
                rstd = sb("rstd", [128, TOK], F32, s2)
                psN = [ps(f"psN_{i}", [128, 512], F32, s2) for i in range(2)]
                for c in range(4):
                    rd = [f"CVx{c}", "CVxh0", "CVxh1", "cw"]
                    op("dve", lambda e, c=c: e.tensor_scalar(out=T1[:], in0=CVx[:, c, 0:TOK], scalar1=cw_sb[:, c, 0:1], scalar2=None, op0=ALU.mult),
                       reads=rd, writes=["T1"])
                    op("dve", lambda e, c=c: e.scalar_tensor_tensor(out=T1[:], in0=CVx[:, c, 1:TOK + 1], scalar=cw_sb[:, c, 1:2], in1=T1[:], op0=ALU.mult, op1=ALU.add),
                       reads=rd + ["T1"], writes=["T1"])
                    op("dve", lambda e, c=c: e.scalar_tensor_tensor(out=T1[:], in0=CVx[:, c, 2:TOK + 2], scalar=cw_sb[:, c, 2:3], in1=T1[:], op0=ALU.mult, op1=ALU.add),
                       reads=rd + ["T1"], writes=["T1"])
                    op("dve", lambda e, c=c: e.tensor_tensor(out=BT[:, c, :], in0=T1[:], in1=BT[:, c, :], op=ALU.mult), reads=["T1", f"BT{c}"], writes=[f"BT{c}"])

                def branch_norm(Y, ynames, goff):
                    for tb in range(4):
                        cols = slice(tb * 512, (tb + 1) * 512)
                        n2 = tb % 2
                        for c in range(4):
                            s2i = (tb * 4 + c) % 2
                            op("act", lambda e, c=c: e.activation(out=sq[s2i][:], in_=Y[:, c, cols], func=AF.Square), reads=[ynames[c]], writes=[f"sq{s2i}"])
                            op("pe", lambda e, c=c: e.matmul(out=psN[n2][:], lhsT=onesb[:], rhs=sq[s2i][:], start=(c == 0), stop=(c == 3)),
                               reads=["onesb", f"sq{s2i}"], writes=[f"psN{n2}"])
                        op("act", lambda e: e.activation(out=rstd[:, cols], in_=psN[n2][:], func=AF.Sqrt, bias=epsb[:, 0:1], scale=1.0 / 512),
                           reads=[f"psN{n2}", "epsb"], writes=[f"rstd{tb}"])
                        op("dve", lambda e: e.reciprocal(out=rstd[:, cols], in_=rstd[:, cols]), reads=[f"rstd{tb}"], writes=[f"rstd{tb}"])
                        for c in range(4):
                            op("dve", lambda e, c=c: e.scalar_tensor_tensor(out=ynT[:, goff + c, cols], in0=Y[:, c, cols], scalar=gcf_sb[:, goff + c:goff + c + 1],
                                                                          in1=rstd[:, cols], op0=ALU.mult, op1=ALU.mult),
                               reads=[ynames[c], "gcf", f"rstd{tb}"], writes=[f"ynT{goff + c}"])

                branch_norm(BT, [f"BT{c}" for c in range(4)], 0)
                if dbg:
                    dump("yc", [128, 4, TOK], F32, BT[:], [f"BT{c}" for c in range(4)])
        S.barrier()
        Wo_ = sb("Wout", [128, 8, D], BF16, sA)
        load_w("Wout", w_out, Wo_[:], D)
        S.barrier()
        with ExitStack() as s3:
            Us = sb("Us", [128, 64, 512], BF16, s3)
            f1 = sb("f1", [128, 2, 128], BF16, s3)
            dma("pool", "f1", lambda e: e.dma_start(out=f1[:, 0, :], in_=f1c), writes=["f1"])
            dma("pool", "f1", lambda e: e.dma_start(out=f1[:, 1, :], in_=f1s), writes=["f1"])
            Ast = [sb(f"Ast_{i}", [128, 2, 4, 512], BF16, s3) for i in range(2)]
            psA = [ps(f"psA_{i}", [128, 512], F32, s3) for i in range(4)]
            for h in range(4):
                dma("sp", f"Us{h}", lambda e, h=h: e.dma_start(out=Us[:, h * 16:(h + 1) * 16, :],
                                                          in_=U_d.rearrange("(s1 s2) c -> s1 s2 c", s2=64)[:, h * 16:(h + 1) * 16, :]),
                    reads=[f"U_d{i}" for i in range(64)], writes=[f"Us{h}"])
            zfill_burst(1)
            A_v = A_d.rearrange("r s k c -> k r s c")
            for sblk in range(16):
                a2 = sblk % 2
                for sl in range(4):
                    s2_ = sblk * 4 + sl
                    for ri in range(2):
                        p4 = (s2_ * 2 + ri) % 4
                        op("pe", lambda e, ri=ri, s2_=s2_: e.matmul(out=psA[p4][:], lhsT=f1[:, ri, :], rhs=Us[:, s2_, :], start=True, stop=True),
                           reads=["f1", f"Us{s2_ // 16}"], writes=[f"psA{p4}"])
                        if ri == 0:
                            op("act", lambda e, sl=sl: e.copy(out=Ast[a2][:, 0, sl, :], in_=psA[p4][:]), reads=[f"psA{p4}"], writes=[f"Ast{a2}"])
                        else:
                            op("dve", lambda e, sl=sl: e.tensor_copy(out=Ast[a2][:, 1, sl, :], in_=psA[p4][:]), reads=[f"psA{p4}"], writes=[f"Ast{a2}"])
                for ri in range(2):
                    dma("pool", f"Ast{a2}_{ri}", lambda e, ri=ri, sblk=sblk: e.dma_start(out=A_v[:, ri, sblk * 4:(sblk + 1) * 4, :], in_=Ast[a2][:, ri, :, :]),
                        reads=[f"Ast{a2}"], writes=[f"A_d{sblk}_{ri}"])
        S.barrier()
        with ExitStack() as s4:
            f3_sb = sb("f3_sb", [128, 128, 32], BF16, s4)
            dma("pool", "f3", lambda e: e.dma_start(out=f3_sb[:], in_=f3), writes=["f3"])
            bd_sb = sb("bd_sb", [128, 2, 128], BF16, s4)
            dma("pool", "bd", lambda e: e.dma_start(out=bd_sb[:, 0, :], in_=bdc), writes=["bd"])
            dma("pool", "bd", lambda e: e.dma_start(out=bd_sb[:, 1, :], in_=bds), writes=["bd"])
            zfill_burst(2)
            Ach = [sb(f"Ach_{i}", [128, 16, 512], BF16, s4) for i in range(2)]
            XT = sb("XT", [128, 4, 128, 32], BF16, s4)
            yf = sb("yf", [128, 4, TOK], F32, s4)
            sq = [sb(f"sqf_{i}", [128, 512], BF16, s4) for i in range(2)]
            rstd = sb("rstdf", [128, TOK], F32, s4)
            psX = [ps(f"psX_{i}", [128, 16, 32], F32, s4) for i in range(4)]
            psY = [ps(f"psY_{i}", [128, 32, 16], F32, s4) for i in range(2)]
            psN = [ps(f"psNf_{i}", [128, 512], F32, s4) for i in range(2)]
            A_r = A_d.rearrange("r s k c -> (r s) k c")
            for kc in range(8):
                a2 = kc % 2
                dma("sp", f"Ach{a2}", lambda e, kc=kc: e.dma_start(out=Ach[a2][:], in_=A_r[:, kc * 16:(kc + 1) * 16, :]), reads=[f"A_d{sb_}_{ri_}" for sb_ in range(16) for ri_ in range(2)], writes=[f"Ach{a2}"])
                for cc in range(4):
                    for kl in range(16):
                        k1 = kc * 16 + kl
                        op("pe", lambda e, cc=cc, kl=kl, k1=k1: e.matmul(out=psX[cc][:, kl, :], lhsT=Ach[a2][:, kl, cc * 128:(cc + 1) * 128], rhs=f3_sb[:, k1, :],
                                                                       start=True, stop=True),
                           reads=[f"Ach{a2}", "f3"], writes=[f"psX{cc}"], inc=(kl == 15))
                    if cc % 2 == 0:
                        op("act", lambda e, cc=cc, kc=kc: e.copy(out=XT[:, cc, kc * 16:(kc + 1) * 16, :], in_=psX[cc][:]), reads=[f"psX{cc}"], writes=[f"XT{cc}"])
                    else:
                        op("dve", lambda e, cc=cc, kc=kc: e.tensor_copy(out=XT[:, cc, kc * 16:(kc + 1) * 16, :], in_=psX[cc][:]), reads=[f"psX{cc}"], writes=[f"XT{cc}"])
            scale = 1.0 / float(np.sqrt(8192.0 * 64.0))
            for cc in range(4):
                yv = yf[:, cc, :].rearrange("p (k2 k1) -> p k1 k2", k1=128)
                for kq in range(4):
                    y2 = (cc * 4 + kq) % 2
                    op("pe", lambda e, cc=cc, kq=kq: e.matmul(out=psY[y2][:], lhsT=bd_sb[:, 0, :], rhs=XT[:, cc, kq * 32:(kq + 1) * 32, 0:16], start=True, stop=False),
                       reads=["bd", f"XT{cc}"], writes=[f"psY{y2}"], inc=False)
                    op("pe", lambda e, cc=cc, kq=kq: e.matmul(out=psY[y2][:], lhsT=bd_sb[:, 1, :], rhs=XT[:, cc, kq * 32:(kq + 1) * 32, 16:32], start=False, stop=True),
                       reads=["bd", f"XT{cc}"], writes=[f"psY{y2}"])
                    op("act", lambda e, kq=kq, yv=yv: e.activation(out=yv[:, kq * 32:(kq + 1) * 32, :], in_=psY[y2][:], func=AF.Copy, scale=scale),
                       reads=[f"psY{y2}"], writes=[f"yf{cc}"])
            branch_norm_names = [f"yf{c}" for c in range(4)]
            for tb in range(4):
                cols = slice(tb * 512, (tb + 1) * 512)
                n2 = tb % 2
                for c in range(4):
                    s2i = (tb * 4 + c) % 2
                    op("act", lambda e, c=c: e.activation(out=sq[s2i][:], in_=yf[:, c, cols], func=AF.Square), reads=[f"yf{c}"], writes=[f"sqf{s2i}"])
                    op("pe", lambda e, c=c: e.matmul(out=psN[n2][:], lhsT=onesb[:], rhs=sq[s2i][:], start=(c == 0), stop=(c == 3)),
                       reads=["onesb", f"sqf{s2i}"], writes=[f"psNf{n2}"])
                op("act", lambda e: e.activation(out=rstd[:, cols], in_=psN[n2][:], func=AF.Sqrt, bias=epsb[:, 0:1], scale=1.0 / 512),
                   reads=[f"psNf{n2}", "epsb"], writes=[f"rstdf{tb}"])
                op("dve", lambda e: e.reciprocal(out=rstd[:, cols], in_=rstd[:, cols]), reads=[f"rstdf{tb}"], writes=[f"rstdf{tb}"])
                for c in range(4):
                    op("dve", lambda e, c=c: e.scalar_tensor_tensor(out=ynT[:, 4 + c, cols], in0=yf[:, c, cols], scalar=gcf_sb[:, 4 + c:5 + c],
                                                                  in1=rstd[:, cols], op0=ALU.mult, op1=ALU.mult),
                       reads=[f"yf{c}", "gcf", f"rstdf{tb}"], writes=[f"ynT{4 + c}"])
            if dbg:
                dump("yf", [128, 4, TOK], F32, yf[:], branch_norm_names)
        S.barrier()
        with ExitStack() as s5:
            zfill_burst(3)
            xb = [sb(f"xr_{i}", [128, D], F32, s5) for i in range(2)]
            psW = [ps(f"psW_{i}", [128, 512], F32, s5) for i in range(4)]
            yn_names = [f"ynT{c}" for c in range(8)]
            for i in range(NT):
                k2 = i % 2
                dma("sp", f"xr{k2}", lambda e, i=i: e.dma_start(out=xb[k2][:], in_=xrot[i * 128:(i + 1) * 128, :]), writes=[f"xr{k2}"])
                for dh in range(2):
                    p4 = (i * 2 + dh) % 4
                    for c in range(8):
                        op("pe", lambda e, c=c, dh=dh, i=i: e.matmul(out=psW[p4][:], lhsT=ynT[:, c, i * 128:(i + 1) * 128], rhs=Wo_[:, c, dh * 512:(dh + 1) * 512],
                                                                   start=(c == 0), stop=(c == 7)),
                           reads=yn_names + ["Wout"], writes=[f"psW{p4}"], inc=(c == 7))
                    op("dve", lambda e, dh=dh: e.tensor_tensor(out=xb[k2][:, dh * 512:(dh + 1) * 512], in0=psW[p4][:], in1=xb[k2][:, dh * 512:(dh + 1) * 512], op=ALU.add),
                       reads=[f"psW{p4}", f"xr{k2}"], writes=[f"xr{k2}"])
                dma("pool", f"x1st{k2}", lambda e, i=i: e.dma_start(out=X1_d[i * 128:(i + 1) * 128, :], in_=xb[k2][:]), reads=[f"xr{k2}"], writes=[f"X1_d{i}"])

    if stage == "A":
        dma("sp", "fin", lambda e: e.dma_start(out=out, in_=X1_d), reads=[f"X1_d{i}" for i in range(NT)], writes=["out"])
        S.finish("sp", ["out"] + ["dbg_" + n for n in dbg_outs])
        return nc, es, dbg_outs

    S.barrier()
    with ExitStack() as sM:
        bc_reg = nc.gpsimd.to_reg(NSLOT - 1)
        IDX = sb("IDX", [128, NT, 4], I32, sM)
        WK = sb("WK", [128, NT, 4], F32, sM)
        S.nobar.update(["Wgu0", "Wgu1", "Wdn0", "Wdn1", "zfill", "bgu"])
        if full:
            Wgu = [sb(f"Wgu_{i}", [128, 8, 2048], BF16, sM) for i in range(2)]
            Wdn = [sb(f"Wdn_{i}", [128, 8, D], BF16, sM) for i in range(2)]
            bgu = sb("bgu", [128, NE, 16], F32, sM)
            dma("sp", "bgu", lambda e: e.dma_start(out=bgu[:], in_=b_gu), writes=["bgu"])

        def load_expert(e_):
            k = e_ % 2
            load_w(f"Wgu{k}", w_gu[e_], Wgu[k][:], 2048)
            load_w(f"Wdn{k}", w_dn[e_], Wdn[k][:], D)

        S.barrier()
        with ExitStack() as sB:
            KT = sb("KT", [128, 8, 256], BF16, sB)
            Vb = sb("Vb", [128, 2, D], BF16, sB)
            junk = sb("junkB", [128, D], BF16, sB)
            ss = sb("ssB", [128, 1], F32, sB)
            load_gain(2)
            load_gain(1)
            rs = sb("rsB", [128, 1], F32, sB)
            S.barrier()
            with ExitStack() as sK:
                Wk = sb("Wk", [128, 8, D], BF16, sK)
                Wv = sb("Wv", [128, 8, D], BF16, sK)
                load_w("Wk", w_k, Wk[:], D)
                load_w("Wv", w_v, Wv[:], D)
                mt_ = [sb(f"mt_{i}", [128, D], F32, sK) for i in range(2)]
                mb_ = [sb(f"mb_{i}", [128, D], BF16, sK) for i in range(2)]
                memT = sb("memT", [128, 8, 256], BF16, sK)
                psT = [ps(f"psTk_{i}", [128, 8, 128], BF16, sK) for i in range(2)]
                psK = [ps(f"psK_{i}", [128, 512], F32, sK) for i in range(2)]
                for m in range(2):
                    dma("sp", f"mt{m}", lambda e, m=m: e.dma_start(out=mt_[m][:], in_=memb[m * 128:(m + 1) * 128, :]), writes=[f"mt{m}"])
                    rmsnorm_tile(mt_[m][:], f"mt{m}", 2, mb_[m][:], f"mb{m}", ss, rs, junk)
                    for dc in range(8):
                        op("pe", lambda e, dc=dc, m=m: e.transpose(out=psT[m][:, dc, :], in_=mb_[m][:, dc * 128:(dc + 1) * 128], identity=identb[:]),
                           reads=[f"mb{m}", "identb"], writes=[f"psTk{m}"], inc=(dc == 7))
                    op("act", lambda e, m=m: e.copy(out=memT[:, :, m * 128:(m + 1) * 128], in_=psT[m][:]), reads=[f"psTk{m}"], writes=["memT"])
                for j in range(8):
                    k2 = j % 2
                    for dc in range(8):
                        op("pe", lambda e, j=j, dc=dc: e.matmul(out=psK[k2][:, 0:256], lhsT=Wk[:, dc, j * 128:(j + 1) * 128], rhs=memT[:, dc, :], start=(dc == 0), stop=(dc == 7)),
                           reads=["Wk", "memT"], writes=[f"psK{k2}"], inc=(dc == 7))
                    op("act", lambda e, j=j: e.copy(out=KT[:, j, :], in_=psK[k2][:, 0:256]), reads=[f"psK{k2}"], writes=["KT"])
                for m in range(2):
                    for dh in range(2):
                        k2 = (m * 2 + dh) % 2
                        for dc in range(8):
                            op("pe", lambda e, m=m, dh=dh, dc=dc: e.matmul(out=psK[k2][:], lhsT=memT[:, dc, m * 128:(m + 1) * 128], rhs=Wv[:, dc, dh * 512:(dh + 1) * 512],
                                                                         start=(dc == 0), stop=(dc == 7)),
                               reads=["Wv", "memT"], writes=[f"psK{k2}"], inc=(dc == 7))
                        op("dve", lambda e, m=m, dh=dh: e.tensor_copy(out=Vb[:, m, dh * 512:(dh + 1) * 512], in_=psK[k2][:]), reads=[f"psK{k2}"], writes=["Vb"])
            S.barrier()
            with ExitStack() as sQ:
                Wq = sb("Wq", [128, 8, D], BF16, sQ)
                Wo = sb("Wo", [128, 8, D], BF16, sQ)
                load_w("Wq", w_q, Wq[:], D)
                load_w("Wo", w_o, Wo[:], D)
                if full:
                    load_expert(0)
                    load_expert(1)
                xt = [sb(f"x1_{i}", [128, D], F32, sQ) for i in range(4)]
                hb = [sb(f"h2b_{i}", [128, D], BF16, sQ) for i in range(4)]
                hT4 = sb("h2T4", [128, 8, 512], BF16, sQ)
                QT4 = sb("QT4", [128, 8, 512], BF16, sQ)
                E = sb("E", [128, 4, 256], F32, sQ)
                Pb = sb("Pb", [128, 4, 256], BF16, sQ)
                PT = sb("PT", [128, 8, 128], BF16, sQ)
                OT = sb("OT", [128, 8, 128], BF16, sQ)
                mx = sb("mx", [128, 4], F32, sQ)
                nmx = sb("nmx", [128, 4], F32, sQ)
                sm = sb("sm", [128, 4], F32, sQ)
                rsm = sb("rsm", [128, 4], F32, sQ)
                psT = ps("psTq", [128, 8, 128], BF16, sQ)
                psQ = ps("psQ", [128, 512], F32, sQ)
                psS = ps("psS", [128, 4, 256], F32, sQ)
                psPT = ps("psPT", [128, 8, 128], BF16, sQ)
                psO = ps("psO", [128, 8, 128], F32, sQ)
                psW = ps("psWo", [128, 512], F32, sQ)
                Pb2 = [Pb, sb("Pb_1", [128, 4, 256], BF16, sQ)]

                def b_pro(grp):
                    for t in range(4):
                        i = grp * 4 + t
                        dma("sp", f"x1l{t}", lambda e, i=i, t=t: e.dma_start(out=xt[t][:], in_=X1_d[i * 128:(i + 1) * 128, :]), reads=[f"X1_d{i}"], writes=[f"x1_{t}"])
                        rmsnorm_tile(xt[t][:], f"x1_{t}", 1, hb[t][:], f"h2b{t}", ss, rs, junk)
                    for t in range(4):
                        k2 = t
                        for dc in range(8):
                            op("pe", lambda e, dc=dc, k2=k2: e.transpose(out=psT[:, dc, :], in_=hb[k2][:, dc * 128:(dc + 1) * 128], identity=identb[:]),
                               reads=[f"h2b{k2}", "identb"], writes=["psTq"], inc=(dc == 7))
                        op("act", lambda e, t=t: e.copy(out=hT4[:, :, t * 128:(t + 1) * 128], in_=psT[:]), reads=["psTq"], writes=["h2T4"])
                    for j in range(8):
                        pq, pqn = (psQ, "psQ") if j % 2 == 0 else (psW, "psWo")
                        for dc in range(8):
                            op("pe", lambda e, j=j, dc=dc, pq=pq: e.matmul(out=pq[:], lhsT=Wq[:, dc, j * 128:(j + 1) * 128], rhs=hT4[:, dc, :], start=(dc == 0), stop=(dc == 7)),
                               reads=["Wq", "h2T4"], writes=[pqn], inc=(dc == 7))
                        if j % 2 == 0:
                            op("act", lambda e, j=j, pq=pq: e.copy(out=QT4[:, j, :], in_=pq[:]), reads=[pqn], writes=["QT4"])
                        else:
                            op("dve", lambda e, j=j, pq=pq: e.tensor_copy(out=QT4[:, j, :], in_=pq[:]), reads=[pqn], writes=["QT4"])

                def b_s1(i):
                    t = i % 4
                    p2 = i % 2
                    tc_ = slice(t * 128, (t + 1) * 128)
                    for hh in range(4):
                        for hf in range(2):
                            op("pe", lambda e, hh=hh, hf=hf: e.matmul(out=psS[:, hh, :], lhsT=QT4[:, hh * 2 + hf, tc_], rhs=KT[:, hh * 2 + hf, :], start=(hf == 0), stop=(hf == 1)),
                               reads=["QT4", "KT"], writes=["psS"], inc=(hf == 1))
                    op("dve", lambda e: e.tensor_reduce(out=mx[:], in_=psS[:], axis=AX.X, op=ALU.max), reads=["psS"], writes=["mx"])
                    op("dve", lambda e: e.tensor_scalar(out=nmx[:], in0=mx[:], scalar1=-1.0 / 16.0, scalar2=None, op0=ALU.mult), reads=["mx"], writes=["nmx"])
                    for hh in range(4):
                        op("act", lambda e, hh=hh: e.activation(out=E[:, hh, :], in_=psS[:, hh, :], func=AF.Exp, bias=nmx[:, hh:hh + 1], scale=1.0 / 16.0,
                                                               accum_out=sm[:, hh:hh + 1]),
                           reads=["psS", "nmx"], writes=["E", "sm"])
                    op("dve", lambda e: e.reciprocal(out=rsm[:], in_=sm[:]), reads=["sm"], writes=["rsm"])
                    for hh in range(4):
                        op("dve", lambda e, hh=hh: e.tensor_scalar(out=Pb2[p2][:, hh, :], in0=E[:, hh, :], scalar1=rsm[:, hh:hh + 1], scalar2=None, op0=ALU.mult),
                           reads=["E", "rsm"], writes=[f"Pb{p2}"])

                def b_s2(i):
                    t = i % 4
                    p2 = i % 2
                    for hh in range(4):
                        for m in range(2):
                            op("pe", lambda e, hh=hh, m=m: e.transpose(out=psPT[:, hh * 2 + m, :], in_=Pb2[p2][:, hh, m * 128:(m + 1) * 128], identity=identb[:]),
                               reads=[f"Pb{p2}", "identb"], writes=["psPT"], inc=(hh == 3 and m == 1))
                    op("act", lambda e: e.copy(out=PT[:], in_=psPT[:]), reads=["psPT"], writes=["PT"])
                    for hh in range(4):
                        for hf in range(2):
                            c = hh * 2 + hf
                            for m in range(2):
                                op("pe", lambda e, hh=hh, m=m, c=c: e.matmul(out=psO[:, c, :], lhsT=Vb[:, m, c * 128:(c + 1) * 128], rhs=PT[:, hh * 2 + m, :],
                                                                           start=(m == 0), stop=(m == 1)),
                                   reads=["Vb", "PT"], writes=["psO"], inc=(c == 7 and m == 1))
                    op("act", lambda e: e.copy(out=OT[:], in_=psO[:]), reads=["psO"], writes=["OT"])
                    for dh in range(2):
                        for c in range(8):
                            op("pe", lambda e, c=c, dh=dh: e.matmul(out=psW[:], lhsT=OT[:, c, :], rhs=Wo[:, c, dh * 512:(dh + 1) * 512], start=(c == 0), stop=(c == 7)),
                               reads=["OT", "Wo"], writes=["psWo"], inc=(c == 7))
                        op("dve", lambda e, dh=dh: e.tensor_tensor(out=xt[t][:, dh * 512:(dh + 1) * 512], in0=psW[:], in1=xt[t][:, dh * 512:(dh + 1) * 512], op=ALU.add),
                           reads=["psWo", f"x1_{t}"], writes=[f"x1_{t}"])
                    dma("pool", f"x2st{t}", lambda e: e.dma_start(out=X2_d[i * 128:(i + 1) * 128, :], in_=xt[t][:]), reads=[f"x1_{t}"], writes=[f"X2_d{i}"])

                for grp in range(4):
                    b_pro(grp)
                    for t in range(4):
                        b_s1(grp * 4 + t)
                        if t > 0:
                            b_s2(grp * 4 + t - 1)
                    b_s2(grp * 4 + 3)

        if stage == "B":
            dma("sp", "fin", lambda e: e.dma_start(out=out, in_=X2_d), reads=[f"X2_d{i}" for i in range(NT)], writes=["out"])
            S.finish("sp", ["out"] + ["dbg_" + n for n in dbg_outs])
            return nc, es, dbg_outs


        S.barrier()
        with ExitStack() as sC:
            Wr = sb("Wr", [128, 8, NE], F32, sC)
            dma("sp", "Wr", lambda e: e.dma_start(out=Wr[:], in_=w_r.rearrange("(dc p) f -> p dc f", p=128)), writes=["Wr"])
            brb = sb("brb", [128, NE], F32, sC)
            dma("sp", "brb", lambda e: e.dma_start(out=brb[:], in_=b_r.partition_broadcast(128)), writes=["brb"])
            eb1 = sb("eb1", [128, NE], F32, sC)
            dma("sp", "eb1", lambda e: e.dma_start(out=eb1[:], in_=ebase), writes=["eb1"])
            load_gain(3)
            masks = sb("masks", [128, NT, NE], BF16, sC)
            xt = [sb(f"x2_{i}", [128, D], F32, sC) for i in range(2)]
            hf = [sb(f"h3f_{i}", [128, D], F32, sC) for i in range(2)]
            hb = [sb(f"h3b_{i}", [128, D], BF16, sC) for i in range(4)]
            hT = sb("h3T", [128, 8, 128], F32, sC)
            junk = sb("junkC", [128, D], BF16, sC)
            ss = sb("ssC", [128, 1], F32, sC)
            rs = sb("rsC", [128, 1], F32, sC)
            lg = sb("lg", [128, NE], F32, sC)
            m8 = sb("m8", [128, 8], F32, sC)
            nm = sb("nm", [128, 1], F32, sC)
            mk = sb("mk", [128, NE], F32, sC)
            ex = sb("ex", [128, NE], F32, sC)
            em = sb("em", [128, NE], F32, sC)
            sme = sb("sme", [128, 1], F32, sC)
            wt = sb("wt", [128, NE], F32, sC)
            key = sb("key", [128, NE], F32, sC)
            k8 = sb("k8", [128, 8], F32, sC)
            eq = sb("eq", [128, NE], F32, sC)
            psT = [ps(f"psTr_{i}", [128, 4, 128], F32, sC) for i in range(2)]
            psL = ps("psL", [128, NE], F32, sC)
            psP = ps("psP", [128, NE], F32, sC)
            def c_s0(i):
                k2 = i % 2
                k4 = i % 4
                dma("sp", f"x2l{k2}", lambda e: e.dma_start(out=xt[k2][:], in_=X2_d[i * 128:(i + 1) * 128, :]), reads=[f"X2_d{i}"], writes=[f"x2_{k2}"])
                rmsnorm_tile(xt[k2][:], f"x2_{k2}", 3, hb[k4][:], f"h3b{k4}", ss, rs, junk, hf=hf[k2][:], hfname=f"h3f{k2}")

            def c_s1(i):
                k2 = i % 2
                k4 = i % 4
                for dc in range(8):
                    op("pe", lambda e, dc=dc: e.transpose(out=psT[dc // 4][:, dc % 4, :], in_=hf[k2][:, dc * 128:(dc + 1) * 128], identity=identf[:]),
                       reads=[f"h3f{k2}", "identf"], writes=[f"psTr{dc // 4}"], inc=(dc % 4 == 3))
                op("act", lambda e: e.copy(out=hT[:, 0:4, :], in_=psT[0][:]), reads=["psTr0"], writes=["h3Ta"])
                op("dve", lambda e: e.tensor_copy(out=hT[:, 4:8, :], in_=psT[1][:]), reads=["psTr1"], writes=["h3Tb"])
                for dc in range(8):
                    op("pe", lambda e, dc=dc: e.matmul(out=psL[:], lhsT=hT[:, dc, :], rhs=Wr[:, dc, :], start=(dc == 0), stop=(dc == 7)),
                       reads=["h3Ta", "h3Tb", "Wr"], writes=["psL"], inc=(dc == 7))
                op("dve", lambda e: e.tensor_tensor(out=lg[:], in0=psL[:], in1=brb[:], op=ALU.add), reads=["psL", "brb"], writes=["lg"])
                op("dve", lambda e: e.max(out=m8[:], in_=lg[:]), reads=["lg"], writes=["m8"])
                op("dve", lambda e: e.tensor_scalar(out=mk[:], in0=lg[:], scalar1=m8[:, 3:4], scalar2=None, op0=ALU.is_ge), reads=["lg", "m8"], writes=["mk"])
                op("dve", lambda e, i=i: e.tensor_copy(out=masks[:, i, :], in_=mk[:]), reads=["mk"], writes=[f"masks{i}"])
                op("dve", lambda e: e.tensor_scalar(out=nm[:], in0=m8[:, 0:1], scalar1=-1.0, scalar2=None, op0=ALU.mult), reads=["m8"], writes=["nm"])
                op("act", lambda e: e.activation(out=ex[:], in_=lg[:], func=AF.Exp, bias=nm[:, 0:1], scale=1.0), reads=["lg", "nm"], writes=["ex"])
                op("dve", lambda e: e.tensor_tensor(out=em[:], in0=ex[:], in1=mk[:], op=ALU.mult), reads=["ex", "mk"], writes=["em"])
                op("dve", lambda e: e.reduce_sum(out=sme[:], in_=em[:], axis=AX.X), reads=["em"], writes=["sme"])
                op("dve", lambda e: e.reciprocal(out=sme[:], in_=sme[:]), reads=["sme"], writes=["sme"])
                op("dve", lambda e: e.tensor_scalar(out=wt[:], in0=em[:], scalar1=sme[:, 0:1], scalar2=None, op0=ALU.mult), reads=["em", "sme"], writes=["wt"])
                op("pe", lambda e, i=i: e.matmul(out=psP[:], lhsT=ltri[:], rhs=masks[:, i, :], start=True, stop=(i == 0)),
                   reads=["ltri", f"masks{i}"], writes=["psP"], inc=(i == 0))
                for j in range(i):
                    op("pe", lambda e, j=j, i=i: e.matmul(out=psP[:], lhsT=onesb[:], rhs=masks[:, j, :], start=False, stop=(j == i - 1)),
                       reads=["onesb", f"masks{j}"], writes=["psP"], inc=(j == i - 1))
                op("dve", lambda e: e.tensor_tensor(out=key[:], in0=psP[:], in1=eb1[:], op=ALU.add), reads=["psP", "eb1"], writes=["key"])
                op("dve", lambda e: e.tensor_tensor(out=key[:], in0=key[:], in1=mk[:], op=ALU.mult), reads=["key", "mk"], writes=["key"])
                op("dve", lambda e: e.max(out=k8[:], in_=key[:]), reads=["key"], writes=["k8"])
                op("dve", lambda e, i=i: e.tensor_scalar(out=IDX[:, i, :], in0=k8[:, 0:4], scalar1=-1.0, scalar2=None, op0=ALU.add), reads=["k8"], writes=[f"IDX{i}"])
                for k in range(4):
                    op("dve", lambda e, k=k: e.tensor_scalar(out=eq[:], in0=key[:], scalar1=k8[:, k:k + 1], scalar2=None, op0=ALU.is_equal), reads=["key", "k8"], writes=["eq"])
                    op("dve", lambda e: e.tensor_tensor(out=eq[:], in0=eq[:], in1=wt[:], op=ALU.mult), reads=["eq", "wt"], writes=["eq"])
                    op("dve", lambda e, k=k, i=i: e.reduce_sum(out=WK[:, i, k:k + 1], in_=eq[:], axis=AX.X), reads=["eq"], writes=[f"WK{i}"])
                for k in range(4):
                    S._deps("pool", ["Xd"], [])
                    dma("pool", f"disp{k4}_{k}", lambda e, k=k, i=i: e.indirect_dma_start(out=Xd, out_offset=bass.IndirectOffsetOnAxis(ap=IDX[:, i, k:k + 1], axis=0),
                                                                                      in_=hb[k4][:, :], in_offset=None, bounds_check=bc_reg, oob_is_err=False),
                        reads=[f"h3b{k4}", f"IDX{i}"], writes=[f"Xd_disp{k4}_{k}"])

            c_s0(0)
            for i in range(NT):
                if i + 1 < NT:
                    c_s0(i + 1)
                c_s1(i)
        S.barrier()
        with ExitStack() as sD:
            Xe = [sb(f"Xe_{i}", [128, 3, D], BF16, sD) for i in range(2)]
            XTe = [sb(f"XTe_{i}", [128, 8, CAP], BF16, sD) for i in range(2)]
            actT = [sb(f"actT_{i}", [128, 8, CAP], BF16, sD) for i in range(2)]
            bdb = [sb(f"bdb_{i}", [128, D], F32, sD) for i in range(2)]
            Oe = [sb(f"Oe_{i}", [128, 3, D], F32, sD) for i in range(2)]
            g_ = [sb(f"g_{i}", [128, CAP], F32, sD) for i in range(2)]
            sg_ = [sb(f"sg_{i}", [128, CAP], F32, sD) for i in range(2)]
            u_ = [sb(f"u_{i}", [128, CAP], F32, sD) for i in range(2)]
            psXT = [ps(f"psXT_{i}", [128, 3, 128], BF16, sD) for i in range(2)]
            psG = [ps(f"psG_{i}", [128, 512], F32, sD) for i in range(2)]
            psUp = [ps(f"psUp_{i}", [128, 512], F32, sD) for i in range(2)]
            psD = [ps(f"psD_{i}", [128, 512], F32, sD) for i in range(2)]
            def ex_load(e_):
                k = e_ % 2
                dma("sp", f"Xe{k}", lambda e: e.dma_start(out=Xe[k][:], in_=Xd[e_ * CAP:(e_ + 1) * CAP, :].rearrange("(b p) d -> p b d", p=128)),
                    reads=["Xd"] + [f"Xd_disp{a_}_{b_}" for a_ in range(4) for b_ in range(4)], writes=[f"Xe{k}"])
                dma("sp", f"bdb{k}", lambda e: e.dma_start(out=bdb[k][:], in_=b_dn[e_:e_ + 1, :].partition_broadcast(128)), writes=[f"bdb{k}"])

            def ex_tr(e_):
                k = e_ % 2
                for dc in range(8):
                    x2 = dc % 2
                    for b in range(3):
                        op("pe", lambda e, dc=dc, b=b: e.transpose(out=psXT[x2][:, b, :], in_=Xe[k][:, b, dc * 128:(dc + 1) * 128], identity=identb[:]),
                           reads=[f"Xe{k}", "identb"], writes=[f"psXT{x2}"], inc=(b == 2))
                    if dc % 2 == 0:
                        op("act", lambda e, dc=dc: e.copy(out=XTe[k][:, dc, :], in_=psXT[x2][:]), reads=[f"psXT{x2}"], writes=[f"XTe{k}"])
                    else:
                        op("dve", lambda e, dc=dc: e.tensor_copy(out=XTe[k][:, dc, :], in_=psXT[x2][:]), reads=[f"psXT{x2}"], writes=[f"XTe{k}"])

            def ex_gu(e_):
                k = e_ % 2
                for j in range(8):
                    j2 = j % 2
                    for dc in range(8):
                        op("pe", lambda e, j=j, dc=dc: e.matmul(out=psG[j2][:, 0:CAP], lhsT=Wgu[k][:, dc, j * 128:(j + 1) * 128], rhs=XTe[k][:, dc, :], start=(dc == 0), stop=(dc == 7)),
                           reads=[f"Wgu{k}", f"XTe{k}"], writes=[f"psG{j2}"], inc=(dc == 7))
                    for dc in range(8):
                        op("pe", lambda e, j=j, dc=dc: e.matmul(out=psUp[j2][:, 0:CAP], lhsT=Wgu[k][:, dc, 1024 + j * 128:1024 + (j + 1) * 128], rhs=XTe[k][:, dc, :],
                                                               start=(dc == 0), stop=(dc == 7)),
                           reads=[f"Wgu{k}", f"XTe{k}"], writes=[f"psUp{j2}"], inc=(dc == 7))
                    op("dve", lambda e, j=j: e.tensor_scalar(out=g_[j2][:], in0=psG[j2][:, 0:CAP], scalar1=bgu[:, e_, j:j + 1], scalar2=7.0, op0=ALU.add, op1=ALU.min),
                       reads=[f"psG{j2}", "bgu"], writes=[f"g{j2}"])
                    op("act", lambda e: e.activation(out=sg_[j2][:], in_=g_[j2][:], func=AF.Silu, scale=1.702), reads=[f"g{j2}"], writes=[f"sg{j2}"])
                    op("act", lambda e, j=j: e.activation(out=u_[j2][:], in_=psUp[j2][:, 0:CAP], func=AF.Identity, bias=bgu[:, e_, 8 + j:9 + j], scale=1.0),
                       reads=[f"psUp{j2}", "bgu"], writes=[f"u{j2}"])
                    op("dve", lambda e: e.tensor_scalar(out=u_[j2][:], in0=u_[j2][:], scalar1=7.0, scalar2=-7.0, op0=ALU.min, op1=ALU.max), reads=[f"u{j2}"], writes=[f"u{j2}"])
                    op("dve", lambda e, j=j: e.scalar_tensor_tensor(out=actT[k][:, j, :], in0=u_[j2][:], scalar=1.0, in1=sg_[j2][:], op0=ALU.add, op1=ALU.mult),
                       reads=[f"sg{j2}", f"u{j2}"], writes=[f"actT{k}"])

            def ex_dn(e_):
                k = e_ % 2
                for b in range(3):
                    for dh in range(2):
                        d2 = (b * 2 + dh) % 2
                        for j in range(8):
                            op("pe", lambda e, b=b, dh=dh, j=j: e.matmul(out=psD[d2][:], lhsT=actT[k][:, j, b * 128:(b + 1) * 128], rhs=Wdn[k][:, j, dh * 512:(dh + 1) * 512],
                                                                       start=(j == 0), stop=(j == 7)),
                               reads=[f"actT{k}", f"Wdn{k}"], writes=[f"psD{d2}"], inc=(j == 7))
                        op("dve", lambda e, b=b, dh=dh: e.scalar_tensor_tensor(out=Oe[k][:, b, dh * 512:(dh + 1) * 512], in0=psD[d2][:], scalar=1.0 / 1.702,
                                                                                in1=bdb[k][:, dh * 512:(dh + 1) * 512], op0=ALU.mult, op1=ALU.add),
                           reads=[f"psD{d2}", f"bdb{k}"], writes=[f"Oe{k}"])
                dma("sp", f"Oest{k}", lambda e: e.dma_start(out=O_d[e_ * CAP:(e_ + 1) * CAP, :].rearrange("(b p) d -> p b d", p=128), in_=Oe[k][:]),
                    reads=[f"Oe{k}"], writes=["O_d"])

            ex_load(0)
            ex_tr(0)
            for e_ in range(NE):
                if e_ + 1 < NE:
                    ex_load(e_ + 1)
                ex_gu(e_)
                if e_ + 1 < NE:
                    ex_tr(e_ + 1)
                ex_dn(e_)
                if e_ + 2 < NE:
                    load_expert(e_ + 2)
        S.barrier()
        with ExitStack() as sE:
            xt = [sb(f"x2c_{i}", [128, D], F32, sE) for i in range(2)]
            G = [sb(f"G_{i}", [128, D], F32, sE) for i in range(4)]
            ob = [sb(f"ob_{i}", [128, D], F32, sE) for i in range(2)]
            load_gain(4)
            junk = sb("junkE", [128, D], BF16, sE)
            ss = sb("ssE", [128, 1], F32, sE)
            rs = sb("rsE", [128, 1], F32, sE)
            G8 = G + [sb(f"G_{i}", [128, D], F32, sE) for i in range(4, 8)]

            def e_s0(i):
                k2 = i % 2
                dma("sp", f"x2c{k2}", lambda e: e.dma_start(out=xt[k2][:], in_=X2_d[i * 128:(i + 1) * 128, :]), reads=[f"X2_d{i}"], writes=[f"x2c{k2}"])
                for k in range(4):
                    gi = k2 * 4 + k
                    dma("pool", f"G{gi}", lambda e, k=k, gi=gi: e.indirect_dma_start(out=G8[gi][:, :], out_offset=None, in_=O_d,
                                                                                  in_offset=bass.IndirectOffsetOnAxis(ap=IDX[:, i, k:k + 1], axis=0),
                                                                                  bounds_check=bc_reg, oob_is_err=False),
                        reads=["O_d", f"IDX{i}"], writes=[f"G{gi}"])

            def e_s1(i):
                k2 = i % 2
                for k in range(4):
                    gi = k2 * 4 + k
                    op("dve", lambda e, k=k, gi=gi: e.scalar_tensor_tensor(out=xt[k2][:], in0=G8[gi][:], scalar=WK[:, i, k:k + 1], in1=xt[k2][:], op0=ALU.mult, op1=ALU.add),
                       reads=[f"G{gi}", f"WK{i}", f"x2c{k2}"], writes=[f"x2c{k2}"])
                op("act", lambda e: e.activation(out=junk[:], in_=xt[k2][:], func=AF.Square, accum_out=ss[:, 0:1]), reads=[f"x2c{k2}"], writes=["junkE", "ssE"])
                op("act", lambda e: e.activation(out=rs[:, 0:1], in_=ss[:, 0:1], func=AF.Sqrt, bias=epsb[:, 0:1], scale=1.0 / D), reads=["ssE", "epsb"], writes=["rsE"])
                op("dve", lambda e: e.reciprocal(out=rs[:, 0:1], in_=rs[:, 0:1]), reads=["rsE"], writes=["rsE"])
                op("dve", lambda e: e.scalar_tensor_tensor(out=ob[k2][:], in0=xt[k2][:], scalar=rs[:, 0:1], in1=gv[:, GSLOT[4], :], op0=ALU.mult, op1=ALU.mult),
                   reads=[f"x2c{k2}", "rsE", f"gv{GSLOT[4]}"], writes=[f"ob{k2}"])
                dma("sp", f"ost{k2}", lambda e: e.dma_start(out=out[i * 128:(i + 1) * 128, :], in_=ob[k2][:]), reads=[f"ob{k2}"], writes=[f"out{i}"])

            e_s0(0)
            for i in range(NT):
                if i + 1 < NT:
                    e_s0(i + 1)
                e_s1(i)
    S.finish("sp", [f"out{i}" for i in range(NT)] + ["dbg_" + n for n in dbg_outs])
    return nc, es, dbg_outs


def host_inputs(inputs, stage="full", cores=range(8)):
    f = np.float32
    x = np.asarray(inputs["x"], f)
    mem = np.asarray(inputs["mem"], f)
    gvec = np.stack([inputs["norm_mix"][0], inputs["norm_xattn"][0], inputs["norm_mem"][0], inputs["norm_ffn"][0], inputs["norm_final"]]).astype(f)
    cwv = np.asarray(inputs["conv_w"][0], f)
    cw = np.ascontiguousarray(cwv.reshape(3, 4, 128).transpose(2, 1, 0))
    gc = np.asarray(inputs["g_conv_out"][0], f).reshape(4, 128).T
    gf = np.asarray(inputs["g_fft_out"][0], f).reshape(4, 128).T
    gcf = np.ascontiguousarray(np.concatenate([gc, gf], axis=1))
    a = np.arange(128)
    f1c = np.cos(2 * np.pi * np.outer(a, a) / 128).astype(f)
    f1s = (-np.sin(2 * np.pi * np.outer(a, a) / 128)).astype(f)
    c64 = np.arange(64)
    C64 = np.cos(2 * np.pi * np.outer(c64, c64) / 64)
    S64 = np.sin(2 * np.pi * np.outer(c64, c64) / 64)
    bdc = np.zeros((128, 128)); bds = np.zeros((128, 128))
    for g in range(2):
        bdc[g * 64:(g + 1) * 64, g * 64:(g + 1) * 64] = C64
        bds[g * 64:(g + 1) * 64, g * 64:(g + 1) * 64] = S64
    ebase = np.broadcast_to((np.arange(NE) * CAP + 1).astype(f), (128, NE)).copy()
    common = {
        "gvec": gvec, "w_in": np.asarray(inputs["w_in"][0], f), "cw": cw, "gcf": gcf,
        "w_out": np.asarray(inputs["w_out"][0], f), "w_q": np.asarray(inputs["w_q"][0], f), "w_k": np.asarray(inputs["w_k"][0], f),
        "w_v": np.asarray(inputs["w_v"][0], f), "w_o": np.asarray(inputs["w_o"][0], f), "w_r": np.asarray(inputs["w_router"][0], f),
        "b_r": np.asarray(inputs["b_router"], f).reshape(1, NE), "f1c": f1c, "f1s": f1s, "bdc": bdc.astype(f), "bds": bds.astype(f), "ebase": ebase,
    }
    if stage == "full":
        common["w_gu"] = np.asarray(inputs["w_gate_up"][0], f)
        common["b_gu"] = np.ascontiguousarray(np.asarray(inputs["b_gate_up"][0], f).reshape(NE, 16, 128).transpose(2, 0, 1))
        common["w_dn"] = np.asarray(inputs["w_down"][0], f)
        common["b_dn"] = np.asarray(inputs["b_down"][0], f)
    maps = []
    s2 = np.arange(64)
    k1 = np.arange(128)
    for c in cores:
        b, q = c // 4, c % 4
        xr = np.roll(x[b], -TOK * q, axis=0)
        xh = np.zeros((128, D), f)
        if q > 0:
            xh[0] = x[b, TOK * q - 1]
        if q < 3:
            xh[1] = x[b, TOK * (q + 1)]
        k2 = 16 * q + np.arange(16)
        kk = k1[:, None] + 128 * k2[None, :]
        phi = 2 * np.pi * s2[:, None, None] * kk[None] / 8192.0 + np.pi * kk[None] * q / 2.0
        gcos, gsin = np.cos(phi), np.sin(phi)
        t = np.zeros((2, 64, 128, 32))
        t[0, :, :, 0:16] = gcos; t[1, :, :, 0:16] = gsin
        t[0, :, :, 16:32] = -gsin; t[1, :, :, 16:32] = gcos
        m = dict(common)
        m.update({"xrot": np.ascontiguousarray(xr), "xhalo": xh, "memb": np.ascontiguousarray(mem[b]), "f3": t.reshape(128, 128, 32).astype(f)})
        maps.append(m)
    return maps


def kernel(**inputs):
    nc, es, _ = build("full")
    maps = host_inputs(inputs)
    res = run_bass_kernel_spmd(nc, maps, core_ids=list(range(8)))
    outs = [np.asarray(r["out"], np.float32) for r in res.results]
    y = np.stack(outs).reshape(2, 4 * TOK, D)
    return y
```

```python
import numpy as np
from contextlib import ExitStack
import concourse.bass as bass
import concourse.mybir as mybir
from concourse.bass_utils import run_bass_kernel_spmd

F32 = mybir.dt.float32
BF16 = mybir.dt.bfloat16
I32 = mybir.dt.int32
ALU = mybir.AluOpType
AF = mybir.ActivationFunctionType
AX = mybir.AxisListType

D = 1024
SEQ = 8192
TOK = 2048
NT = TOK // 128
NE = 32
CAP = 384
NSLOT = NE * CAP
EPS = 1e-5


class Sched:
    def __init__(self, nc, es):
        self.nc = nc
        self.es = es
        self.eng = {"pe": nc.tensor, "act": nc.scalar, "dve": nc.vector, "pool": nc.gpsimd, "sp": nc.sync}
        self.sem = {k: es.enter_context(nc.semaphore("c_" + k)) for k in self.eng}
        self.cnt = {k: 0 for k in self.eng}
        self.waited = {k: {} for k in self.eng}
        self.dsem = {}
        self.dcnt = {}
        self.res = {}
        self.nobar = set()
        self.semname = {}

    def _wait(self, e, ev):
        if ev is None:
            return
        s, v, owner = ev
        if owner == "pe" and e == "pe":
            return
        w = self.waited[e]
        if w.get(id(s), 0) >= v:
            return
        self.eng[e].wait_ge(s, v)
        w[id(s)] = v

    def _deps(self, e, reads, writes):
        for r in reads:
            st = self.res.get(r)
            if st:
                self._wait(e, st[0])
        for wname in writes:
            st = self.res.get(wname)
            if st:
                self._wait(e, st[0])
                for ev in list(st[1].values()):
                    self._wait(e, ev)

    def _record(self, ev, reads, writes):
        for r in reads:
            st = self.res.setdefault(r, [None, {}])
            old = st[1].get(id(ev[0]))
            if old is None or old[1] < ev[1]:
                st[1][id(ev[0])] = ev
        for wname in writes:
            self.res[wname] = [ev, {}]

    def op(self, e, fn, reads=(), writes=(), inc=True):
        self._deps(e, reads, writes)
        ins = fn(self.eng[e])
        if inc:
            self.cnt[e] += 1
            ins.then_inc(self.sem[e], 1)
            ev = (self.sem[e], self.cnt[e], e)
        else:
            ev = (self.sem[e], self.cnt[e] + 1, e)
        self._record(ev, reads, writes)
        return ins

    def dma(self, q, key, fn, reads=(), writes=()):
        self._deps(q, reads, writes)
        if key not in self.dsem:
            self.dsem[key] = self.es.enter_context(self.nc.semaphore("d_" + key))
            self.dcnt[key] = 0
        ins = fn(self.eng[q])
        self.dcnt[key] += 16
        ins.then_inc(self.dsem[key], 16)
        ev = (self.dsem[key], self.dcnt[key], "dma")
        self._record(ev, reads, writes)
        return ins

    def barrier(self):
        evs = [(self.sem[o], self.cnt[o], o) for o in self.eng if self.cnt[o] > 0]
        evs += [(self.dsem[k], self.dcnt[k], "dma") for k in self.dsem if k not in self.nobar]
        for e in self.eng:
            for ev in evs:
                if ev[2] == e and e != "pe":
                    continue
                if ev[2] == "pe" and e == "pe":
                    continue
                self._wait(e, ev)
        self.res = {k: v for k, v in self.res.items()}

    def finish(self, q, names):
        for n in names:
            st = self.res.get(n)
            if st:
                self._wait(q, st[0])
                for ev in list(st[1].values()):
                    self._wait(q, ev)


def build(stage="full", dbg=False):
    nc = bass.Bass("TRN2", target_bir_lowering=False)
    es = ExitStack()

    def din(name, shape, dt=F32):
        return nc.dram_tensor(name, list(shape), dt, kind="ExternalInput").ap()

    def dscr(name, shape, dt):
        return nc.dram_tensor(name, list(shape), dt, kind="Internal").ap()

    xrot = din("xrot", [SEQ, D])
    xhalo = din("xhalo", [128, D])
    memb = din("memb", [256, D])
    gvec = din("gvec", [5, D])
    w_in = din("w_in", [D, 2048])
    cw = din("cw", [128, 4, 3])
    gcf = din("gcf", [128, 8])
    w_out = din("w_out", [D, D])
    w_q = din("w_q", [D, D]); w_k = din("w_k", [D, D]); w_v = din("w_v", [D, D]); w_o = din("w_o", [D, D])
    w_r = din("w_r", [D, NE])
    b_r = din("b_r", [1, NE])
    f1c = din("f1c", [128, 128]); f1s = din("f1s", [128, 128])
    f3 = din("f3", [128, 128, 32])
    bdc = din("bdc", [128, 128]); bds = din("bds", [128, 128])
    ebase = din("ebase", [128, NE])
    full = stage == "full"
    if full:
        w_gu = din("w_gu", [NE, D, 2048])
        b_gu = din("b_gu", [128, NE, 16])
        w_dn = din("w_dn", [NE, D, D])
        b_dn = din("b_dn", [NE, D])
    out = nc.dram_tensor("out", [TOK, D], F32, kind="ExternalOutput").ap()

    U_d = dscr("U_d", [SEQ, 512], BF16)
    A_d = dscr("A_d", [2, 64, 128, 512], BF16)
    X1_d = dscr("X1_d", [TOK, D], F32)
    X2_d = dscr("X2_d", [TOK, D], F32)
    Xd = dscr("Xd", [NSLOT, D], BF16)
    O_d = dscr("O_d", [NSLOT, D], F32)

    S = Sched(nc, es)
    op, dma = S.op, S.dma

    def sb(name, shape, dt, stack):
        return stack.enter_context(nc.sbuf_tensor(name, list(shape), dt))

    def ps(name, shape, dt, stack):
        return stack.enter_context(nc.psum_tensor(name, list(shape), dt))

    identf = sb("identf", [128, 128], F32, es)
    identb = sb("identb", [128, 128], BF16, es)
    onesb = sb("onesb", [128, 128], BF16, es)
    ltri = sb("ltri", [128, 128], BF16, es)
    ltrif = sb("ltrif", [128, 128], F32, es)
    epsb = sb("epsb", [128, 1], F32, es)
    gv = sb("gv", [128, 2, D], F32, es)
    GSLOT = {0: 0, 2: 0, 1: 1, 3: 0, 4: 1}

    def load_gain(g):
        sl = GSLOT[g]
        dma("sp", f"gv{sl}", lambda e: e.dma_start(out=gv[:, sl, :], in_=gvec[g:g + 1, :].partition_broadcast(128)), writes=[f"gv{sl}"])
    op("pool", lambda e: e.memset(identf[:], 0.0), writes=["identf"])
    op("pool", lambda e: e.affine_select(out=identf[:], in_=identf[:], pattern=[[-1, 128]], compare_op=ALU.not_equal,
                                          fill=1.0, base=0, channel_multiplier=1), reads=["identf"], writes=["identf"])
    op("dve", lambda e: e.tensor_copy(out=identb[:], in_=identf[:]), reads=["identf"], writes=["identb"])
    op("dve", lambda e: e.memset(onesb[:], 1.0), writes=["onesb"])
    op("dve", lambda e: e.memset(epsb[:], EPS), writes=["epsb"])
    op("pool", lambda e: e.memset(ltrif[:], 1.0), writes=["ltrif"])
    op("pool", lambda e: e.affine_select(out=ltrif[:], in_=ltrif[:], pattern=[[1, 128]], compare_op=ALU.is_gt,
                                          fill=0.0, base=0, channel_multiplier=-1), reads=["ltrif"], writes=["ltrif"])
    op("dve", lambda e: e.tensor_copy(out=ltri[:], in_=ltrif[:]), reads=["ltrif"], writes=["ltri"])
    load_gain(0)

    zt = sb("zt", [128, 2, D], BF16, es)
    op("pool", lambda e: e.memset(zt[:], 0.0), writes=["zt"])
    ZB = NSLOT // 256 // 4

    def zfill_burst(bi):
        if not full:
            return
        for r in range(bi * ZB, (bi + 1) * ZB):
            dma("pool", "zfill", lambda e, r=r: e.dma_start(out=Xd[r * 256:(r + 1) * 256, :].rearrange("(p n) d -> p n d", n=2), in_=zt[:]),
                reads=["zt"], writes=[])
        if bi == 3:
            S.res["Xd"] = [(S.dsem["zfill"], S.dcnt["zfill"], "dma"), {}]

    dbg_outs = {}

    def dump(name, shape, dt, src_ap, reads):
        if not dbg:
            return
        t = nc.dram_tensor("dbg_" + name, list(shape), dt, kind="ExternalOutput").ap()
        dbg_outs[name] = t
        dma("sp", "dbg_" + name, lambda e: e.dma_start(out=t, in_=src_ap), reads=reads, writes=["dbg_" + name])

    def rmsnorm_tile(xt, xname, gi, hb, hname, ss, rs, junk, hf=None, hfname=None):
        op("act", lambda e: e.activation(out=junk[:], in_=xt, func=AF.Square, accum_out=ss[:, 0:1]),
           reads=[xname], writes=["junk", "ss"])
        if gi in (1, 3):
            op("act", lambda e: e.activation(out=rs[:, 0:1], in_=ss[:, 0:1], func=AF.Ln, bias=epsb[:, 0:1], scale=1.0 / D),
               reads=["ss", "epsb"], writes=["rs"])
            op("act", lambda e: e.activation(out=rs[:, 0:1], in_=rs[:, 0:1], func=AF.Exp, scale=-0.5), reads=["rs"], writes=["rs"])
        else:
            op("act", lambda e: e.activation(out=rs[:, 0:1], in_=ss[:, 0:1], func=AF.Sqrt, bias=epsb[:, 0:1], scale=1.0 / D),
               reads=["ss", "epsb"], writes=["rs"])
            op("dve", lambda e: e.reciprocal(out=rs[:, 0:1], in_=rs[:, 0:1]), reads=["rs"], writes=["rs"])
        if hf is not None:
            op("dve", lambda e: e.scalar_tensor_tensor(out=hf, in0=xt, scalar=rs[:, 0:1], in1=gv[:, GSLOT[gi], :], op0=ALU.mult, op1=ALU.mult),
               reads=[xname, "rs", f"gv{GSLOT[gi]}"], writes=[hfname])
            op("act", lambda e: e.copy(out=hb, in_=hf), reads=[hfname], writes=[hname])
        else:
            op("dve", lambda e: e.scalar_tensor_tensor(out=hb, in0=xt, scalar=rs[:, 0:1], in1=gv[:, GSLOT[gi], :], op0=ALU.mult, op1=ALU.mult),
               reads=[xname, "rs", f"gv{GSLOT[gi]}"], writes=[hname])

    def load_w(name, src2d, dst, ncols, q="pool"):
        dma(q, name, lambda e: e.dma_start(out=dst, in_=src2d.rearrange("(dc p) f -> p dc f", p=128)), writes=[name])

    with ExitStack() as sA:
        ynT = sb("ynT", [128, 8, TOK], BF16, sA)
        gcf_sb = sb("gcf_sb", [128, 8], F32, sA)
        dma("sp", "gcf", lambda e: e.dma_start(out=gcf_sb[:], in_=gcf), writes=["gcf"])
        S.barrier()
        with ExitStack() as sA0:
            BT = sb("BT", [128, 4, TOK], F32, sA0)
            CVx = sb("CVx", [128, 4, TOK + 2], F32, sA0)
            S.barrier()
            with ExitStack() as s1:
                Win = sb("Win", [128, 8, 2048], BF16, s1)
                load_w("Win", w_in, Win[:], 2048)
                hT4 = [sb(f"hT4_{i}", [128, 8, 512], BF16, s1) for i in range(2)]
                xb = [sb(f"xb_{i}", [128, D], F32, s1) for i in range(3)]
                hb = [sb(f"hb_{i}", [128, D], BF16, s1) for i in range(2)]
                ub = [sb(f"ub_{i}", [128, 512], BF16, s1) for i in range(2)]
                junk = sb("junk", [128, D], BF16, s1)
                ss = sb("ss", [128, 1], F32, s1)
                rs = sb("rs", [128, 1], F32, s1)
                Ctmp = sb("Ctmp", [128, 4, 512], F32, s1)
                hTh = sb("hTh", [128, 8, 128], BF16, s1)
                Chs = sb("Chs", [128, 8], F32, s1)
                CVh = sb("CVh", [128, 8], F32, s1)
                psT = [ps(f"psT_{i}", [128, 8, 128], BF16, s1) for i in range(2)]
                psU = [ps(f"psU_{i}", [128, 512], F32, s1) for i in range(2)]
                psZ = [ps(f"psZ_{i}", [128, 512], F32, s1) for i in range(3)]
                psH = ps("psH", [128, 16], F32, s1)

                def norm_and_transpose(src_ap, i, dstT, dstname, dst_cols):
                    k3, k2 = i % 3, i % 2
                    dma("sp", f"xb{k3}", lambda e: e.dma_start(out=xb[k3][:], in_=src_ap), writes=[f"xb{k3}"])
                    rmsnorm_tile(xb[k3][:], f"xb{k3}", 0, hb[k2][:], f"hb{k2}", ss, rs, junk)
                    for dc in range(8):
                        op("pe", lambda e, dc=dc: e.transpose(out=psT[k2][:, dc, :], in_=hb[k2][:, dc * 128:(dc + 1) * 128], identity=identb[:]),
                           reads=[f"hb{k2}", "identb"], writes=[f"psT{k2}"], inc=(dc == 7))
                    op("act", lambda e: e.copy(out=dstT[:, :, dst_cols], in_=psT[k2][:]), reads=[f"psT{k2}"], writes=[dstname])

                norm_and_transpose(xhalo, 0, hTh, "hTh", slice(0, 128))
                for j in range(8):
                    for dc in range(8):
                        op("pe", lambda e, j=j, dc=dc: e.matmul(out=psH[:, 2 * j:2 * j + 2], lhsT=Win[:, dc, 512 + j * 128:512 + (j + 1) * 128],
                                                               rhs=hTh[:, dc, 0:2], start=(dc == 0), stop=(dc == 7)),
                           reads=["Win", "hTh"], writes=["psH"], inc=(dc == 7))
                op("act", lambda e: e.copy(out=Chs[:], in_=psH[:, 0:8]), reads=["psH"], writes=["Chs"])
                op("dve", lambda e: e.tensor_tensor(out=CVh[:], in0=psH[:, 8:16], in1=Chs[:], op=ALU.mult), reads=["psH", "Chs"], writes=["CVh"])
                CVh3 = CVh[:].rearrange("p (c t) -> p c t", t=2)
                op("dve", lambda e: e.tensor_copy(out=CVx[:, :, 0:1], in_=CVh3[:, :, 0:1]), reads=["CVh"], writes=["CVxh0"])
                op("dve", lambda e: e.tensor_copy(out=CVx[:, :, TOK + 1:TOK + 2], in_=CVh3[:, :, 1:2]), reads=["CVh"], writes=["CVxh1"])

                def a1_s0(i):
                    k3, k2 = (i + 1) % 3, (i + 1) % 2
                    dma("sp", f"xb{k3}", lambda e: e.dma_start(out=xb[k3][:], in_=xrot[i * 128:(i + 1) * 128, :]), writes=[f"xb{k3}"])
                    rmsnorm_tile(xb[k3][:], f"xb{k3}", 0, hb[k2][:], f"hb{k2}", ss, rs, junk)

                def a1_s1(i):
                    k2 = (i + 1) % 2
                    grp, t = i // 4, i % 4
                    g2 = grp % 2
                    for dc in range(8):
                        op("pe", lambda e, dc=dc: e.transpose(out=psT[k2][:, dc, :], in_=hb[k2][:, dc * 128:(dc + 1) * 128], identity=identb[:]),
                           reads=[f"hb{k2}", "identb"], writes=[f"psT{k2}"], inc=(dc == 7))
                    op("act", lambda e: e.copy(out=hT4[g2][:, :, t * 128:(t + 1) * 128], in_=psT[k2][:]), reads=[f"psT{k2}"], writes=[f"hT4_{g2}"])

                def a1_s2(i):
                    grp, t = i // 4, i % 4
                    g2 = grp % 2
                    u2 = i % 2
                    for dc in range(8):
                        op("pe", lambda e, dc=dc: e.matmul(out=psU[u2][:], lhsT=hT4[g2][:, dc, t * 128:(t + 1) * 128], rhs=Win[:, dc, 1536:2048],
                                                          start=(dc == 0), stop=(dc == 7)),
                           reads=["Win", f"hT4_{g2}"], writes=[f"psU{u2}"], inc=(dc == 7))
                    op("dve", lambda e: e.tensor_copy(out=ub[u2][:], in_=psU[u2][:]), reads=[f"psU{u2}"], writes=[f"ub{u2}"])
                    dma("pool", f"ubst{u2}", lambda e: e.dma_start(out=U_d[i * 128:(i + 1) * 128, :], in_=ub[u2][:]), reads=[f"ub{u2}"], writes=[f"U_d{i}"])
                    if t == 3 and grp < 4:
                        for j in range(12):
                            z3 = j % 3
                            for dc in range(8):
                                op("pe", lambda e, j=j, dc=dc: e.matmul(out=psZ[z3][:], lhsT=Win[:, dc, j * 128:(j + 1) * 128], rhs=hT4[g2][:, dc, :],
                                                                       start=(dc == 0), stop=(dc == 7)),
                                   reads=["Win", f"hT4_{g2}"], writes=[f"psZ{z3}"], inc=(dc == 7))
                            cols = slice(grp * 512, (grp + 1) * 512)
                            if j < 4:
                                op("act", lambda e, j=j: e.copy(out=BT[:, j, cols], in_=psZ[z3][:]), reads=[f"psZ{z3}"], writes=[f"BT{j}"])
                            elif j < 8:
                                op("act", lambda e, j=j: e.copy(out=Ctmp[:, j - 4, :], in_=psZ[z3][:]), reads=[f"psZ{z3}"], writes=[f"Ctmp{j - 4}"])
                            else:
                                op("dve", lambda e, j=j: e.tensor_tensor(out=CVx[:, j - 8, 1 + grp * 512:1 + (grp + 1) * 512], in0=psZ[z3][:], in1=Ctmp[:, j - 8, :], op=ALU.mult),
                                   reads=[f"psZ{z3}", f"Ctmp{j - 8}"], writes=[f"CVx{j - 8}"])

                for step in range(64 + 2):
                    if step < 64:
                        a1_s0(step)
                    if 0 <= step - 1 < 64:
                        a1_s1(step - 1)
                    if 0 <= step - 2 < 64:
                        a1_s2(step - 2)
            S.barrier()
            with ExitStack() as s2:
                cw_sb = sb("cw_sb", [128, 4, 3], F32, s2)
                dma("sp", "cw", lambda e: e.dma_start(out=cw_sb[:], in_=cw), writes=["cw"])
                zfill_burst(0)
                T1 = sb("T1", [128, TOK], F32, s2)
                sq = [sb(f"sq_{i}", [128, 512], BF16, s2) for i in range(2)]
                rstd = sb("rstd", [128, TOK], F32, s2)
                psN = [ps(f"psN_{i}", [128, 512], F32, s2) for i in range(2)]
                for c in range(4):
                    rd = [f"CVx{c}", "CVxh0", "CVxh1", "cw"]
                    op("dve", lambda e, c=c: e.tensor_scalar(out=T1[:], in0=CVx[:, c, 0:TOK], scalar1=cw_sb[:, c, 0:1], scalar2=None, op0=ALU.mult),
                       reads=rd, writes=["T1"])
                    op("dve", lambda e, c=c: e.scalar_tensor_tensor(out=T1[:], in0=CVx[:, c, 1:TOK + 1], scalar=cw_sb[:, c, 1:2], in1=T1[:], op0=ALU.mult, op1=ALU.add),
                       reads=rd + ["T1"], writes=["T1"])
                    op("dve", lambda e, c=c: e.scalar_tensor_tensor(out=T1[:], in0=CVx[:, c, 2:TOK + 2], scalar=cw_sb[:, c, 2:3], in1=T1[:], op0=ALU.mult, op1=ALU.add),
                       reads=rd + ["T1"], writes=["T1"])
                    op("dve", lambda e, c=c: e.tensor_tensor(out=BT[:, c, :], in0=T1[:], in1=BT[:, c, :], op=ALU.mult), reads=["T1", f"BT{c}"], writes=[f"BT{c}"])

                def branch_norm(Y, ynames, goff):
                    for tb in range(4):
                        cols = slice(tb * 512, (tb + 1) * 512)
                        n2 = tb % 2
                        for c in range(4):
                            s2i = (tb * 4 + c) % 2
                            op("act", lambda e, c=c: e.activation(out=sq[s2i][:], in_=Y[:, c, cols], func=AF.Square), reads=[ynames[c]], writes=[f"sq{s2i}"])
                            op("pe", lambda e, c=c: e.matmul(out=psN[n2][:], lhsT=onesb[:], rhs=sq[s2i][:], start=(c == 0), stop=(c == 3)),
                               reads=["onesb", f"sq{s2i}"], writes=[f"psN{n2}"])
                        op("act", lambda e: e.activation(out=rstd[:, cols], in_=psN[n2][:], func=AF.Sqrt, bias=epsb[:, 0:1], scale=1.0 / 512),
                           reads=[f"psN{n2}", "epsb"], writes=[f"rstd{tb}"])
                        op("dve", lambda e: e.reciprocal(out=rstd[:, cols], in_=rstd[:, cols]), reads=[f"rstd{tb}"], writes=[f"rstd{tb}"])
                        for c in range(4):
                            op("dve", lambda e, c=c: e.scalar_tensor_tensor(out=ynT[:, goff + c, cols], in0=Y[:, c, cols], scalar=gcf_sb[:, goff + c:goff + c + 1],
                                                                          in1=rstd[:, cols], op0=ALU.mult, op1=ALU.mult),
                               reads=[ynames[c], "gcf", f"rstd{tb}"], writes=[f"ynT{goff + c}"])

                branch_norm(BT, [f"BT{c}" for c in range(4)], 0)
                if dbg:
                    dump("yc", [128, 4, TOK], F32, BT[:], [f"BT{c}" for c in range(4)])
        S.barrier()
        Wo_ = sb("Wout", [128, 8, D], BF16, sA)
        load_w("Wout", w_out, Wo_[:], D)
        S.barrier()
        with ExitStack() as s3:
            Us = sb("Us", [128, 64, 512], BF16, s3)
            f1 = sb("f1", [128, 2, 128], BF16, s3)
            dma("pool", "f1", lambda e: e.dma_start(out=f1[:, 0, :], in_=f1c), writes=["f1"])
            dma("pool", "f1", lambda e: e.dma_start(out=f1[:, 1, :], in_=f1s), writes=["f1"])
            Ast = [sb(f"Ast_{i}", [128, 2, 4, 512], BF16, s3) for i in range(2)]
            psA = [ps(f"psA_{i}", [128, 512], F32, s3) for i in range(4)]
            for h in range(4):
                dma("sp", f"Us{h}", lambda e, h=h: e.dma_start(out=Us[:, h * 16:(h + 1) * 16, :],
                                                          in_=U_d.rearrange("(s1 s2) c -> s1 s2 c", s2=64)[:, h * 16:(h + 1) * 16, :]),
                    reads=[f"U_d{i}" for i in range(64)], writes=[f"Us{h}"])
            zfill_burst(1)
            A_v = A_d.rearrange("r s k c -> k r s c")
            for sblk in range(16):
                a2 = sblk % 2
                for sl in range(4):
                    s2_ = sblk * 4 + sl
                    for ri in range(2):
                        p4 = (s2_ * 2 + ri) % 4
                        op("pe", lambda e, ri=ri, s2_=s2_: e.matmul(out=psA[p4][:], lhsT=f1[:, ri, :], rhs=Us[:, s2_, :], start=True, stop=True),
                           reads=["f1", f"Us{s2_ // 16}"], writes=[f"psA{p4}"])
                        if ri == 0:
                            op("act", lambda e, sl=sl: e.copy(out=Ast[a2][:, 0, sl, :], in_=psA[p4][:]), reads=[f"psA{p4}"], writes=[f"Ast{a2}"])
                        else:
                            op("dve", lambda e, sl=sl: e.tensor_copy(out=Ast[a2][:, 1, sl, :], in_=psA[p4][:]), reads=[f"psA{p4}"], writes=[f"Ast{a2}"])
                for ri in range(2):
                    dma("pool", f"Ast{a2}_{ri}", lambda e, ri=ri, sblk=sblk: e.dma_start(out=A_v[:, ri, sblk * 4:(sblk + 1) * 4, :], in_=Ast[a2][:, ri, :, :]),
                        reads=[f"Ast{a2}"], writes=[f"A_d{sblk}_{ri}"])
        S.barrier()
        with ExitStack() as s4:
            f3_sb = sb("f3_sb", [128, 128, 32], BF16, s4)
            dma("pool", "f3", lambda e: e.dma_start(out=f3_sb[:], in_=f3), writes=["f3"])
            bd_sb = sb("bd_sb", [128, 2, 128], BF16, s4)
            dma("pool", "bd", lambda e: e.dma_start(out=bd_sb[:, 0, :], in_=bdc), writes=["bd"])
            dma("pool", "bd", lambda e: e.dma_start(out=bd_sb[:, 1, :], in_=bds), writes=["bd"])
            zfill_burst(2)
            Ach = [sb(f"Ach_{i}", [128, 16, 512], BF16, s4) for i in range(2)]
            XT = sb("XT", [128, 4, 128, 32], BF16, s4)
            yf = sb("yf", [128, 4, TOK], F32, s4)
            sq = [sb(f"sqf_{i}", [128, 512], BF16, s4) for i in range(2)]
            rstd = sb("rstdf", [128, TOK], F32, s4)
            psX = [ps(f"psX_{i}", [128, 16, 32], F32, s4) for i in range(4)]
            psY = [ps(f"psY_{i}", [128, 32, 16], F32, s4) for i in range(2)]
            psN = [ps(f"psNf_{i}", [128, 512], F32, s4) for i in range(2)]
            A_r = A_d.rearrange("r s k c -> (r s) k c")
            for kc in range(8):
                a2 = kc % 2
                dma("sp", f"Ach{a2}", lambda e, kc=kc: e.dma_start(out=Ach[a2][:], in_=A_r[:, kc * 16:(kc + 1) * 16, :]), reads=[f"A_d{sb_}_{ri_}" for sb_ in range(16) for ri_ in range(2)], writes=[f"Ach{a2}"])
                for cc in range(4):
                    for kl in range(16):
                        k1 = kc * 16 + kl
                        op("pe", lambda e, cc=cc, kl=kl, k1=k1: e.matmul(out=psX[cc][:, kl, :], lhsT=Ach[a2][:, kl, cc * 128:(cc + 1) * 128], rhs=f3_sb[:, k1, :],
                                                                       start=True, stop=True),
                           reads=[f"Ach{a2}", "f3"], writes=[f"psX{cc}"], inc=(kl == 15))
                    if cc % 2 == 0:
                        op("act", lambda e, cc=cc, kc=kc: e.copy(out=XT[:, cc, kc * 16:(kc + 1) * 16, :], in_=psX[cc][:]), reads=[f"psX{cc}"], writes=[f"XT{cc}"])
                    else:
                        op("dve", lambda e, cc=cc, kc=kc: e.tensor_copy(out=XT[:, cc, kc * 16:(kc + 1) * 16, :], in_=psX[cc][:]), reads=[f"psX{cc}"], writes=[f"XT{cc}"])
            scale = 1.0 / float(np.sqrt(8192.0 * 64.0))
            for cc in range(4):
                yv = yf[:, cc, :].rearrange("p (k2 k1) -> p k1 k2", k1=128)
                for kq in range(4):
                    y2 = (cc * 4 + kq) % 2
                    op("pe", lambda e, cc=cc, kq=kq: e.matmul(out=psY[y2][:], lhsT=bd_sb[:, 0, :], rhs=XT[:, cc, kq * 32:(kq + 1) * 32, 0:16], start=True, stop=False),
                       reads=["bd", f"XT{cc}"], writes=[f"psY{y2}"], inc=False)
                    op("pe", lambda e, cc=cc, kq=kq: e.matmul(out=psY[y2][:], lhsT=bd_sb[:, 1, :], rhs=XT[:, cc, kq * 32:(kq + 1) * 32, 16:32], start=False, stop=True),
                       reads=["bd", f"XT{cc}"], writes=[f"psY{y2}"])
                    op("act", lambda e, kq=kq, yv=yv: e.activation(out=yv[:, kq * 32:(kq + 1) * 32, :], in_=psY[y2][:], func=AF.Copy, scale=scale),
                       reads=[f"psY{y2}"], writes=[f"yf{cc}"])
            branch_norm_names = [f"yf{c}" for c in range(4)]
            for tb in range(4):
                cols = slice(tb * 512, (tb + 1) * 512)
                n2 = tb % 2
                for c in range(4):
                    s2i = (tb * 4 + c) % 2
                    op("act", lambda e, c=c: e.activation(out=sq[s2i][:], in_=yf[:, c, cols], func=AF.Square), reads=[f"yf{c}"], writes=[f"sqf{s2i}"])
                    op("pe", lambda e, c=c: e.matmul(out=psN[n2][:], lhsT=onesb[:], rhs=sq[s2i][:], start=(c == 0), stop=(c == 3)),
                       reads=["onesb", f"sqf{s2i}"], writes=[f"psNf{n2}"])
                op("act", lambda e: e.activation(out=rstd[:, cols], in_=psN[n2][:], func=AF.Sqrt, bias=epsb[:, 0:1], scale=1.0 / 512),
                   reads=[f"psNf{n2}", "epsb"], writes=[f"rstdf{tb}"])
                op("dve", lambda e: e.reciprocal(out=rstd[:, cols], in_=rstd[:, cols]), reads=[f"rstdf{tb}"], writes=[f"rstdf{tb}"])
                for c in range(4):
                    op("dve", lambda e, c=c: e.scalar_tensor_tensor(out=ynT[:, 4 + c, cols], in0=yf[:, c, cols], scalar=gcf_sb[:, 4 + c:5 + c],
                                                                  in1=rstd[:, cols], op0=ALU.mult, op1=ALU.mult),
                       reads=[f"yf{c}", "gcf", f"rstdf{tb}"], writes=[f"ynT{4 + c}"])
            if dbg:
                dump("yf", [128, 4, TOK], F32, yf[:], branch_norm_names)
        S.barrier()
        with ExitStack() as s5:
            zfill_burst(3)
            xb = [sb(f"xr_{i}", [128, D], F32, s5) for i in range(2)]
            psW = [ps(f"psW_{i}", [128, 512], F32, s5) for i in range(4)]
            yn_names = [f"ynT{c}" for c in range(8)]
            for i in range(NT):
                k2 = i % 2
                dma("sp", f"xr{k2}", lambda e, i=i: e.dma_start(out=xb[k2][:], in_=xrot[i * 128:(i + 1) * 128, :]), writes=[f"xr{k2}"])
                for dh in range(2):
                    p4 = (i * 2 + dh) % 4
                    for c in range(8):
                        op("pe", lambda e, c=c, dh=dh, i=i: e.matmul(out=psW[p4][:], lhsT=ynT[:, c, i * 128:(i + 1) * 128], rhs=Wo_[:, c, dh * 512:(dh + 1) * 512],
                                                                   start=(c == 0), stop=(c == 7)),
                           reads=yn_names + ["Wout"], writes=[f"psW{p4}"], inc=(c == 7))
                    op("dve", lambda e, dh=dh: e.tensor_tensor(out=xb[k2][:, dh * 512:(dh + 1) * 512], in0=psW[p4][:], in1=xb[k2][:, dh * 512:(dh + 1) * 512], op=ALU.add),
                       reads=[f"psW{p4}", f"xr{k2}"], writes=[f"xr{k2}"])
                dma("pool", f"x1st{k2}", lambda e, i=i: e.dma_start(out=X1_d[i * 128:(i + 1) * 128, :], in_=xb[k2][:]), reads=[f"xr{k2}"], writes=[f"X1_d{i}"])

    if stage == "A":
        dma("sp", "fin", lambda e: e.dma_start(out=out, in_=X1_d), reads=[f"X1_d{i}" for i in range(NT)], writes=["out"])
        S.finish("sp", ["out"] + ["dbg_" + n for n in dbg_outs])
        return nc, es, dbg_outs

    S.barrier()
    with ExitStack() as sM:
        bc_reg = nc.gpsimd.to_reg(NSLOT - 1)
        IDX = sb("IDX", [128, NT, 4], I32, sM)
        WK = sb("WK", [128, NT, 4], F32, sM)
        S.nobar.update(["Wgu0", "Wgu1", "Wdn0", "Wdn1", "zfill", "bgu"])
        if full:
            Wgu = [sb(f"Wgu_{i}", [128, 8, 2048], BF16, sM) for i in range(2)]
            Wdn = [sb(f"Wdn_{i}", [128, 8, D], BF16, sM) for i in range(2)]
            bgu = sb("bgu", [128, NE, 16], F32, sM)
            dma("sp", "bgu", lambda e: e.dma_start(out=bgu[:], in_=b_gu), writes=["bgu"])

        def load_expert(e_):
            k = e_ % 2
            load_w(f"Wgu{k}", w_gu[e_], Wgu[k][:], 2048)
            load_w(f"Wdn{k}", w_dn[e_], Wdn[k][:], D)

        S.barrier()
        with ExitStack() as sB:
            KT = sb("KT", [128, 8, 256], BF16, sB)
            Vb = sb("Vb", [128, 2, D], BF16, sB)
            junk = sb("junkB", [128, D], BF16, sB)
            ss = sb("ssB", [128, 1], F32, sB)
            load_gain(2)
            load_gain(1)
            rs = sb("rsB", [128, 1], F32, sB)
            S.barrier()
            with ExitStack() as sK:
                Wk = sb("Wk", [128, 8, D], BF16, sK)
                Wv = sb("Wv", [128, 8, D], BF16, sK)
                load_w("Wk", w_k, Wk[:], D)
                load_w("Wv", w_v, Wv[:], D)
                mt_ = [sb(f"mt_{i}", [128, D], F32, sK) for i in range(2)]
                mb_ = [sb(f"mb_{i}", [128, D], BF16, sK) for i in range(2)]
                memT = sb("memT", [128, 8, 256], BF16, sK)
                psT = [ps(f"psTk_{i}", [128, 8, 128], BF16, sK) for i in range(2)]
                psK = [ps(f"psK_{i}", [128, 512], F32, sK) for i in range(2)]
                for m in range(2):
                    dma("sp", f"mt{m}", lambda e, m=m: e.dma_start(out=mt_[m][:], in_=memb[m * 128:(m + 1) * 128, :]), writes=[f"mt{m}"])
                    rmsnorm_tile(mt_[m][:], f"mt{m}", 2, mb_[m][:], f"mb{m}", ss, rs, junk)
                    for dc in range(8):
                        op("pe", lambda e, dc=dc, m=m: e.transpose(out=psT[m][:, dc, :], in_=mb_[m][:, dc * 128:(dc + 1) * 128], identity=identb[:]),
                           reads=[f"mb{m}", "identb"], writes=[f"psTk{m}"], inc=(dc == 7))
                    op("act", lambda e, m=m: e.copy(out=memT[:, :, m * 128:(m + 1) * 128], in_=psT[m][:]), reads=[f"psTk{m}"], writes=["memT"])
                for j in range(8):
                    k2 = j % 2
                    for dc in range(8):
                        op("pe", lambda e, j=j, dc=dc: e.matmul(out=psK[k2][:, 0:256], lhsT=Wk[:, dc, j * 128:(j + 1) * 128], rhs=memT[:, dc, :], start=(dc == 0), stop=(dc == 7)),
                           reads=["Wk", "memT"], writes=[f"psK{k2}"], inc=(dc == 7))
                    op("act", lambda e, j=j: e.copy(out=KT[:, j, :], in_=psK[k2][:, 0:256]), reads=[f"psK{k2}"], writes=["KT"])
                for m in range(2):
                    for dh in range(2):
                        k2 = (m * 2 + dh) % 2
                        for dc in range(8):
                            op("pe", lambda e, m=m, dh=dh, dc=dc: e.matmul(out=psK[k2][:], lhsT=memT[:, dc, m * 128:(m + 1) * 128], rhs=Wv[:, dc, dh * 512:(dh + 1) * 512],
                                                                         start=(dc == 0), stop=(dc == 7)),
                               reads=["Wv", "memT"], writes=[f"psK{k2}"], inc=(dc == 7))
                        op("dve", lambda e, m=m, dh=dh: e.tensor_copy(out=Vb[:, m, dh * 512:(dh + 1) * 512], in_=psK[k2][:]), reads=[f"psK{k2}"], writes=["Vb"])
            S.barrier()
            with ExitStack() as sQ:
                Wq = sb("Wq", [128, 8, D], BF16, sQ)
                Wo = sb("Wo", [128, 8, D], BF16, sQ)
                load_w("Wq", w_q, Wq[:], D)
                load_w("Wo", w_o, Wo[:], D)
                if full:
                    load_expert(0)
                    load_expert(1)
                xt = [sb(f"x1_{i}", [128, D], F32, sQ) for i in range(4)]
                hb = [sb(f"h2b_{i}", [128, D], BF16, sQ) for i in range(4)]
                hT4 = sb("h2T4", [128, 8, 512], BF16, sQ)
                QT4 = sb("QT4", [128, 8, 512], BF16, sQ)
                E = sb("E", [128, 4, 256], F32, sQ)
                Pb = sb("Pb", [128, 4, 256], BF16, sQ)
                PT = sb("PT", [128, 8, 128], BF16, sQ)
                OT = sb("OT", [128, 8, 128], BF16, sQ)
                mx = sb("mx", [128, 4], F32, sQ)
                nmx = sb("nmx", [128, 4], F32, sQ)
                sm = sb("sm", [128, 4], F32, sQ)
                rsm = sb("rsm", [128, 4], F32, sQ)
                psT = ps("psTq", [128, 8, 128], BF16, sQ)
                psQ = ps("psQ", [128, 512], F32, sQ)
                psS = ps("psS", [128, 4, 256], F32, sQ)
                psPT = ps("psPT", [128, 8, 128], BF16, sQ)
                psO = ps("psO", [128, 8, 128], F32, sQ)
                psW = ps("psWo", [128, 512], F32, sQ)
                Pb2 = [Pb, sb("Pb_1", [128, 4, 256], BF16, sQ)]

                def b_pro(grp):
                    for t in range(4):
                        i = grp * 4 + t
                        dma("sp", f"x1l{t}", lambda e, i=i, t=t: e.dma_start(out=xt[t][:], in_=X1_d[i * 128:(i + 1) * 128, :]), reads=[f"X1_d{i}"], writes=[f"x1_{t}"])
                        rmsnorm_tile(xt[t][:], f"x1_{t}", 1, hb[t][:], f"h2b{t}", ss, rs, junk)
                    for t in range(4):
                        k2 = t
                        for dc in range(8):
                            op("pe", lambda e, dc=dc, k2=k2: e.transpose(out=psT[:, dc, :], in_=hb[k2][:, dc * 128:(dc + 1) * 128], identity=identb[:]),
                               reads=[f"h2b{k2}", "identb"], writes=["psTq"], inc=(dc == 7))
                        op("act", lambda e, t=t: e.copy(out=hT4[:, :, t * 128:(t + 1) * 128], in_=psT[:]), reads=["psTq"], writes=["h2T4"])
                    for j in range(8):
                        pq, pqn = (psQ, "psQ") if j % 2 == 0 else (psW, "psWo")
                        for dc in range(8):
                            op("pe", lambda e, j=j, dc=dc, pq=pq: e.matmul(out=pq[:], lhsT=Wq[:, dc, j * 128:(j + 1) * 128], rhs=hT4[:, dc, :], start=(dc == 0), stop=(dc == 7)),
                               reads=["Wq", "h2T4"], writes=[pqn], inc=(dc == 7))
                        if j % 2 == 0:
                            op("act", lambda e, j=j, pq=pq: e.copy(out=QT4[:, j, :], in_=pq[:]), reads=[pqn], writes=["QT4"])
                        else:
                            op("dve", lambda e, j=j, pq=pq: e.tensor_copy(out=QT4[:, j, :], in_=pq[:]), reads=[pqn], writes=["QT4"])

                def b_s1(i):
                    t = i % 4
                    p2 = i % 2
                    tc_ = slice(t * 128, (t + 1) * 128)
                    for hh in range(4):
                        for hf in range(2):
                            op("pe", lambda e, hh=hh, hf=hf: e.matmul(out=psS[:, hh, :], lhsT=QT4[:, hh * 2 + hf, tc_], rhs=KT[:, hh * 2 + hf, :], start=(hf == 0), stop=(hf == 1)),
                               reads=["QT4", "KT"], writes=["psS"], inc=(hf == 1))
                    op("dve", lambda e: e.tensor_reduce(out=mx[:], in_=psS[:], axis=AX.X, op=ALU.max), reads=["psS"], writes=["mx"])
                    op("dve", lambda e: e.tensor_scalar(out=nmx[:], in0=mx[:], scalar1=-1.0 / 16.0, scalar2=None, op0=ALU.mult), reads=["mx"], writes=["nmx"])
                    for hh in range(4):
                        op("act", lambda e, hh=hh: e.activation(out=E[:, hh, :], in_=psS[:, hh, :], func=AF.Exp, bias=nmx[:, hh:hh + 1], scale=1.0 / 16.0,
                                                               accum_out=sm[:, hh:hh + 1]),
                           reads=["psS", "nmx"], writes=["E", "sm"])
                    op("dve", lambda e: e.reciprocal(out=rsm[:], in_=sm[:]), reads=["sm"], writes=["rsm"])
                    for hh in range(4):
                        op("dve", lambda e, hh=hh: e.tensor_scalar(out=Pb2[p2][:, hh, :], in0=E[:, hh, :], scalar1=rsm[:, hh:hh + 1], scalar2=None, op0=ALU.mult),
                           reads=["E", "rsm"], writes=[f"Pb{p2}"])

                def b_s2(i):
                    t = i % 4
                    p2 = i % 2
                    for hh in range(4):
                        for m in range(2):
                            op("pe", lambda e, hh=hh, m=m: e.transpose(out=psPT[:, hh * 2 + m, :], in_=Pb2[p2][:, hh, m * 128:(m + 1) * 128], identity=identb[:]),
                               reads=[f"Pb{p2}", "identb"], writes=["psPT"], inc=(hh == 3 and m == 1))
                    op("act", lambda e: e.copy(out=PT[:], in_=psPT[:]), reads=["psPT"], writes=["PT"])
                    for hh in range(4):
                        for hf in range(2):
                            c = hh * 2 + hf
                            for m in range(2):
                                op("pe", lambda e, hh=hh, m=m, c=c: e.matmul(out=psO[:, c, :], lhsT=Vb[:, m, c * 128:(c + 1) * 128], rhs=PT[:, hh * 2 + m, :],
                                                                           start=(m == 0), stop=(m == 1)),
                                   reads=["Vb", "PT"], writes=["psO"], inc=(c == 7 and m == 1))
                    op("act", lambda e: e.copy(out=OT[:], in_=psO[:]), reads=["psO"], writes=["OT"])
                    for dh in range(2):
                        pw, pwn = (psW, "psWo") if dh == 0 else (psQ, "psQ")
                        for c in range(8):
                            op("pe", lambda e, c=c, dh=dh, pw=pw: e.matmul(out=pw[:], lhsT=OT[:, c, :], rhs=Wo[:, c, dh * 512:(dh + 1) * 512], start=(c == 0), stop=(c == 7)),
                               reads=["OT", "Wo"], writes=[pwn], inc=(c == 7))
                        op("dve", lambda e, dh=dh, pw=pw: e.tensor_tensor(out=xt[t][:, dh * 512:(dh + 1) * 512], in0=pw[:], in1=xt[t][:, dh * 512:(dh + 1) * 512], op=ALU.add),
                           reads=[pwn, f"x1_{t}"], writes=[f"x1_{t}"])
                    dma("pool", f"x2st{t}", lambda e: e.dma_start(out=X2_d[i * 128:(i + 1) * 128, :], in_=xt[t][:]), reads=[f"x1_{t}"], writes=[f"X2_d{i}"])

                for grp in range(4):
                    b_pro(grp)
                    for t in range(4):
                        b_s1(grp * 4 + t)
                        if t > 0:
                            b_s2(grp * 4 + t - 1)
                    b_s2(grp * 4 + 3)

        if stage == "B":
            dma("sp", "fin", lambda e: e.dma_start(out=out, in_=X2_d), reads=[f"X2_d{i}" for i in range(NT)], writes=["out"])
            S.finish("sp", ["out"] + ["dbg_" + n for n in dbg_outs])
            return nc, es, dbg_outs


        S.barrier()
        with ExitStack() as sC:
            Wr = sb("Wr", [128, 8, NE], F32, sC)
            dma("sp", "Wr", lambda e: e.dma_start(out=Wr[:], in_=w_r.rearrange("(dc p) f -> p dc f", p=128)), writes=["Wr"])
            brb = sb("brb", [128, NE], F32, sC)
            dma("sp", "brb", lambda e: e.dma_start(out=brb[:], in_=b_r.partition_broadcast(128)), writes=["brb"])
            eb1 = sb("eb1", [128, NE], F32, sC)
            dma("sp", "eb1", lambda e: e.dma_start(out=eb1[:], in_=ebase), writes=["eb1"])
            load_gain(3)
            masks = sb("masks", [128, NT, NE], BF16, sC)
            xt = [sb(f"x2_{i}", [128, D], F32, sC) for i in range(2)]
            hf = [sb(f"h3f_{i}", [128, D], F32, sC) for i in range(2)]
            hb = [sb(f"h3b_{i}", [128, D], BF16, sC) for i in range(4)]
            hT = sb("h3T", [128, 8, 128], F32, sC)
            junk = sb("junkC", [128, D], BF16, sC)
            ss = sb("ssC", [128, 1], F32, sC)
            rs = sb("rsC", [128, 1], F32, sC)
            lg = sb("lg", [128, NE], F32, sC)
            m8 = sb("m8", [128, 8], F32, sC)
            nm = sb("nm", [128, 1], F32, sC)
            mk = sb("mk", [128, NE], F32, sC)
            ex = sb("ex", [128, NE], F32, sC)
            em = sb("em", [128, NE], F32, sC)
            sme = sb("sme", [128, 1], F32, sC)
            wt = sb("wt", [128, NE], F32, sC)
            key = sb("key", [128, NE], F32, sC)
            k8 = sb("k8", [128, 8], F32, sC)
            eq = sb("eq", [128, NE], F32, sC)
            psT = [ps(f"psTr_{i}", [128, 4, 128], F32, sC) for i in range(2)]
            psL = ps("psL", [128, NE], F32, sC)
            psP = ps("psP", [128, NE], F32, sC)
            def c_s0(i):
                k2 = i % 2
                k4 = i % 4
                dma("sp", f"x2l{k2}", lambda e: e.dma_start(out=xt[k2][:], in_=X2_d[i * 128:(i + 1) * 128, :]), reads=[f"X2_d{i}"], writes=[f"x2_{k2}"])
                rmsnorm_tile(xt[k2][:], f"x2_{k2}", 3, hb[k4][:], f"h3b{k4}", ss, rs, junk, hf=hf[k2][:], hfname=f"h3f{k2}")

            def c_s1(i):
                k2 = i % 2
                k4 = i % 4
                for dc in range(8):
                    op("pe", lambda e, dc=dc: e.transpose(out=psT[dc // 4][:, dc % 4, :], in_=hf[k2][:, dc * 128:(dc + 1) * 128], identity=identf[:]),
                       reads=[f"h3f{k2}", "identf"], writes=[f"psTr{dc // 4}"], inc=(dc % 4 == 3))
                op("act", lambda e: e.copy(out=hT[:, 0:4, :], in_=psT[0][:]), reads=["psTr0"], writes=["h3Ta"])
                op("dve", lambda e: e.tensor_copy(out=hT[:, 4:8, :], in_=psT[1][:]), reads=["psTr1"], writes=["h3Tb"])
                for dc in range(8):
                    op("pe", lambda e, dc=dc: e.matmul(out=psL[:], lhsT=hT[:, dc, :], rhs=Wr[:, dc, :], start=(dc == 0), stop=(dc == 7)),
                       reads=["h3Ta", "h3Tb", "Wr"], writes=["psL"], inc=(dc == 7))
                op("dve", lambda e: e.tensor_tensor(out=lg[:], in0=psL[:], in1=brb[:], op=ALU.add), reads=["psL", "brb"], writes=["lg"])
                op("dve", lambda e: e.max(out=m8[:], in_=lg[:]), reads=["lg"], writes=["m8"])
                op("dve", lambda e: e.tensor_scalar(out=mk[:], in0=lg[:], scalar1=m8[:, 3:4], scalar2=None, op0=ALU.is_ge), reads=["lg", "m8"], writes=["mk"])
                op("dve", lambda e, i=i: e.tensor_copy(out=masks[:, i, :], in_=mk[:]), reads=["mk"], writes=[f"masks{i}"])
                op("dve", lambda e: e.tensor_scalar(out=nm[:], in0=m8[:, 0:1], scalar1=-1.0, scalar2=None, op0=ALU.mult), reads=["m8"], writes=["nm"])
                op("act", lambda e: e.activation(out=ex[:], in_=lg[:], func=AF.Exp, bias=nm[:, 0:1], scale=1.0), reads=["lg", "nm"], writes=["ex"])
                op("dve", lambda e: e.tensor_tensor(out=em[:], in0=ex[:], in1=mk[:], op=ALU.mult), reads=["ex", "mk"], writes=["em"])
                op("dve", lambda e: e.reduce_sum(out=sme[:], in_=em[:], axis=AX.X), reads=["em"], writes=["sme"])
                op("dve", lambda e: e.reciprocal(out=sme[:], in_=sme[:]), reads=["sme"], writes=["sme"])
                op("dve", lambda e: e.tensor_scalar(out=wt[:], in0=em[:], scalar1=sme[:, 0:1], scalar2=None, op0=ALU.mult), reads=["em", "sme"], writes=["wt"])
                op("pe", lambda e, i=i: e.matmul(out=psP[:], lhsT=ltri[:], rhs=masks[:, i, :], start=True, stop=(i == 0)),
                   reads=["ltri", f"masks{i}"], writes=["psP"], inc=(i == 0))
                for j in range(i):
                    op("pe", lambda e, j=j, i=i: e.matmul(out=psP[:], lhsT=onesb[:], rhs=masks[:, j, :], start=False, stop=(j == i - 1)),
                       reads=["onesb", f"masks{j}"], writes=["psP"], inc=(j == i - 1))
                op("dve", lambda e: e.tensor_tensor(out=key[:], in0=psP[:], in1=eb1[:], op=ALU.add), reads=["psP", "eb1"], writes=["key"])
                op("dve", lambda e: e.tensor_tensor(out=key[:], in0=key[:], in1=mk[:], op=ALU.mult), reads=["key", "mk"], writes=["key"])
                op("dve", lambda e: e.max(out=k8[:], in_=key[:]), reads=["key"], writes=["k8"])
                op("dve", lambda e, i=i: e.tensor_scalar(out=IDX[:, i, :], in0=k8[:, 0:4], scalar1=-1.0, scalar2=None, op0=ALU.add), reads=["k8"], writes=[f"IDX{i}"])
                for k in range(4):
                    op("dve", lambda e, k=k: e.tensor_scalar(out=eq[:], in0=key[:], scalar1=k8[:, k:k + 1], scalar2=None, op0=ALU.is_equal), reads=["key", "k8"], writes=["eq"])
                    op("dve", lambda e: e.tensor_tensor(out=eq[:], in0=eq[:], in1=wt[:], op=ALU.mult), reads=["eq", "wt"], writes=["eq"])
                    op("dve", lambda e, k=k, i=i: e.reduce_sum(out=WK[:, i, k:k + 1], in_=eq[:], axis=AX.X), reads=["eq"], writes=[f"WK{i}"])
                for k in range(4):
                    S._deps("pool", ["Xd"], [])
                    dma("pool", f"disp{k4}_{k}", lambda e, k=k, i=i: e.indirect_dma_start(out=Xd, out_offset=bass.IndirectOffsetOnAxis(ap=IDX[:, i, k:k + 1], axis=0),
                                                                                      in_=hb[k4][:, :], in_offset=None, bounds_check=bc_reg, oob_is_err=False),
                        reads=[f"h3b{k4}", f"IDX{i}"], writes=[f"Xd_disp{k4}_{k}"])

            c_s0(0)
            for i in range(NT):
                if i + 1 < NT:
                    c_s0(i + 1)
                c_s1(i)
        S.barrier()
        with ExitStack() as sD:
            Xe = [sb(f"Xe_{i}", [128, 3, D], BF16, sD) for i in range(2)]
            XTe = [sb(f"XTe_{i}", [128, 8, CAP], BF16, sD) for i in range(2)]
            actT = [sb(f"actT_{i}", [128, 8, CAP], BF16, sD) for i in range(2)]
            bdb = [sb(f"bdb_{i}", [128, D], F32, sD) for i in range(2)]
            Oe = [sb(f"Oe_{i}", [128, 3, D], F32, sD) for i in range(2)]
            g_ = [sb(f"g_{i}", [128, CAP], F32, sD) for i in range(2)]
            sg_ = [sb(f"sg_{i}", [128, CAP], F32, sD) for i in range(2)]
            u_ = [sb(f"u_{i}", [128, CAP], F32, sD) for i in range(2)]
            psXT = [ps(f"psXT_{i}", [128, 3, 128], BF16, sD) for i in range(2)]
            psG = [ps(f"psG_{i}", [128, 512], F32, sD) for i in range(2)]
            psUp = [ps(f"psUp_{i}", [128, 512], F32, sD) for i in range(2)]
            psD = [ps(f"psD_{i}", [128, 512], F32, sD) for i in range(2)]
            def ex_load(e_):
                k = e_ % 2
                dma("sp", f"Xe{k}", lambda e: e.dma_start(out=Xe[k][:], in_=Xd[e_ * CAP:(e_ + 1) * CAP, :].rearrange("(b p) d -> p b d", p=128)),
                    reads=["Xd"] + [f"Xd_disp{a_}_{b_}" for a_ in range(4) for b_ in range(4)], writes=[f"Xe{k}"])
                dma("sp", f"bdb{k}", lambda e: e.dma_start(out=bdb[k][:], in_=b_dn[e_:e_ + 1, :].partition_broadcast(128)), writes=[f"bdb{k}"])

            def ex_tr(e_):
                k = e_ % 2
                for dc in range(8):
                    x2 = dc % 2
                    for b in range(3):
                        op("pe", lambda e, dc=dc, b=b: e.transpose(out=psXT[x2][:, b, :], in_=Xe[k][:, b, dc * 128:(dc + 1) * 128], identity=identb[:]),
                           reads=[f"Xe{k}", "identb"], writes=[f"psXT{x2}"], inc=(b == 2))
                    if dc % 2 == 0:
                        op("act", lambda e, dc=dc: e.copy(out=XTe[k][:, dc, :], in_=psXT[x2][:]), reads=[f"psXT{x2}"], writes=[f"XTe{k}"])
                    else:
                        op("dve", lambda e, dc=dc: e.tensor_copy(out=XTe[k][:, dc, :], in_=psXT[x2][:]), reads=[f"psXT{x2}"], writes=[f"XTe{k}"])

            def ex_gu(e_):
                k = e_ % 2
                for j in range(8):
                    j2 = j % 2
                    for dc in range(8):
                        op("pe", lambda e, j=j, dc=dc: e.matmul(out=psG[j2][:, 0:CAP], lhsT=Wgu[k][:, dc, j * 128:(j + 1) * 128], rhs=XTe[k][:, dc, :], start=(dc == 0), stop=(dc == 7)),
                           reads=[f"Wgu{k}", f"XTe{k}"], writes=[f"psG{j2}"], inc=(dc == 7))
                    for dc in range(8):
                        op("pe", lambda e, j=j, dc=dc: e.matmul(out=psUp[j2][:, 0:CAP], lhsT=Wgu[k][:, dc, 1024 + j * 128:1024 + (j + 1) * 128], rhs=XTe[k][:, dc, :],
                                                               start=(dc == 0), stop=(dc == 7)),
                           reads=[f"Wgu{k}", f"XTe{k}"], writes=[f"psUp{j2}"], inc=(dc == 7))
                    op("dve", lambda e, j=j: e.tensor_scalar(out=g_[j2][:], in0=psG[j2][:, 0:CAP], scalar1=bgu[:, e_, j:j + 1], scalar2=7.0, op0=ALU.add, op1=ALU.min),
                       reads=[f"psG{j2}", "bgu"], writes=[f"g{j2}"])
                    op("act", lambda e: e.activation(out=sg_[j2][:], in_=g_[j2][:], func=AF.Silu, scale=1.702), reads=[f"g{j2}"], writes=[f"sg{j2}"])
                    op("act", lambda e, j=j: e.activation(out=u_[j2][:], in_=psUp[j2][:, 0:CAP], func=AF.Identity, bias=bgu[:, e_, 8 + j:9 + j], scale=1.0),
                       reads=[f"psUp{j2}", "bgu"], writes=[f"u{j2}"])
                    op("dve", lambda e: e.tensor_scalar(out=u_[j2][:], in0=u_[j2][:], scalar1=7.0, scalar2=-7.0, op0=ALU.min, op1=ALU.max), reads=[f"u{j2}"], writes=[f"u{j2}"])
                    op("dve", lambda e, j=j: e.scalar_tensor_tensor(out=actT[k][:, j, :], in0=u_[j2][:], scalar=1.0, in1=sg_[j2][:], op0=ALU.add, op1=ALU.mult),
                       reads=[f"sg{j2}", f"u{j2}"], writes=[f"actT{k}"])

            def ex_dn(e_):
                k = e_ % 2
                for b in range(3):
                    for dh in range(2):
                        d2 = (b * 2 + dh) % 2
                        for j in range(8):
                            op("pe", lambda e, b=b, dh=dh, j=j: e.matmul(out=psD[d2][:], lhsT=actT[k][:, j, b * 128:(b + 1) * 128], rhs=Wdn[k][:, j, dh * 512:(dh + 1) * 512],
                                                                       start=(j == 0), stop=(j == 7)),
                               reads=[f"actT{k}", f"Wdn{k}"], writes=[f"psD{d2}"], inc=(j == 7))
                        op("dve", lambda e, b=b, dh=dh: e.scalar_tensor_tensor(out=Oe[k][:, b, dh * 512:(dh + 1) * 512], in0=psD[d2][:], scalar=1.0 / 1.702,
                                                                                in1=bdb[k][:, dh * 512:(dh + 1) * 512], op0=ALU.mult, op1=ALU.add),
                           reads=[f"psD{d2}", f"bdb{k}"], writes=[f"Oe{k}"])
                dma("sp", f"Oest{k}", lambda e: e.dma_start(out=O_d[e_ * CAP:(e_ + 1) * CAP, :].rearrange("(b p) d -> p b d", p=128), in_=Oe[k][:]),
                    reads=[f"Oe{k}"], writes=["O_d"])

            ex_load(0)
            ex_tr(0)
            for e_ in range(NE):
                if e_ + 1 < NE:
                    ex_load(e_ + 1)
                ex_gu(e_)
                if e_ + 1 < NE:
                    ex_tr(e_ + 1)
                ex_dn(e_)
                if e_ + 2 < NE:
                    load_expert(e_ + 2)
        S.barrier()
        with ExitStack() as sE:
            xt = [sb(f"x2c_{i}", [128, D], F32, sE) for i in range(2)]
            G = [sb(f"G_{i}", [128, D], F32, sE) for i in range(4)]
            ob = [sb(f"ob_{i}", [128, D], F32, sE) for i in range(2)]
            load_gain(4)
            junk = sb("junkE", [128, D], BF16, sE)
            ss = sb("ssE", [128, 1], F32, sE)
            rs = sb("rsE", [128, 1], F32, sE)
            G8 = G + [sb(f"G_{i}", [128, D], F32, sE) for i in range(4, 8)]

            def e_s0(i):
                k2 = i % 2
                dma("sp", f"x2c{k2}", lambda e: e.dma_start(out=xt[k2][:], in_=X2_d[i * 128:(i + 1) * 128, :]), reads=[f"X2_d{i}"], writes=[f"x2c{k2}"])
                for k in range(4):
                    gi = k2 * 4 + k
                    dma("pool", f"G{gi}", lambda e, k=k, gi=gi: e.indirect_dma_start(out=G8[gi][:, :], out_offset=None, in_=O_d,
                                                                                  in_offset=bass.IndirectOffsetOnAxis(ap=IDX[:, i, k:k + 1], axis=0),
                                                                                  bounds_check=bc_reg, oob_is_err=False),
                        reads=["O_d", f"IDX{i}"], writes=[f"G{gi}"])

            def e_s1(i):
                k2 = i % 2
                for k in range(4):
                    gi = k2 * 4 + k
                    op("dve", lambda e, k=k, gi=gi: e.scalar_tensor_tensor(out=xt[k2][:], in0=G8[gi][:], scalar=WK[:, i, k:k + 1], in1=xt[k2][:], op0=ALU.mult, op1=ALU.add),
                       reads=[f"G{gi}", f"WK{i}", f"x2c{k2}"], writes=[f"x2c{k2}"])
                op("act", lambda e: e.activation(out=junk[:], in_=xt[k2][:], func=AF.Square, accum_out=ss[:, 0:1]), reads=[f"x2c{k2}"], writes=["junkE", "ssE"])
                op("act", lambda e: e.activation(out=rs[:, 0:1], in_=ss[:, 0:1], func=AF.Sqrt, bias=epsb[:, 0:1], scale=1.0 / D), reads=["ssE", "epsb"], writes=["rsE"])
                op("dve", lambda e: e.reciprocal(out=rs[:, 0:1], in_=rs[:, 0:1]), reads=["rsE"], writes=["rsE"])
                op("dve", lambda e: e.scalar_tensor_tensor(out=ob[k2][:], in0=xt[k2][:], scalar=rs[:, 0:1], in1=gv[:, GSLOT[4], :], op0=ALU.mult, op1=ALU.mult),
                   reads=[f"x2c{k2}", "rsE", f"gv{GSLOT[4]}"], writes=[f"ob{k2}"])
                dma("sp", f"ost{k2}", lambda e: e.dma_start(out=out[i * 128:(i + 1) * 128, :], in_=ob[k2][:]), reads=[f"ob{k2}"], writes=[f"out{i}"])

            e_s0(0)
            for i in range(NT):
                if i + 1 < NT:
                    e_s0(i + 1)
                e_s1(i)
    S.finish("sp", [f"out{i}" for i in range(NT)] + ["dbg_" + n for n in dbg_outs])
    return nc, es, dbg_outs


def host_inputs(inputs, stage="full", cores=range(8)):
    f = np.float32
    x = np.asarray(inputs["x"], f)
    mem = np.asarray(inputs["mem"], f)
    gvec = np.stack([inputs["norm_mix"][0], inputs["norm_xattn"][0], inputs["norm_mem"][0], inputs["norm_ffn"][0], inputs["norm_final"]]).astype(f)
    cwv = np.asarray(inputs["conv_w"][0], f)
    cw = np.ascontiguousarray(cwv.reshape(3, 4, 128).transpose(2, 1, 0))
    gc = np.asarray(inputs["g_conv_out"][0], f).reshape(4, 128).T
    gf = np.asarray(inputs["g_fft_out"][0], f).reshape(4, 128).T
    gcf = np.ascontiguousarray(np.concatenate([gc, gf], axis=1))
    a = np.arange(128)
    f1c = np.cos(2 * np.pi * np.outer(a, a) / 128).astype(f)
    f1s = (-np.sin(2 * np.pi * np.outer(a, a) / 128)).astype(f)
    c64 = np.arange(64)
    C64 = np.cos(2 * np.pi * np.outer(c64, c64) / 64)
    S64 = np.sin(2 * np.pi * np.outer(c64, c64) / 64)
    bdc = np.zeros((128, 128)); bds = np.zeros((128, 128))
    for g in range(2):
        bdc[g * 64:(g + 1) * 64, g * 64:(g + 1) * 64] = C64
        bds[g * 64:(g + 1) * 64, g * 64:(g + 1) * 64] = S64
    ebase = np.broadcast_to((np.arange(NE) * CAP + 1).astype(f), (128, NE)).copy()
    common = {
        "gvec": gvec, "w_in": np.asarray(inputs["w_in"][0], f), "cw": cw, "gcf": gcf,
        "w_out": np.asarray(inputs["w_out"][0], f), "w_q": np.asarray(inputs["w_q"][0], f), "w_k": np.asarray(inputs["w_k"][0], f),
        "w_v": np.asarray(inputs["w_v"][0], f), "w_o": np.asarray(inputs["w_o"][0], f), "w_r": np.asarray(inputs["w_router"][0], f),
        "b_r": np.asarray(inputs["b_router"], f).reshape(1, NE), "f1c": f1c, "f1s": f1s, "bdc": bdc.astype(f), "bds": bds.astype(f), "ebase": ebase,
    }
    if stage == "full":
        common["w_gu"] = np.asarray(inputs["w_gate_up"][0], f)
        common["b_gu"] = np.ascontiguousarray(np.asarray(inputs["b_gate_up"][0], f).reshape(NE, 16, 128).transpose(2, 0, 1))
        common["w_dn"] = np.asarray(inputs["w_down"][0], f)
        common["b_dn"] = np.asarray(inputs["b_down"][0], f)
    maps = []
    s2 = np.arange(64)
    k1 = np.arange(128)
    for c in cores:
        b, q = c // 4, c % 4
        xr = np.roll(x[b], -TOK * q, axis=0)
        xh = np.zeros((128, D), f)
        if q > 0:
            xh[0] = x[b, TOK * q - 1]
        if q < 3:
            xh[1] = x[b, TOK * (q + 1)]
        k2 = 16 * q + np.arange(16)
        kk = k1[:, None] + 128 * k2[None, :]
        phi = 2 * np.pi * s2[:, None, None] * kk[None] / 8192.0 + np.pi * kk[None] * q / 2.0
        gcos, gsin = np.cos(phi), np.sin(phi)
        t = np.zeros((2, 64, 128, 32))
        t[0, :, :, 0:16] = gcos; t[1, :, :, 0:16] = gsin
        t[0, :, :, 16:32] = -gsin; t[1, :, :, 16:32] = gcos
        m = dict(common)
        m.update({"xrot": np.ascontiguousarray(xr), "xhalo": xh, "memb": np.ascontiguousarray(mem[b]), "f3": t.reshape(128, 128, 32).astype(f)})
        maps.append(m)
    return maps


def kernel(**inputs):
    nc, es, _ = build("full")
    maps = host_inputs(inputs)
    res = run_bass_kernel_spmd(nc, maps, core_ids=list(range(8)))
    outs = [np.asarray(r["out"], np.float32) for r in res.results]
    y = np.stack(outs).reshape(2, 4 * TOK, D)
    return y
```
